# Optimizing a Trainium2 kernel written in Bass

```python
import math
import jax, jax.numpy as jnp
from jax import lax
import numpy as np

D_MODEL = 1024
BATCH = 8
SEQ = 4096
DEPTH = 1

PLE_DIM = 256
RMS_EPS = 1e-6

RWKV_HEADS = 8
RWKV_HEAD_DIM = 64
RWKV_WIDTH = RWKV_HEADS * RWKV_HEAD_DIM
DECAY_LORA = 64
AAA_LORA = 64
GATE_LORA = 128
GN_EPS = 64e-5
RWKV_COLS = 3 * RWKV_WIDTH + DECAY_LORA + AAA_LORA + GATE_LORA

DIFF_HEADS = 4
DIFF_HEAD_DIM = 64
DIFF_QK_WIDTH = DIFF_HEADS * 2 * DIFF_HEAD_DIM
DIFF_V_DIM = 2 * DIFF_HEAD_DIM
DIFF_V_WIDTH = DIFF_HEADS * DIFF_V_DIM
DIFF_COLS = 2 * DIFF_QK_WIDTH + DIFF_V_WIDTH
ROPE_THETA = 10000.0
Q_BLOCK = 128

GATE_COLS = 2 * D_MODEL
IN_COLS = RWKV_COLS + DIFF_COLS + GATE_COLS

N_GROUPS = 4
EXPERTS_PER_GROUP = 8
N_EXPERTS = N_GROUPS * EXPERTS_PER_GROUP
TOP_K = 2
EXPERT_FF = 512
ROW_BLOCK = 128

kernel_name = 'hybrid_rwkv7_diffattn_hmoe_block'


def rms_norm(x, gain, eps=RMS_EPS):
    xf = x.astype(jnp.float32)
    y = xf * lax.rsqrt(jnp.mean(xf * xf, axis=-1, keepdims=True) + eps)
    return (y * gain.astype(jnp.float32)).astype(x.dtype)


def rope_tables(seq, dim):
    inv = ROPE_THETA ** (-jnp.arange(0, dim, 2, dtype=jnp.float32) / dim)
    ang = jnp.arange(seq, dtype=jnp.float32)[:, None] * inv[None, :]
    ang = jnp.concatenate([ang, ang], axis=-1)
    return jnp.cos(ang), jnp.sin(ang)


def apply_rope(x, cos, sin):
    c = cos[:, None, None, :]
    s = sin[:, None, None, :]
    x1, x2 = jnp.split(x, 2, axis=-1)
    rot = jnp.concatenate([-x2, x1], axis=-1)
    return (x.astype(jnp.float32) * c + rot.astype(jnp.float32) * s).astype(x.dtype)


def wkv7_scan(r, decay, k, kk, a, v):
    b, s, h, n = r.shape

    def step(state, inp):
        r_t, w_t, k_t, kk_t, a_t, v_t = inp
        sa = jnp.einsum('bhvk,bhk->bhv', state, -kk_t)
        state = (state * w_t[:, :, None, :]
                 + sa[..., None] * (kk_t * a_t)[:, :, None, :]
                 + v_t[..., None] * k_t[:, :, None, :])
        return state, jnp.einsum('bhvk,bhk->bhv', state, r_t)

    s0 = jnp.zeros((b, h, n, n), jnp.float32)
    xs = (jnp.swapaxes(r, 0, 1), jnp.swapaxes(decay, 0, 1), jnp.swapaxes(k, 0, 1),
          jnp.swapaxes(kk, 0, 1), jnp.swapaxes(a, 0, 1), jnp.swapaxes(v, 0, 1))
    _, y = lax.scan(step, s0, xs)
    return jnp.swapaxes(y, 0, 1)


def rwkv7_branch(pr, mu, w0, w2, a0, a2, g2, k_k, k_a, r_k, ln_w, ln_b):
    b, s, _ = pr.shape
    f32 = jnp.float32
    prev = jnp.pad(pr[:, :-1], ((0, 0), (1, 0), (0, 0)))
    xs = pr + (prev - pr) * mu
    c0 = RWKV_WIDTH
    r, k, v, xw, xa, xg = jnp.split(
        xs, [c0, 2 * c0, 3 * c0, 3 * c0 + DECAY_LORA, 3 * c0 + DECAY_LORA + AAA_LORA], axis=-1)
    w_log = -jax.nn.softplus(-(w0 + jnp.tanh(xw) @ w2).astype(f32)) - 0.5
    decay = jnp.exp(-jnp.exp(w_log))
    a = jax.nn.sigmoid((a0 + xa @ a2).astype(f32))
    g = jax.nn.sigmoid(xg) @ g2

    def heads(t):
        return t.reshape(b, s, RWKV_HEADS, RWKV_HEAD_DIM).astype(f32)

    kk = heads(k * k_k)
    kk = kk / jnp.maximum(jnp.sqrt(jnp.sum(kk * kk, axis=-1, keepdims=True)), 1e-12)
    k_mod = k.astype(f32) * (1.0 + (a - 1.0) * k_a.astype(f32))
    r_h, k_h, v_h = heads(r), heads(k_mod), heads(v)
    y = wkv7_scan(r_h, heads(decay), k_h, kk, heads(a), v_h)
    mean = jnp.mean(y, axis=-1, keepdims=True)
    var = jnp.mean(jnp.square(y - mean), axis=-1, keepdims=True)
    y = ((y - mean) * lax.rsqrt(var + GN_EPS)).reshape(b, s, RWKV_WIDTH)
    y = y * ln_w.astype(f32) + ln_b.astype(f32)
    bonus = jnp.sum(r_h * k_h * r_k.astype(f32), axis=-1, keepdims=True) * v_h
    out = (y + bonus.reshape(b, s, RWKV_WIDTH)) * g.astype(f32)
    return out.astype(pr.dtype)


def diff_attention_branch(pd, q_gain, k_gain, lq1, lk1, lq2, lk2, subln_w, lam_init, cos, sin):
    b, s, _ = pd.shape
    f32 = jnp.float32
    q, k, v = jnp.split(pd, [DIFF_QK_WIDTH, 2 * DIFF_QK_WIDTH], axis=-1)
    q = apply_rope(rms_norm(q.reshape(b, s, DIFF_HEADS, 2, DIFF_HEAD_DIM), q_gain), cos, sin)
    k = apply_rope(rms_norm(k.reshape(b, s, DIFF_HEADS, 2, DIFF_HEAD_DIM), k_gain), cos, sin)
    q = q.transpose(0, 2, 3, 1, 4)
    k = k.transpose(0, 2, 3, 1, 4)
    v = v.reshape(b, s, DIFF_HEADS, DIFF_V_DIM).transpose(0, 2, 1, 3)
    lam = (jnp.exp(jnp.sum(lq1.astype(f32) * lk1.astype(f32)))
           - jnp.exp(jnp.sum(lq2.astype(f32) * lk2.astype(f32))) + lam_init)
    scale = DIFF_HEAD_DIM ** -0.5
    neg = jnp.finfo(f32).min
    blocks = []
    for i in range(s // Q_BLOCK):
        q0 = i * Q_BLOCK
        end = q0 + Q_BLOCK
        sc = jnp.einsum('bhcqd,bhckd->bhcqk', q[:, :, :, q0:end], k[:, :, :, :end]).astype(f32) * scale
        causal = jnp.arange(end)[None, :] <= (q0 + jnp.arange(Q_BLOCK))[:, None]
        prob = jax.nn.softmax(jnp.where(causal, sc, neg), axis=-1)
        attn = prob[:, :, 0] - lam * prob[:, :, 1]
        blocks.append(jnp.einsum('bhqk,bhkv->bhqv', attn.astype(v.dtype), v[:, :, :end]))
    o = jnp.concatenate(blocks, axis=2)
    o = rms_norm(o, subln_w) * (1.0 - lam_init)
    return o.transpose(0, 2, 1, 3).reshape(b, s, DIFF_V_WIDTH)


def hierarchical_moe(h, w_group, b_group, w_er, b_er, w_gate, w_up, w_down):
    b, s, d = h.shape
    t = b * s
    f32 = jnp.float32
    hf = h.reshape(t, d)
    g_prob = jax.nn.softmax((hf @ w_group).astype(f32) + b_group.astype(f32), axis=-1)
    g_top, g_idx = lax.top_k(g_prob, 1)
    e_logits = ((hf @ w_er).astype(f32) + b_er.astype(f32)).reshape(t, N_GROUPS, EXPERTS_PER_GROUP)
    sel = jnp.broadcast_to(g_idx[:, :, None], (t, 1, EXPERTS_PER_GROUP))
    e_logits = jnp.take_along_axis(e_logits, sel, axis=1)[:, 0]
    e_top, e_idx = lax.top_k(e_logits, TOP_K)
    weights = g_top * jax.nn.softmax(e_top, axis=-1)
    expert_id = g_idx * EXPERTS_PER_GROUP + e_idx

    n_assign = t * TOP_K
    eid = expert_id.reshape(n_assign).astype(jnp.int32)
    tid = jnp.repeat(jnp.arange(t, dtype=jnp.int32), TOP_K)
    wt = weights.reshape(n_assign)
    order = jnp.argsort(eid)
    se = eid[order]
    counts = jnp.bincount(eid, length=N_EXPERTS).astype(jnp.int32)
    starts = jnp.cumsum(counts) - counts
    pcounts = ((counts + ROW_BLOCK - 1) // ROW_BLOCK) * ROW_BLOCK
    pends = jnp.cumsum(pcounts)
    pstarts = pends - pcounts
    dest = pstarts[se] + (jnp.arange(n_assign, dtype=jnp.int32) - starts[se])
    rows = n_assign + N_EXPERTS * ROW_BLOCK
    n_blocks = rows // ROW_BLOCK
    buf_tok = jnp.full((rows,), t, jnp.int32).at[dest].set(tid[order])
    buf_w = jnp.zeros((rows,), wt.dtype).at[dest].set(wt[order])
    blk_e = jnp.minimum(jnp.searchsorted(pends, jnp.arange(n_blocks, dtype=jnp.int32) * ROW_BLOCK,
                                         side='right'), N_EXPERTS - 1)
    hpad = jnp.concatenate([hf, jnp.zeros((1, d), hf.dtype)], axis=0)
    xs = hpad[buf_tok].reshape(n_blocks, ROW_BLOCK, d)

    def expert_block(args):
        xb, e = args
        return (jax.nn.silu(xb @ w_gate[e]) * (xb @ w_up[e])) @ w_down[e]

    yb = lax.map(expert_block, (xs, blk_e)).reshape(rows, d)
    yb = yb * buf_w[:, None].astype(yb.dtype)
    out = jax.ops.segment_sum(yb, buf_tok, num_segments=t + 1)[:t]
    return out.reshape(b, s, d)


def setup_inputs(seed: int = 0) -> dict:
    key = jax.random.key(seed)
    ks = iter(jax.random.split(key, 40))
    L = DEPTH
    f32 = jnp.float32

    def nrm(shape, scale):
        return scale * jax.random.normal(next(ks), shape, f32)

    def gain(shape, base=1.0):
        return base + 0.1 * jax.random.normal(next(ks), shape, f32)

    return {
        'x': nrm((BATCH, SEQ, D_MODEL), 1.0),
        'p': nrm((L, BATCH, SEQ, PLE_DIM), 1.0),
        'norm_mix': gain((L, D_MODEL)),
        'w_in': nrm((L, D_MODEL, IN_COLS), D_MODEL ** -0.5),
        'rwkv_mu': jax.random.uniform(next(ks), (L, RWKV_COLS), f32),
        'rwkv_w0': jax.random.uniform(next(ks), (L, RWKV_WIDTH), f32, -6.0, 0.0),
        'rwkv_w2': nrm((L, DECAY_LORA, RWKV_WIDTH), DECAY_LORA ** -0.5),
        'rwkv_a0': nrm((L, RWKV_WIDTH), 0.5),
        'rwkv_a2': nrm((L, AAA_LORA, RWKV_WIDTH), AAA_LORA ** -0.5),
        'rwkv_g2': nrm((L, GATE_LORA, RWKV_WIDTH), GATE_LORA ** -0.5),
        'rwkv_k_k': gain((L, RWKV_WIDTH), 0.85),
        'rwkv_k_a': gain((L, RWKV_WIDTH)),
        'rwkv_r_k': gain((L, RWKV_HEADS, RWKV_HEAD_DIM), 0.5),
        'rwkv_ln_w': gain((L, RWKV_WIDTH)),
        'rwkv_ln_b': nrm((L, RWKV_WIDTH), 0.02),
        'q_norm': gain((L, DIFF_HEAD_DIM)),
        'k_norm': gain((L, DIFF_HEAD_DIM)),
        'lambda_q1': nrm((L, DIFF_HEAD_DIM), 0.1),
        'lambda_k1': nrm((L, DIFF_HEAD_DIM), 0.1),
        'lambda_q2': nrm((L, DIFF_HEAD_DIM), 0.1),
        'lambda_k2': nrm((L, DIFF_HEAD_DIM), 0.1),
        'subln_w': gain((L, DIFF_V_DIM)),
        'w_branch_rwkv': nrm((L, RWKV_WIDTH, D_MODEL), RWKV_WIDTH ** -0.5),
        'w_branch_diff': nrm((L, DIFF_V_WIDTH, D_MODEL), DIFF_V_WIDTH ** -0.5),
        'w_out': nrm((L, D_MODEL, D_MODEL), D_MODEL ** -0.5),
        'norm_ffn': gain((L, D_MODEL)),
        'w_group': nrm((L, D_MODEL, N_GROUPS), D_MODEL ** -0.5),
        'b_group': nrm((L, N_GROUPS), 0.01),
        'w_expert_router': nrm((L, D_MODEL, N_EXPERTS), D_MODEL ** -0.5),
        'b_expert_router': nrm((L, N_EXPERTS), 0.01),
        'w_gate': nrm((L, N_EXPERTS, D_MODEL, EXPERT_FF), D_MODEL ** -0.5),
        'w_up': nrm((L, N_EXPERTS, D_MODEL, EXPERT_FF), D_MODEL ** -0.5),
        'w_down': nrm((L, N_EXPERTS, EXPERT_FF, D_MODEL), EXPERT_FF ** -0.5),
        'norm_ple': gain((L, D_MODEL)),
        'w_ple_gate': nrm((L, D_MODEL, D_MODEL), D_MODEL ** -0.5),
        'w_ple_proj': nrm((L, PLE_DIM, D_MODEL), PLE_DIM ** -0.5),
    }


def reference(x, p, norm_mix, w_in, rwkv_mu, rwkv_w0, rwkv_w2, rwkv_a0, rwkv_a2, rwkv_g2,
              rwkv_k_k, rwkv_k_a, rwkv_r_k, rwkv_ln_w, rwkv_ln_b, q_norm, k_norm,
              lambda_q1, lambda_k1, lambda_q2, lambda_k2, subln_w, w_branch_rwkv,
              w_branch_diff, w_out, norm_ffn, w_group, b_group, w_expert_router,
              b_expert_router, w_gate, w_up, w_down, norm_ple, w_ple_gate, w_ple_proj):
    seq = x.shape[1]
    cos, sin = rope_tables(seq, DIFF_HEAD_DIM)
    for l in range(DEPTH):
        lam_init = 0.8 - 0.6 * math.exp(-0.3 * l)
        h = rms_norm(x, norm_mix[l])
        proj = h @ w_in[l]
        p_rwkv = proj[..., :RWKV_COLS]
        p_diff = proj[..., RWKV_COLS:RWKV_COLS + DIFF_COLS]
        g_rwkv, g_diff = jnp.split(proj[..., RWKV_COLS + DIFF_COLS:], 2, axis=-1)
        o_rwkv = rwkv7_branch(p_rwkv, rwkv_mu[l], rwkv_w0[l], rwkv_w2[l], rwkv_a0[l], rwkv_a2[l],
                              rwkv_g2[l], rwkv_k_k[l], rwkv_k_a[l], rwkv_r_k[l], rwkv_ln_w[l],
                              rwkv_ln_b[l])
        o_diff = diff_attention_branch(p_diff, q_norm[l], k_norm[l], lambda_q1[l], lambda_k1[l],
                                       lambda_q2[l], lambda_k2[l], subln_w[l], lam_init, cos, sin)
        mixed = (jax.nn.sigmoid(g_rwkv) * (o_rwkv @ w_branch_rwkv[l])
                 + jax.nn.sigmoid(g_diff) * (o_diff @ w_branch_diff[l]))
        x = x + mixed @ w_out[l]
        x = x + hierarchical_moe(rms_norm(x, norm_ffn[l]), w_group[l], b_group[l],
                                 w_expert_router[l], b_expert_router[l], w_gate[l], w_up[l],
                                 w_down[l])
        ple_gate = jax.nn.sigmoid(rms_norm(x, norm_ple[l]) @ w_ple_gate[l])
        x = x + ple_gate * (p[l] @ w_ple_proj[l])
    return x
```

```python
import numpy as np
from contextlib import ExitStack
import concourse.bass as bass
import concourse.mybir as mybir
from concourse.bass_utils import run_bass_kernel_spmd

F32 = mybir.dt.float32
BF16 = mybir.dt.bfloat16
ALU = mybir.AluOpType
AF = mybir.ActivationFunctionType
AX = mybir.AxisListType

S = 4096
D = 1024
NT = 32
ENG = ("pe", "act", "dve", "pool", "sp")
EPOCH = 8000
NDMA = 24
ARENA_WORDS = 53000

CP_GMIX = 0
CP_GPLE = 8
CP_W0 = 16
CP_A0 = 20
CP_KK = 24
CP_KA = 28
CP_RK = 32
CP_QN = 36
CP_KN = 37
CP_MU1 = 38
CP_MU = 52
NCP = 66
RB_GFFN = 0
RB_RBIAS = 1024
RB_LNW = 1060
RB_LNB = 1572
RB_SUBLN = 2084
RB_LAM = 2212
RB_W0 = 2468
NRB = 2980
C_ID = 0
C_US = 128
C_UI = 256
C_LS = 384
C_BO = 512
C_RM = 640
C_HS = 768
C_ONE = 770
NCST = 898


class _Stop(Exception):
    pass


class Buf:
    __slots__ = ("w", "r")

    def __init__(self):
        self.w = None
        self.r = []


class T:
    def __init__(self, ap, b=None):
        self.ap = ap
        self.b = b if b is not None else Buf()

    def __getitem__(self, k):
        return self.ap[k]


class Prog:
    def __init__(self, nc, arena, psums):
        self.nc = nc
        self.arena = arena
        self.off = 0
        self.streams = {e: [] for e in ENG}
        self.seq = {e: 0 for e in ENG}
        self.seen = {e: {} for e in ENG}
        self.seen_d = {e: {} for e in ENG}
        self.dcnt = [0] * NDMA
        self.drr = 0
        self.ps_tiles = [T(p) for p in psums]
        self.ps_pool = list(range(len(psums)))
        self.ps_rr = 0
        self.sec = ""
        self.annot = False

    def alloc(self, shape, dt=F32):
        n = int(np.prod(shape))
        words = n if dt == F32 else (n + 1) // 2
        assert self.off + words <= ARENA_WORDS, ("arena overflow", self.off, words)
        ap = self.arena[:, self.off:self.off + words]
        self.off += words
        if dt != F32:
            ap = ap.bitcast(dt)[:, 0:n]
        if len(shape) == 2:
            ap = ap.rearrange("p (a b) -> p a b", a=shape[0])
        elif len(shape) == 3:
            ap = ap.rearrange("p (a b c) -> p a b c", a=shape[0], b=shape[1])
        return T(ap)

    def psum(self):
        t = self.ps_tiles[self.ps_pool[self.ps_rr % len(self.ps_pool)]]
        self.ps_rr = (self.ps_rr + 1) % len(self.ps_pool)
        return t

    def _need(self, eng, r, w):
        toks = set()
        for t in r:
            if t.b.w is not None:
                toks.add(t.b.w)
        for t in w:
            if t.b.w is not None:
                toks.add(t.b.w)
            toks.update(t.b.r)
        st = self.streams[eng]
        for tok in sorted(toks):
            if tok[0] == "e":
                _, pe, sq = tok
                if pe == eng and eng in ("pe", "sp"):
                    continue
                if self.seen[eng].get(pe, 0) >= sq:
                    continue
                self.seen[eng][pe] = sq
                st.append(("wait", tok))
            else:
                _, slot, cnt = tok
                if self.seen_d[eng].get(slot, 0) >= cnt:
                    continue
                self.seen_d[eng][slot] = cnt
                st.append(("wait", tok))

    def _mark(self, tok, r, w):
        for t in r:
            t.b.r.append(tok)
        for t in w:
            t.b.w = tok
            t.b.r = []

    def op(self, eng, fn, r, w):
        self._need(eng, r, w)
        self.seq[eng] += 1
        tok = ("e", eng, self.seq[eng])
        if self.annot:
            sec = self.sec
            fn0 = fn
            fn = lambda e: fn0(e).annotate(sec)
        self.streams[eng].append(("op", fn, tok))
        self._mark(tok, r, w)

    def dma(self, q, out, in_, r, w):
        slot = self.drr
        self.drr = (slot + 1) % NDMA
        prev = self.dcnt[slot]
        self._need(q, r, w)
        if prev > 0 and self.seen_d[q].get(slot, 0) < prev:
            self.seen_d[q][slot] = prev
            self.streams[q].append(("wait", ("d", slot, prev)))
        self.dcnt[slot] = prev + 1
        tok = ("d", slot, prev + 1)
        self.streams[q].append(("dma", lambda e: e.dma_start(out=out, in_=in_), tok))
        self._mark(tok, r, w)

    def barrier(self):
        toks = [("e", e, self.seq[e]) for e in ENG if self.seq[e] > 0]
        toks += [("d", s, c) for s, c in enumerate(self.dcnt) if c > 0]
        for eng in ENG:
            for tok in toks:
                if tok[0] == "e":
                    if tok[1] == eng or self.seen[eng].get(tok[1], 0) >= tok[2]:
                        continue
                    self.seen[eng][tok[1]] = tok[2]
                else:
                    if self.seen_d[eng].get(tok[1], 0) >= tok[2]:
                        continue
                    self.seen_d[eng][tok[1]] = tok[2]
                self.streams[eng].append(("wait", tok))

    def act(self, out, in_, func, r, w, **kw):
        self.op("act", lambda e: e.activation(out=out, in_=in_, func=func, **kw), r, w)

    def tt(self, eng, out, in0, in1, op, r, w):
        self.op(eng, lambda e: e.tensor_tensor(out=out, in0=in0, in1=in1, op=op), r, w)

    def ts(self, eng, out, in0, s1, s2, op0, op1, r, w):
        if op1 is None:
            self.op(eng, lambda e: e.tensor_scalar(out=out, in0=in0, scalar1=s1, scalar2=None, op0=op0), r, w)
        else:
            self.op(eng, lambda e: e.tensor_scalar(out=out, in0=in0, scalar1=s1, scalar2=s2, op0=op0, op1=op1), r, w)

    def stt(self, eng, out, in0, scalar, in1, op0, op1, r, w):
        self.op(eng, lambda e: e.scalar_tensor_tensor(out=out, in0=in0, scalar=scalar, in1=in1, op0=op0, op1=op1), r, w)

    def cp(self, eng, out, in_, r, w):
        if eng == "act":
            self.act(out, in_, AF.Copy, r, w)
        else:
            self.op(eng, lambda e: e.tensor_copy(out=out, in_=in_), r, w)

    def red(self, out, in_, op, r, w):
        self.op("dve", lambda e: e.tensor_reduce(out=out, in_=in_, axis=AX.X, op=op), r, w)

    def recip(self, out, in_, r, w):
        self.op("dve", lambda e: e.reciprocal(out=out, in_=in_), r, w)

    def memset(self, eng, ap, val, w):
        self.op(eng, lambda e: e.memset(ap, val), [], w)

    def mm(self, out, lhsT, rhs, start, stop, r, w, skip=False):
        if skip:
            self.op("pe", lambda e: e.matmul(out, lhsT=lhsT, rhs=rhs, start=start, stop=stop, skip_group_check=True), r, w)
        else:
            self.op("pe", lambda e: e.matmul(out, lhsT=lhsT, rhs=rhs, start=start, stop=stop), r, w)

    def rstd(self, out, ssq, scale, eps, r, w):
        et = self.eps_tiles[eps]
        n = out.shape[0]
        self.act(out, ssq, AF.Sqrt, list(r) + [self.eps_T], w, scale=float(scale), bias=et[0:n, :])
        self.recip(out, out, w, w)

    def emit(self, es):
        nc = self.nc
        sems = {}

        def semh(tok):
            if tok[0] == "e":
                key = ("e", tok[1], (tok[2] - 1) // EPOCH)
                val = tok[2] - key[2] * EPOCH
            else:
                key = ("d", tok[1])
                val = 16 * tok[2]
            if key not in sems:
                sems[key] = es.enter_context(nc.semaphore("s_" + "_".join(str(k) for k in key)))
            return sems[key], val

        for s, c in enumerate(self.dcnt):
            if c > 0 and self.seen_d["sp"].get(s, 0) < c:
                self.streams["sp"].append(("wait", ("d", s, c)))
        for e in ENG:
            for it in self.streams[e]:
                semh(it[1] if it[0] == "wait" else it[2])
        block = es.enter_context(nc.Block())

        def replay(name):
            def f(eng):
                for it in self.streams[name]:
                    if it[0] == "wait":
                        s, v = semh(it[1])
                        eng.wait_ge(s, v)
                    elif it[0] == "op":
                        s, _ = semh(it[2])
                        it[1](eng).then_inc(s, 1)
                    else:
                        s, _ = semh(it[2])
                        it[1](eng).then_inc(s, 16)
            return f

        block.sync(replay("sp"))
        block.scalar(replay("act"))
        block.vector(replay("dve"))
        block.gpsimd(replay("pool"))
        block.tensor(replay("pe"))


def bc(ap, shape):
    return ap.to_broadcast(shape)


def build(flags):
    nc = bass.Bass("TRN2", target_bir_lowering=False)
    dr = {}

    def din(name, shape, dt=F32):
        dr[name] = nc.dram_tensor(name, list(shape), dt, kind="ExternalInput").ap()
        return dr[name]

    x_d = din("x", [S, D])
    p_d = din("p", [S, 256])
    w_in = din("w_in", [D, 5376])
    w2_d = din("w2", [64, 512])
    a2_d = din("a2", [64, 512])
    g2_d = din("g2", [128, 512])
    wbr_d = din("wbr", [512, D])
    wbd_d = din("wbd", [512, D])
    wout_d = din("wout", [D, D])
    wr_d = din("wr", [D, 36])
    wg_d = din("w_gate", [32, D, 512])
    wu_d = din("w_up", [32, D, 512])
    wd_d = din("w_down", [32, 512, D])
    wpg_d = din("wpg", [D, D])
    wpp_d = din("wpp", [256, D])
    colp_d = din("colp", [128, NCP])
    rowb_d = din("rowb", [128, NRB])
    cst_d = din("cst", [128, NCST])
    rope_d = din("rope", [128, 2 * S])
    murow_d = din("murow", [128, 1792])
    y_d = nc.dram_tensor("y", [S, D], F32, kind="ExternalOutput").ap()
    X2 = nc.dram_tensor("x2s", [S, D], F32, kind="Internal").ap()
    H2T = nc.dram_tensor("h2ts", [8, 128, S], BF16, kind="Internal").ap()
    okind = "ExternalOutput" if flags.get("dbgout") else "Internal"
    ORT = nc.dram_tensor("orts", [4, 128, S], BF16, kind=okind).ap()
    ODT = nc.dram_tensor("odts", [4, 128, S], BF16, kind=okind).ap()
    HTD = nc.dram_tensor("htd", [128, 8, S], BF16, kind="Internal").ap()
    dbg_d = None
    if flags.get("dbg"):
        dbg_d = nc.dram_tensor("dbg", [128, flags["dbg"]], F32, kind="ExternalOutput").ap()

    with ExitStack() as es:
        arena = es.enter_context(nc.sbuf_tensor("arena", [128, ARENA_WORDS], F32))
        psums = [es.enter_context(nc.psum_tensor("ps%d" % i, [128, 512], F32)) for i in range(8)]
        P = Prog(nc, arena, [p[:, :] for p in psums])
        P.annot = bool(flags.get("annot"))

        cst = P.alloc([NCST])
        colp = P.alloc([NCP])
        rowb = P.alloc([NRB])
        cstb = P.alloc([NCST], BF16)
        P.dma("sp", cst.ap, cst_d, [], [cst])
        P.dma("sp", colp.ap, colp_d, [], [colp])
        P.dma("sp", rowb.ap, rowb_d, [], [rowb])
        P.cp("dve", cstb.ap, cst.ap, [cst], [cstb])
        identb = cstb[:, C_ID:C_ID + 128]
        identf = cst[:, C_ID:C_ID + 128]
        LG = P.alloc([NT, 36])
        WT = P.alloc([NT, 32])
        epsc = P.alloc([4])
        P.eps_T = epsc
        P.eps_tiles = {}
        for ci, ev in enumerate((1e-6, 1e-24, 64e-5)):
            P.memset("dve", epsc[:, ci:ci + 1], ev, [epsc])
            P.eps_tiles[ev] = epsc[:, ci:ci + 1]
        base_off = P.off
        hblk = [P.alloc([8, 512], BF16) for _ in range(2)]
        HTDt = T(HTD)

        def wload(dst, src, r, w):
            P.dma("pool", dst, src, r, w)

        def run_rr4(gen_list, width):
            active = []
            it = iter(gen_list)
            done = False
            while True:
                while len(active) < width and not done:
                    try:
                        active.append(next(it))
                    except StopIteration:
                        done = True
                if not active:
                    break
                for g in list(active):
                    try:
                        next(g)
                    except StopIteration:
                        active.remove(g)

        mark1 = P.off
        xt1 = [P.alloc([D]) for _ in range(3)]
        xn1 = [P.alloc([D], BF16) for _ in range(3)]
        junk1 = [P.alloc([D], BF16) for _ in range(3)]
        ssq1 = [P.alloc([1]) for _ in range(3)]
        rs1 = [P.alloc([1]) for _ in range(3)]

        def gen1(i):
            k = i % 3
            a, b_ = xt1[k], xn1[k]
            P.dma("sp", a.ap, x_d[i * 128:(i + 1) * 128, :], [], [a])
            P.act(junk1[k].ap, a.ap, AF.Square, [a], [junk1[k], ssq1[k]], accum_out=ssq1[k].ap)
            yield
            P.rstd(rs1[k].ap, ssq1[k].ap, 1.0 / D, 1e-6, [ssq1[k]], [rs1[k]])
            yield
            P.act(b_.ap, a.ap, AF.Copy, [a, rs1[k]], [b_], scale=rs1[k][:, 0:1])
            yield
            for hf in range(2):
                ps = P.psum()
                for kk in range(4):
                    kc = hf * 4 + kk
                    P.mm(ps[:, kk * 128:(kk + 1) * 128], b_[:, kc * 128:(kc + 1) * 128], identb, True, True,
                         [b_, cstb], [ps])
                hb_ = hblk[(i // 4) % 2]
                P.tt("dve", hb_[:, hf * 4:hf * 4 + 4, (i % 4) * 128:(i % 4 + 1) * 128],
                     ps.ap.rearrange("p (a b) -> p a b", a=4),
                     bc(colp[:, CP_GMIX + hf * 4:CP_GMIX + hf * 4 + 4].unsqueeze(2), [128, 4, 128]), ALU.mult,
                     [ps, colp], [hb_])
                yield
            if i % 4 == 3:
                P.dma("sp", HTD[:, :, (i // 4) * 512:(i // 4 + 1) * 512], hblk[(i // 4) % 2].ap, [hblk[(i // 4) % 2]], [HTDt])
        run_rr4((gen1(i) for i in range(NT)), 3)
        P.barrier()
        P.off = mark1

        if flags.get("stop") == 1:
            P.emit(es)
            return nc
        if flags.get("mixers", True):

            EC = float(flags.get("ec", np.exp(-0.5)))
            if flags.get("rwkv", True):
                W1 = P.alloc([8, 1792], BF16)
                W2 = P.alloc([8, 1792], BF16)
                w2a2 = P.alloc([512], BF16)
                g2b = P.alloc([512], BF16)
                wload(w2a2[0:64, :], w2_d, [], [w2a2])
                wload(w2a2[64:128, :], a2_d, [], [w2a2])
                wload(g2b.ap, g2_d, [], [g2b])
                mk = P.off
                stage = P.alloc([1792])
                mur = P.alloc([1792])
                omm = P.alloc([1792])
                P.dma("sp", mur.ap, murow_d, [], [mur])
                P.ts("dve", omm.ap, mur.ap, -1.0, 1.0, ALU.mult, ALU.add, [mur], [omm])
                for kc in range(8):
                    P.dma("sp", stage.ap, w_in[kc * 128:(kc + 1) * 128, 0:1792], [], [stage])
                    P.tt("dve", W1[:, kc, :], stage.ap, omm.ap, ALU.mult, [stage, omm], [W1])
                    P.tt("pool", W2[:, kc, :], stage.ap, mur.ap, ALU.mult, [stage, mur], [W2])
                P.barrier()
                P.off = mk
                hprevs = [P.alloc([8, 512], BF16) for _ in range(2)]
                loras = [P.alloc([512], BF16) for _ in range(2)]
                sxgs = [P.alloc([512], BF16) for _ in range(2)]
                r32, k32, v32, a32, kq, rn, t1_, kmod = [P.alloc([512]) for _ in range(8)]
                kk_, tmp = kq, rn
                eng, egm = [P.alloc([512]) for _ in range(2)]
                egs = [P.alloc([512]) for _ in range(2)]
                vb, kq2b, BT, KTt = [P.alloc([512], BF16) for _ in range(4)]
                prodks = [P.alloc([512], BF16) for _ in range(2)]
                ARs = [P.alloc([4, 2, 128], BF16) for _ in range(2)]
                TMs = [P.alloc([4, 384], BF16) for _ in range(2)]
                ztoks = [P.alloc([128])] * 2
                sgt32s = [P.alloc([128])] * 2
                sghis = [P.alloc([128], BF16) for _ in range(2)]
                sglos = [P.alloc([128], BF16) for _ in range(2)]
                NRss = [[P.alloc([2, 2, 128], BF16) for _ in range(4)] for _ in range(2)]
                AKss = [[P.alloc([2, 2, 128], BF16) for _ in range(4)] for _ in range(2)]
                WtAss = [[P.alloc([2, 128], BF16) for _ in range(4)] for _ in range(2)]
                WtBss = [[P.alloc([2, 128], BF16) for _ in range(4)] for _ in range(2)]
                LLs = [P.alloc([2, 128], BF16) for _ in range(4)]
                PQs = [[P.alloc([512], BF16) for _ in range(2)] for _ in range(4)]
                Xb = P.alloc([128], BF16)
                Ub = P.alloc([128], BF16)
                T32 = [P.alloc([128]) for _ in range(4)]
                Tb = [P.alloc([128], BF16) for _ in range(4)]
                y32s = [P.alloc([128]) for _ in range(2)]
                ysqs = [P.alloc([128]) for _ in range(2)]
                yns = [P.alloc([128]) for _ in range(2)]
                stats = [[P.alloc([2]) for _ in range(7)] for _ in range(2)]
                otoks = [P.alloc([128], BF16) for _ in range(2)]
                orblks = [P.alloc([4, 512], BF16)] * 2
                for j in range(4):
                    P.memset("pool", T32[j].ap, 0.0, [T32[j]])
                    P.memset("pool", Tb[j].ap, 0.0, [Tb[j]])
                idb2 = bc(identb.unsqueeze(1), [128, 2, 128])
                NTB = flags.get("ntb", 8)
                WtFinal = {}

                def proj(tb, g, hc, hprev):
                    ps = P.psum()
                    cs = slice(g * 128, (g + 1) * 128)
                    for kc in range(8):
                        P.mm(ps.ap, W1[:, kc, cs], hc[:, kc, :], kc == 0, False, [W1, hc], [ps])
                        P.mm(ps.ap, W2[:, kc, cs], hprev[:, kc, :], False, kc == 7, [W2, hprev], [ps])
                    return ps

                def genS(tb):
                    P.sec = "r_S"
                    t0 = tb * 512
                    tsl = slice(t0, t0 + 512)
                    hc = hblk[tb % 2]
                    hprev = hprevs[tb % 2]
                    lora = loras[tb % 2]
                    sxg = sxgs[tb % 2]
                    P.dma("sp", hc.ap, HTD[:, :, tsl], [HTDt], [hc])
                    if tb == 0:
                        P.memset("dve", hprev.ap, 0.0, [hprev])
                        P.dma("sp", hprev[:, :, 1:512], HTD[:, :, 0:511], [HTDt], [hprev])
                    else:
                        P.dma("sp", hprev.ap, HTD[:, :, t0 - 1:t0 + 511], [HTDt], [hprev])
                    yield
                    ps = proj(tb, 12, hc, hprev)
                    P.act(lora[0:64, :], ps[0:64, :], AF.Tanh, [ps], [lora])
                    P.cp("dve", lora[64:128, :], ps[64:128, :], [ps], [lora])
                    yield
                    ps = proj(tb, 13, hc, hprev)
                    P.act(sxg.ap, ps.ap, AF.Sigmoid, [ps], [sxg])
                    yield

                def genA(tb, j):
                    par = (tb * 4 + j) % 2
                    hc = hblk[tb % 2]
                    hprev = hprevs[tb % 2]
                    lora = loras[tb % 2]
                    AR, TM, eg, prodk = ARs[par], TMs[par], egs[par], prodks[par]
                    ARf = AR.ap.rearrange("p t a b -> p t (a b)")
                    NRs, AKs, WtAs, WtBs = NRss[par], AKss[par], WtAss[par], WtBss[par]
                    js = slice(j * 128, (j + 1) * 128)
                    P.sec = "r_proj"
                    ps = proj(tb, j, hc, hprev)
                    P.cp("act", r32.ap, ps.ap, [ps], [r32])
                    yield
                    P.sec = "r_proj"
                    ps = proj(tb, 4 + j, hc, hprev)
                    P.cp("dve", k32.ap, ps.ap, [ps], [k32])
                    yield
                    P.sec = "r_proj"
                    ps = proj(tb, 8 + j, hc, hprev)
                    P.cp("act", v32.ap, ps.ap, [ps], [v32])
                    P.cp("pool", vb.ap, v32.ap, [v32], [vb])
                    yield
                    P.sec = "r_preptok"
                    ps = P.psum()
                    P.mm(ps.ap, w2a2[64:128, js], lora[64:128, :], True, True, [w2a2, lora], [ps])
                    P.act(a32.ap, ps.ap, AF.Sigmoid, [ps, colp], [a32], bias=colp[:, CP_A0 + j:CP_A0 + j + 1])
                    for tl in range(4):
                        yield
                        P.sec = "r_preptok"
                        ztok, sgt32, sghi, sglo = ztoks[tl % 2], sgt32s[tl % 2], sghis[tl % 2], sglos[tl % 2]
                        ts_ = slice(tl * 128, (tl + 1) * 128)
                        ps = P.psum()
                        P.mm(ps[:, 0:128], lora[0:64, ts_], w2a2[0:64, js], True, True, [lora, w2a2], [ps])
                        P.tt("dve", ztok.ap, ps[:, 0:128], rowb[:, RB_W0 + j * 128:RB_W0 + (j + 1) * 128], ALU.add,
                             [ps, rowb], [ztok])
                        P.act(sgt32.ap, ztok.ap, AF.Sigmoid, [ztok], [sgt32])
                        P.cp("pool", sghi.ap, sgt32.ap, [sgt32], [sghi])
                        P.tt("pool", sglo.ap, sgt32.ap, sghi.ap, ALU.subtract, [sgt32, sghi], [sglo])
                        yield
                        P.sec = "r_preptok"
                        psG = P.psum()
                        P.mm(psG[:, 0:256], sghi.ap, cstb[:, C_US:C_US + 256], True, False, [sghi, cstb], [psG])
                        P.mm(psG[:, 0:256], sglo.ap, cstb[:, C_US:C_US + 256], False, True, [sglo, cstb], [psG])
                        P.act(egm[:, ts_], psG[:, 0:128], AF.Exp, [psG], [egm], scale=-EC)
                        P.act(eg[:, ts_], psG[:, 128:256], AF.Exp, [psG], [eg], scale=-EC)
                        P.act(eng[:, ts_], psG[:, 128:256], AF.Exp, [psG], [eng], scale=EC)
                    yield
                    P.sec = "r_prepfm"
                    v4 = lambda t: t.ap.rearrange("p (a b) -> p a b", a=4)
                    P.ts("dve", kq.ap, k32.ap, colp[:, CP_KK + j:CP_KK + j + 1], None, ALU.mult, None, [k32, colp], [kq])
                    P.tt("pool", kq2b.ap, kq.ap, kq.ap, ALU.mult, [kq], [kq2b])
                    ps = P.psum()
                    P.mm(ps.ap, cstb[:, C_BO:C_BO + 128], kq2b.ap, True, True, [cstb, kq2b], [ps])
                    P.rstd(rn.ap, ps.ap, 1.0, 1e-24, [ps], [rn])
                    yield
                    P.sec = "r_prepfm"
                    P.tt("dve", kk_.ap, kq.ap, rn.ap, ALU.mult, [kq, rn], [kk_])
                    P.stt("dve", AR[:, :, 0, :], v4(kk_), -1.0, v4(egm), ALU.mult, ALU.mult, [kk_, egm], [AR])
                    yield
                    P.sec = "r_prepfm"
                    P.tt("dve", tmp.ap, kk_.ap, a32.ap, ALU.mult, [kk_, a32], [tmp])
                    P.tt("pool", BT.ap, tmp.ap, eng.ap, ALU.mult, [tmp, eng], [BT])
                    yield
                    P.sec = "r_prepfm"
                    P.ts("dve", t1_.ap, a32.ap, -1.0, colp[:, CP_KA + j:CP_KA + j + 1], ALU.add, ALU.mult, [a32, colp], [t1_])
                    P.stt("dve", kmod.ap, t1_.ap, 1.0, k32.ap, ALU.add, ALU.mult, [t1_, k32], [kmod])
                    yield
                    P.sec = "r_prepfm"
                    P.tt("dve", KTt.ap, kmod.ap, eng.ap, ALU.mult, [kmod, eng], [KTt])
                    P.tt("pool", AR[:, :, 1, :], v4(r32), v4(eg), ALU.mult, [r32, eg], [AR])
                    P.stt("dve", prodk.ap, r32.ap, colp[:, CP_RK + j:CP_RK + j + 1], kmod.ap, ALU.mult, ALU.mult,
                          [r32, colp, kmod], [prodk])
                    for tl in range(4):
                        yield
                        P.sec = "r_transp"
                        ts_ = slice(tl * 128, (tl + 1) * 128)
                        ps = P.psum()
                        P.mm(ps[:, 0:128], BT[:, ts_], identb, True, True, [BT, cstb], [ps])
                        P.mm(ps[:, 128:256], KTt[:, ts_], identb, True, True, [KTt, cstb], [ps])
                        P.mm(ps[:, 256:384], vb[:, ts_], identb, True, True, [vb, cstb], [ps])
                        P.cp("act", TM[:, tl, :], ps[:, 0:384], [ps], [TM])
                    for tl in range(4):
                        ts_ = slice(tl * 128, (tl + 1) * 128)
                        yield
                        P.sec = "r_masks"
                        hss = [slice(0, 64), slice(64, 128)]
                        psXs = [P.psum(), P.psum()]
                        for h in range(2):
                            P.mm(psXs[h][:, 0:256], BT[hss[h], ts_], ARf[hss[h], tl, :], True, True, [BT, AR], [psXs[h]])
                        psYs = [P.psum(), P.psum()]
                        for h in range(2):
                            P.mm(psYs[h][:, 0:256], KTt[hss[h], ts_], ARf[hss[h], tl, :], True, True, [KTt, AR], [psYs[h]])
                        psZs = [P.psum(), P.psum()]
                        for h in range(2):
                            P.mm(psZs[h][:, 0:128], AR[hss[h], tl, 0, :], BT[hss[h], ts_], True, True, [AR, BT], [psZs[h]])
                        for h in range(2):
                            P.tt("dve", NRs[tl][:, h, :, :].rearrange("p a b -> p (a b)"), psXs[h][:, 0:256],
                                 cstb[:, C_US:C_US + 256], ALU.mult, [psXs[h], cstb], [NRs[tl]])
                            P.tt("dve", AKs[tl][:, h, :, :].rearrange("p a b -> p (a b)"), psYs[h][:, 0:256],
                                 cstb[:, C_US:C_US + 256], ALU.mult, [psYs[h], cstb], [AKs[tl]])
                            P.tt("dve", LLs[tl][:, h, :], psZs[h][:, 0:128], cstb[:, C_LS:C_LS + 128], ALU.mult,
                                 [psZs[h], cstb], [LLs[tl]])
                    yield
                    P.sec = "r_inv"
                    Wts = []
                    Pcs, Qcs, Pbs = [], [], []
                    for tl in range(4):
                        P.tt("pool", WtAs[tl].ap, NRs[tl][:, :, 0, :], idb2, ALU.add, [NRs[tl], cstb], [WtAs[tl]])
                        Wts.append([WtAs[tl], WtBs[tl]])
                        Pcs.append([NRs[tl][:, 0, 0, :], NRs[tl][:, 1, 0, :]])
                        Qcs.append([LLs[tl][:, 0, :], LLs[tl][:, 1, :]])
                        Pbs.append([NRs[tl], LLs[tl]])
                    for lvl in range(1, 7):
                        for tl in range(4):
                            yield
                            P.sec = "r_inv"
                            ps = P.psum()
                            for h in range(2):
                                P.mm(ps[:, 128 * h:128 * h + 128], Qcs[tl][h], Pcs[tl][h], True, True, Pbs[tl], [ps])
                            for h in range(2):
                                P.mm(ps[:, 256 + 128 * h:256 + 128 * h + 128], Pcs[tl][h], Qcs[tl][h], True, True, Pbs[tl], [ps])
                            pq = PQs[tl][lvl % 2]
                            P.cp("act" if tl % 2 == 0 else "dve", pq.ap, ps.ap, [ps], [pq])
                            Pcs[tl] = [pq[:, 0:128], pq[:, 128:256]]
                            Qcs[tl] = [pq[:, 256:384], pq[:, 384:512]]
                            Pbs[tl] = [pq]
                        for tl in range(4):
                            yield
                            P.sec = "r_inv"
                            Wt, Wo = Wts[tl]
                            pq = PQs[tl][lvl % 2]
                            ps2 = P.psum()
                            for h in range(2):
                                P.mm(ps2[:, 128 * h:128 * h + 128], identb, Wt[:, h, :], True, False, [cstb, Wt], [ps2])
                                P.mm(ps2[:, 128 * h:128 * h + 128], pq[:, 256 + 128 * h:256 + 128 * h + 128], Wt[:, h, :],
                                     False, True, [pq, Wt], [ps2])
                            P.cp("dve" if tl % 2 == 0 else "act", Wo.ap, ps2[:, 0:256].rearrange("p (h c) -> p h c", h=2), [ps2], [Wo])
                            Wts[tl] = [Wo, Wt]
                    WtFinal[(tb, j)] = [w[0] for w in Wts]
                    yield

                def genB(tb, j):
                    par = (tb * 4 + j) % 2
                    sxg = sxgs[tb % 2]
                    orblk = orblks[tb % 2]
                    AR, TM, eg, prodk = ARs[par], TMs[par], egs[par], prodks[par]
                    NRs, AKs = NRss[par], AKss[par]
                    js = slice(j * 128, (j + 1) * 128)
                    for tl in range(4):
                        ts_ = slice(tl * 128, (tl + 1) * 128)
                        Wt = WtFinal[(tb, j)][tl]
                        NR, AK = NRs[tl], AKs[tl]
                        y32, ysq, yn, otok = y32s[tl % 2], ysqs[tl % 2], yns[tl % 2], otoks[tl % 2]
                        ysum, yss, mean, msq, var, rsd, sb = stats[tl % 2]
                        for h in range(2):
                            P.sec = "r_chain"
                            hs = slice(64 * h, 64 * h + 64)
                            vs = slice(64 * h, 64 * h + 64)
                            psA = P.psum()
                            P.mm(psA[:, 0:64], AR[hs, tl, 0, :], Tb[j][hs, vs], True, False, [AR, Tb[j]], [psA])
                            P.mm(psA[:, 0:64], AK[:, h, 0, :], TM[:, tl, 256 + 64 * h:256 + 64 * h + 64], False, True,
                                 [AK, TM], [psA])
                            P.cp("act" if h == 0 else "dve", Xb[:, vs], psA[:, 0:64], [psA], [Xb])
                        yield
                        P.sec = "r_chain"
                        psB = P.psum()
                        for h in range(2):
                            vs = slice(64 * h, 64 * h + 64)
                            P.mm(psB[:, vs], Wt[:, h, :], Xb[:, vs], True, True, [Wt, Xb], [psB])
                        P.cp("dve", Ub.ap, psB[:, 0:128], [psB], [Ub])
                        yield
                        P.sec = "r_chain"
                        psD = P.psum()
                        P.mm(psD[:, 0:128], TM[:, tl, 0:128], Ub.ap, True, False, [TM, Ub], [psD])
                        P.mm(psD[:, 0:128], TM[:, tl, 128:256], TM[:, tl, 256:384], False, True, [TM], [psD])
                        for h in range(2):
                            hs = slice(64 * h, 64 * h + 64)
                            vs = slice(64 * h, 64 * h + 64)
                            psC = P.psum()
                            P.mm(psC[:, 0:64], AR[hs, tl, 1, :], Tb[j][hs, vs], True, False, [AR, Tb[j]], [psC])
                            P.mm(psC[:, 0:64], NR[:, h, 1, :], Ub[:, vs], False, False, [NR, Ub], [psC])
                            P.mm(psC[:, 0:64], AK[:, h, 1, :], TM[:, tl, 256 + 64 * h:256 + 64 * h + 64], False, True,
                                 [AK, TM], [psC])
                            P.cp("act", y32[:, vs], psC[:, 0:64], [psC], [y32])
                        P.tt("dve", T32[j].ap, psD[:, 0:128], T32[j].ap, ALU.add, [psD, T32[j]], [T32[j]])
                        P.ts("dve", T32[j].ap, T32[j].ap, eg[:, tl * 128 + 127:tl * 128 + 128], None, ALU.mult, None,
                             [T32[j], eg], [T32[j]])
                        P.cp("act", Tb[j].ap, T32[j].ap, [T32[j]], [Tb[j]])
                        yield
                        P.sec = "r_post"
                        y3 = lambda t: t.ap.rearrange("p (h c) -> p h c", h=2)
                        P.red(ysum.ap, y3(y32), ALU.add, [y32], [ysum])
                        P.tt("pool", ysq.ap, y32.ap, y32.ap, ALU.mult, [y32], [ysq])
                        P.red(yss.ap, y3(ysq), ALU.add, [ysq], [yss])
                        P.ts("pool", mean.ap, ysum.ap, 1.0 / 64, None, ALU.mult, None, [ysum], [mean])
                        P.tt("pool", msq.ap, mean.ap, mean.ap, ALU.mult, [mean], [msq])
                        yield
                        P.sec = "r_post"
                        P.stt("dve", var.ap, yss.ap, 1.0 / 64, msq.ap, ALU.mult, ALU.subtract, [yss, msq], [var])
                        P.rstd(rsd.ap, var.ap, 1.0, 64e-5, [var], [rsd])
                        yield
                        P.sec = "r_post"
                        for h in range(2):
                            vs = slice(64 * h, 64 * h + 64)
                            P.ts("pool", yn[:, vs], y32[:, vs], mean[:, h:h + 1], rsd[:, h:h + 1], ALU.subtract, ALU.mult,
                                 [y32, mean, rsd], [yn])
                        P.tt("pool", yn.ap, yn.ap, rowb[:, RB_LNW + j * 128:RB_LNW + (j + 1) * 128], ALU.mult, [yn, rowb], [yn])
                        P.tt("pool", yn.ap, yn.ap, rowb[:, RB_LNB + j * 128:RB_LNB + (j + 1) * 128], ALU.add, [yn, rowb], [yn])
                        psS = P.psum()
                        P.mm(psS[:, 0:2], prodk[:, ts_], cstb[:, C_HS:C_HS + 2], True, True, [prodk, cstb], [psS])
                        P.cp("act", sb.ap, psS[:, 0:2], [psS], [sb])
                        yield
                        P.sec = "r_post"
                        for h in range(2):
                            vs = slice(64 * h, 64 * h + 64)
                            P.stt("dve", yn[:, vs], TM[:, tl, 256 + 64 * h:256 + 64 * h + 64], sb[:, h:h + 1], yn[:, vs],
                                  ALU.mult, ALU.add, [TM, sb, yn], [yn])
                        psT = P.psum()
                        P.mm(psT[:, 0:128], sxg[:, ts_], g2b[:, js], True, True, [sxg, g2b], [psT])
                        P.tt("dve", otok.ap, yn.ap, psT[:, 0:128], ALU.mult, [yn, psT], [otok])
                        yield
                        P.sec = "r_post"
                        psO = P.psum()
                        P.mm(psO[:, 0:128], otok.ap, identb, True, True, [otok, cstb], [psO])
                        P.cp("act", orblk[:, j, ts_], psO[:, 0:128], [psO], [orblk])
                        yield
                    if j == 3:
                        P.dma("sp", ORT[:, :, tb * 512:(tb + 1) * 512].rearrange("k p t -> p k t"), orblk.ap, [orblk], [])

                def chain(*gens):
                    for g in gens:
                        yield from g

                def interleave(ga, gb, ra=1, rb=1):
                    alive_a, alive_b = True, True
                    while alive_a or alive_b:
                        for _ in range(ra):
                            if alive_a:
                                try:
                                    next(ga)
                                except StopIteration:
                                    alive_a = False
                        for _ in range(rb):
                            if alive_b:
                                try:
                                    next(gb)
                                except StopIteration:
                                    alive_b = False
                units = [(tb, j) for tb in range(NTB) for j in range(4)]

                def genAfull(u):
                    tb, j = units[u]
                    if j == 0:
                        return chain(genS(tb), genA(tb, j))
                    return genA(tb, j)
                for _ in genAfull(0):
                    pass
                for u in range(len(units)):
                    gb = genB(*units[u])
                    if flags.get("seqB"):
                        for _ in gb:
                            pass
                        if u + 1 < len(units):
                            for _ in genAfull(u + 1):
                                pass
                    elif u + 1 < len(units):
                        interleave(genAfull(u + 1), gb, flags.get("ra", 1), flags.get("rb", 1))
                    else:
                        for _ in gb:
                            pass
                P.barrier()
                P.off = mark1
            else:
                z = P.alloc([4, S], BF16)
                P.memset("pool", z.ap, 0.0, [z])
                for j in range(4):
                    P.dma("sp", ORT[j], z[:, j, :], [z], [])
                P.barrier()
                P.off = mark1
            if flags.get("attn", True):
                Wq = P.alloc([8, 512], BF16)
                ropeb = P.alloc([2 * S], BF16)
                for kc in range(8):
                    rws = slice(kc * 128, (kc + 1) * 128)
                    wload(Wq[:, kc, :], w_in[rws, 1792:2304], [], [Wq])
                for q4 in range(4):
                    wload(ropeb[:, q4 * 2048:(q4 + 1) * 2048], rope_d[:, q4 * 2048:(q4 + 1) * 2048], [], [ropeb])
                cosb = ropeb[:, 0:S]
                sinb = ropeb[:, S:2 * S]
                KT = P.alloc([4, S], BF16)
                Vx = P.alloc([NT, 4, 130], BF16)
                P.memset("pool", Vx[:, :, :, 128:130], 1.0, [Vx])
                lt = P.alloc([64])
                ls = P.alloc([2])
                lam = P.alloc([1])
                nlam = P.alloc([1])
                for k in range(2):
                    P.tt("dve", lt.ap, rowb[:, RB_LAM + 128 * k:RB_LAM + 128 * k + 64],
                         rowb[:, RB_LAM + 128 * k + 64:RB_LAM + 128 * k + 128], ALU.mult, [rowb], [lt])
                    P.red(ls[:, k:k + 1], lt.ap, ALU.add, [lt], [ls])
                P.act(ls.ap, ls.ap, AF.Exp, [ls], [ls])
                P.tt("dve", lam.ap, ls[:, 0:1], ls[:, 1:2], ALU.subtract, [ls], [lam])
                P.ts("dve", nlam.ap, lam.ap, 0.2, -1.0, ALU.add, ALU.mult, [lam], [nlam])
                qsets = [(P.alloc([512]), P.alloc([512], BF16), P.alloc([512]), P.alloc([512], BF16), P.alloc([512]),
                          P.alloc([512])) for _ in range(3)]
                QT = P.alloc([512], BF16)
                mkW = P.off
                Wk = P.alloc([8, 512], BF16)
                Wv = P.alloc([8, 512], BF16)
                for kc in range(8):
                    rws = slice(kc * 128, (kc + 1) * 128)
                    wload(Wk[:, kc, :], w_in[rws, 2304:2816], [], [Wk])
                    wload(Wv[:, kc, :], w_in[rws, 2816:3328], [], [Wv])

                hld = {}

                def hload(blk):
                    if hld.get(blk % 2) != blk:
                        P.dma("sp", hblk[blk % 2].ap, HTD[:, :, blk * 512:(blk + 1) * 512], [HTDt], [hblk[blk % 2]])
                        hld[blk % 2] = blk
                    return hblk[blk % 2]

                def qk_gen(W, gcol, blk, h, dst, dstbuf, k):
                    raw, sqb, rsb, knb, ta, tb_ = qsets[k]
                    P.sec = "a_prep"
                    tsl = slice(blk * 512, (blk + 1) * 512)
                    hc = hload(blk)
                    ps = P.psum()
                    for kc in range(8):
                        P.mm(ps.ap, W[:, kc, h * 128:(h + 1) * 128], hc[:, kc, :], kc == 0, kc == 7, [W, hc], [ps])
                    P.cp("act", raw.ap, ps.ap, [ps], [raw])
                    yield
                    P.sec = "a_prep"
                    P.tt("pool", sqb.ap, raw.ap, raw.ap, ALU.mult, [raw], [sqb])
                    yield
                    P.sec = "a_prep"
                    ps2 = P.psum()
                    P.mm(ps2.ap, cstb[:, C_BO:C_BO + 128], sqb.ap, True, True, [cstb, sqb], [ps2])
                    P.rstd(rsb.ap, ps2.ap, 1.0 / 64, 1e-6, [ps2], [rsb])
                    yield
                    P.sec = "a_prep"
                    P.stt("dve", knb.ap, raw.ap, colp[:, gcol:gcol + 1], rsb.ap, ALU.mult, ALU.mult, [raw, colp, rsb], [knb])
                    yield
                    P.sec = "a_prep"
                    ps3 = P.psum()
                    P.mm(ps3.ap, cstb[:, C_RM:C_RM + 128], knb.ap, True, True, [cstb, knb], [ps3])
                    P.tt("dve", tb_.ap, ps3.ap, sinb[:, tsl], ALU.mult, [ps3, ropeb], [tb_])
                    yield
                    P.sec = "a_prep"
                    P.tt("pool", ta.ap, knb.ap, cosb[:, tsl], ALU.mult, [knb, ropeb], [ta])
                    yield
                    P.sec = "a_prep"
                    P.tt("pool", dst, ta.ap, tb_.ap, ALU.add, [ta, tb_], [dstbuf])
                    yield

                def qk_prep(W, gcol, blk, h, dst, dstbuf):
                    for _ in qk_gen(W, gcol, blk, h, dst, dstbuf, 0):
                        pass

                def v_gen(blk):
                    hc = hload(blk)
                    for i in range(blk * 4, blk * 4 + 4):
                        P.sec = "a_v"
                        ps = P.psum()
                        for kc in range(8):
                            P.mm(ps.ap, hc[:, kc, (i % 4) * 128:(i % 4 + 1) * 128], Wv[:, kc, :], kc == 0, kc == 7, [hc, Wv], [ps])
                        P.cp("act" if i % 2 else "dve", Vx[:, i, :, 0:128], ps.ap.rearrange("p (h c) -> p h c", h=4), [ps], [Vx])
                        yield

                def run_rr(gen_list, width):
                    active = []
                    it = iter(gen_list)
                    done = False
                    while True:
                        while len(active) < width and not done:
                            try:
                                active.append(next(it))
                            except StopIteration:
                                done = True
                        if not active:
                            break
                        for g in list(active):
                            try:
                                next(g)
                            except StopIteration:
                                active.remove(g)
                KTs = [T(KT.ap) for _ in range(4)]
                kgens = []
                cnt_ = 0
                for blk in range(8):
                    for h in range(4):
                        kgens.append((Wk, CP_KN, blk, h, KT[:, h, blk * 512:(blk + 1) * 512], KTs[h], cnt_ % 3))
                        cnt_ += 1
                    kgens.append(("v", blk))

                def mk(g):
                    if g[0] == "v":
                        return v_gen(g[1])
                    return qk_gen(*g)
                run_rr((mk(g) for g in kgens), 3)
                P.barrier()
                P.off = mkW
                P.ps_pool = [0, 1, 2, 3, 4]
                P.ps_rr = 0
                Ob = [P.ps_tiles[5], P.ps_tiles[6], P.ps_tiles[7]]

                def Oacc(c, qt):
                    n = c * 4 + qt
                    return Ob[n // 3], slice((n % 3) * 130, (n % 3) * 130 + 129)
                ptl = [P.alloc([512], BF16) for _ in range(6)]
                QT2 = P.alloc([512], BF16)
                Osb = [P.alloc([3, 390])] * 2
                pti = 0
                rz = P.alloc([2])
                nrz = P.alloc([1])
                tA = P.alloc([128])
                o32 = P.alloc([128])
                junk2 = P.alloc([128])
                ss = P.alloc([1])
                rs_ = P.alloc([1])
                otk = P.alloc([128], BF16)
                odblk = P.alloc([4, 512], BF16)
                QTs = [QT, QT2]
                LOOK = flags.get("look", 4)
                NQB = flags.get("nqb", 8)
                heads = [(qb, h) for qb in range(NQB) for h in range(4)]
                ptc = [0]
                zb = cstb[:, C_LS:C_LS + 128]

                def clear_banks():
                    for ob_ in Ob:
                        P.mm(ob_[:, 400:402], zb, cstb[:, C_HS:C_HS + 2], True, True, [cstb], [ob_])

                class Ctx:
                    pass

                def make(n):
                    c = Ctx()
                    c.qb, c.h = heads[n]
                    c.QT = QTs[n % 2]
                    c.items = [(kt, cc) for kt in range(4 * c.qb + 4) for cc in range(2)]
                    c.pts = {}
                    return c

                def stage1(c, i):
                    P.sec = "a_s1"
                    kt, cc = c.items[i]
                    r_ = max(kt - 4 * c.qb, 0)
                    c0 = r_ * 128
                    N = 512 - c0
                    cs = slice(64 * cc, 64 * cc + 64)
                    ps = P.psum()
                    P.mm(ps[:, 0:N], KT[cs, c.h, kt * 128:(kt + 1) * 128], c.QT[cs, c0:512], True, True, [KT, c.QT], [ps])
                    pt = ptl[ptc[0] % 6]
                    ptc[0] += 1
                    c.pts[i] = pt
                    P.act(pt[:, 0:N], ps[:, 0:N], AF.Exp, [ps], [pt], scale=0.125)
                    if kt >= 4 * c.qb:
                        P.tt("pool", pt[:, 0:128], pt[:, 0:128], cstb[:, C_UI:C_UI + 128], ALU.mult, [pt, cstb], [pt])

                def stage2(c, i):
                    P.sec = "a_s2"
                    kt, cc = c.items[i]
                    r_ = max(kt - 4 * c.qb, 0)
                    pt = c.pts.pop(i)
                    for qt in range(r_, 4):
                        col = (qt - r_) * 128
                        ob, osl = Oacc(cc, qt)
                        P.mm(ob[:, osl], pt[:, col:col + 128], Vx[:, kt, c.h, 0:129], False, False,
                             [pt, Vx], [ob], skip=True)
                clear_banks()
                qk_prep(Wq, CP_QN, 0, 0, QTs[0].ap, QTs[0])
                cur = make(0)
                for i in range(min(LOOK, len(cur.items))):
                    stage1(cur, i)
                for n in range(len(heads)):
                    c = cur
                    qb, h = c.qb, c.h
                    tsl = slice(qb * 512, (qb + 1) * 512)
                    qg = None
                    if n + 1 < len(heads):
                        nqb, nh_ = heads[n + 1]
                        qg = qk_gen(Wq, CP_QN, nqb, nh_, QTs[(n + 1) % 2].ap, QTs[(n + 1) % 2], n % 3)
                    if flags.get("pair", 1):
                        for i in range(0, len(c.items), 2):
                            for d_ in (0, 1):
                                if i + d_ + LOOK < len(c.items):
                                    stage1(c, i + d_ + LOOK)
                            for d_ in (0, 1):
                                stage2(c, i + d_)
                                if qg is not None:
                                    try:
                                        next(qg)
                                    except StopIteration:
                                        qg = None
                    else:
                        for i in range(len(c.items)):
                            if i + LOOK < len(c.items):
                                stage1(c, i + LOOK)
                            stage2(c, i)
                            if qg is not None:
                                try:
                                    next(qg)
                                except StopIteration:
                                    qg = None
                    if qg is not None:
                        for _ in qg:
                            pass
                    P.sec = "a_tail"
                    osb = Osb[n % 2]
                    for bi, ob_ in enumerate(Ob):
                        P.cp("act" if bi == 1 else "dve", osb[:, bi, :], ob_[:, 0:390], [ob_], [osb])
                    clear_banks()
                    if n + 1 < len(heads):
                        cur = make(n + 1)
                        for i in range(min(LOOK, len(cur.items))):
                            stage1(cur, i)
                    P.sec = "a_tail"

                    def Osl(cc, qt):
                        nn = cc * 4 + qt
                        return osb[:, nn // 3, (nn % 3) * 130:(nn % 3) * 130 + 129]
                    for qt in range(4):
                        O0 = Osl(0, qt)
                        O1 = Osl(1, qt)
                        P.recip(rz[:, 0:1], O0[:, 128:129], [osb], [rz])
                        P.recip(rz[:, 1:2], O1[:, 128:129], [osb], [rz])
                        P.tt("dve", nrz.ap, rz[:, 1:2], nlam.ap, ALU.mult, [rz, nlam], [nrz])
                        P.ts("pool", tA.ap, O1[:, 0:128], nrz[:, 0:1], None, ALU.mult, None, [osb, nrz], [tA])
                        P.stt("dve", o32.ap, O0[:, 0:128], rz[:, 0:1], tA.ap, ALU.mult, ALU.add, [osb, rz, tA], [o32])
                        P.tt("pool", junk2.ap, o32.ap, o32.ap, ALU.mult, [o32], [junk2])
                        P.red(ss.ap, junk2.ap, ALU.add, [junk2], [ss])
                        P.rstd(rs_.ap, ss.ap, 1.0 / 128, 1e-6, [ss], [rs_])
                        P.ts("dve", rs_.ap, rs_.ap, 0.8, None, ALU.mult, None, [rs_], [rs_])
                        P.stt("dve", otk.ap, o32.ap, rs_[:, 0:1], rowb[:, RB_SUBLN:RB_SUBLN + 128], ALU.mult, ALU.mult,
                              [o32, rs_, rowb], [otk])
                        ps = P.psum()
                        P.mm(ps[:, 0:128], otk.ap, identb, True, True, [otk, cstb], [ps])
                        P.cp("dve", odblk[:, h, qt * 128:(qt + 1) * 128], ps[:, 0:128], [ps], [odblk])
                    if h == 3:
                        P.dma("sp", ODT[:, :, tsl].rearrange("k p t -> p k t"), odblk.ap, [odblk], [])
                P.ps_pool = list(range(8))
                P.ps_rr = 0
                P.barrier()
                P.off = mark1
            else:
                z = P.alloc([4, S], BF16)
                P.memset("pool", z.ap, 0.0, [z])
                for j in range(4):
                    P.dma("sp", ODT[j], z[:, j, :], [z], [])
                P.barrier()
                P.off = mark1
        else:
            z = P.alloc([4, S], BF16)
            P.memset("pool", z.ap, 0.0, [z])
            for j in range(4):
                P.dma("sp", ORT[j], z[:, j, :], [z], [])
                P.dma("sp", ODT[j], z[:, j, :], [z], [])
            P.barrier()
            P.off = mark1

        if flags.get("stop") == 2:
            P.emit(es)
            return nc
        Wg = P.alloc([8, 2048], BF16)
        Wbr = P.alloc([4, D], BF16)
        Wbd = P.alloc([4, D], BF16)
        Wout = P.alloc([8, D], BF16)
        Wr = P.alloc([8, 36])
        for kc in range(8):
            wload(Wg[:, kc, :], w_in[kc * 128:(kc + 1) * 128, 3328:5376], [], [Wg])
            wload(Wout[:, kc, :], wout_d[kc * 128:(kc + 1) * 128, :], [], [Wout])
            P.dma("sp", Wr[:, kc, :], wr_d[kc * 128:(kc + 1) * 128, :], [], [Wr])
        for kc in range(4):
            wload(Wbr[:, kc, :], wbr_d[kc * 128:(kc + 1) * 128, :], [], [Wbr])
            wload(Wbd[:, kc, :], wbd_d[kc * 128:(kc + 1) * 128, :], [], [Wbd])
        orb = [P.alloc([4, 512], BF16) for _ in range(2)]
        odb = [P.alloc([4, 512], BF16) for _ in range(2)]
        sgr = [P.alloc([512]) for _ in range(2)]
        sgd = [P.alloc([512]) for _ in range(2)]
        t1 = [P.alloc([512]) for _ in range(2)]
        t2 = [P.alloc([512]) for _ in range(2)]
        mixT = [P.alloc([8, 512], BF16) for _ in range(2)]
        NS4 = 2
        xt = [P.alloc([D]) for _ in range(NS4)]
        x2 = [P.alloc([D]) for _ in range(NS4)]
        h2 = [P.alloc([D]) for _ in range(NS4)]
        junks = [P.alloc([D], BF16) for _ in range(NS4)]
        ssq2 = [P.alloc([1]) for _ in range(NS4)]
        rs2 = [P.alloc([1]) for _ in range(NS4)]
        h2his = [P.alloc([D], BF16) for _ in range(NS4)]
        h2los = [P.alloc([D], BF16) for _ in range(NS4)]
        h2Tls = [P.alloc([8, 128], BF16) for _ in range(NS4)]
        h2Tbs = [P.alloc([8, 128], BF16) for _ in range(NS4)]
        Wrh = P.alloc([8, 36], BF16)
        Wrl = P.alloc([8, 36], BF16)
        P.cp("dve", Wrh.ap, Wr.ap, [Wr], [Wrh])
        P.tt("dve", Wrl.ap, Wr.ap, Wrh.ap, ALU.subtract, [Wr, Wrh], [Wrl])
        LGs = [T(LG.ap) for _ in range(NT)]

        def genM(tb):
            ob, db, mx = orb[tb % 2], odb[tb % 2], mixT[tb % 2]
            tsl = slice(tb * 512, (tb + 1) * 512)
            hc = hblk[tb % 2]
            P.dma("sp", hc.ap, HTD[:, :, tsl], [HTDt], [hc])
            P.dma("sp", ob.ap, ORT[:, :, tsl].rearrange("k p t -> p k t"), [], [ob])
            P.dma("sp", db.ap, ODT[:, :, tsl].rearrange("k p t -> p k t"), [], [db])
            yield
            for m in range(8):
                q = m % 2
                ps = P.psum()
                for kc in range(8):
                    P.mm(ps.ap, Wg[:, kc, m * 128:(m + 1) * 128], hc[:, kc, :], kc == 0, kc == 7, [Wg, hc], [ps])
                P.act(sgr[q].ap, ps.ap, AF.Sigmoid, [ps], [sgr[q]])
                ps = P.psum()
                for kc in range(8):
                    P.mm(ps.ap, Wg[:, kc, 1024 + m * 128:1024 + (m + 1) * 128], hc[:, kc, :], kc == 0, kc == 7,
                         [Wg, hc], [ps])
                P.act(sgd[q].ap, ps.ap, AF.Sigmoid, [ps], [sgd[q]])
                yield
                ps = P.psum()
                for kc in range(4):
                    P.mm(ps.ap, Wbr[:, kc, m * 128:(m + 1) * 128], ob[:, kc, :], kc == 0, kc == 3, [Wbr, ob], [ps])
                P.tt("dve", t1[q].ap, ps.ap, sgr[q].ap, ALU.mult, [ps, sgr[q]], [t1[q]])
                ps = P.psum()
                for kc in range(4):
                    P.mm(ps.ap, Wbd[:, kc, m * 128:(m + 1) * 128], db[:, kc, :], kc == 0, kc == 3, [Wbd, db], [ps])
                P.tt("dve", t2[q].ap, ps.ap, sgd[q].ap, ALU.mult, [ps, sgd[q]], [t2[q]])
                P.tt("pool", mx[:, m, :], t1[q].ap, t2[q].ap, ALU.add, [t1[q], t2[q]], [mx])
                yield

        def genT(i):
            tb, tt_ = divmod(i, 4)
            mx = mixT[tb % 2]
            k = i % NS4
            rows = slice(i * 128, (i + 1) * 128)
            xt_, x2_, h2_, junk_, ssq_, rs_, hi_, lo_, Tl_, Tb_ = (xt[k], x2[k], h2[k], junks[k], ssq2[k], rs2[k], h2his[k],
                                                                  h2los[k], h2Tls[k], h2Tbs[k])
            P.dma("sp", xt_.ap, x_d[rows, :], [], [xt_])
            for hc_ in range(2):
                ps = P.psum()
                for m in range(8):
                    P.mm(ps.ap, mx[:, m, tt_ * 128:(tt_ + 1) * 128], Wout[:, m, hc_ * 512:(hc_ + 1) * 512],
                         m == 0, m == 7, [mx, Wout], [ps])
                P.tt("dve", x2_[:, hc_ * 512:(hc_ + 1) * 512], ps.ap, xt_[:, hc_ * 512:(hc_ + 1) * 512], ALU.add,
                     [ps, xt_], [x2_])
            P.dma("sp", X2[rows, :], x2_.ap, [x2_], [])
            yield
            P.act(junk_.ap, x2_.ap, AF.Square, [x2_], [junk_, ssq_], accum_out=ssq_.ap)
            yield
            P.rstd(rs_.ap, ssq_.ap, 1.0 / D, 1e-6, [ssq_], [rs_])
            yield
            P.act(h2_.ap, x2_.ap, AF.Copy, [x2_, rs_], [h2_], scale=rs_[:, 0:1])
            yield
            P.tt("pool", h2_.ap, h2_.ap, rowb[:, RB_GFFN:RB_GFFN + D], ALU.mult, [h2_, rowb], [h2_])
            yield
            P.cp("act", hi_.ap, h2_.ap, [h2_], [hi_])
            yield
            P.tt("pool", lo_.ap, h2_.ap, hi_.ap, ALU.subtract, [h2_, hi_], [lo_])
            yield
            for src, dst in ((hi_, Tb_), (lo_, Tl_)):
                for hf in range(2):
                    ps = P.psum()
                    for kk in range(4):
                        kc = hf * 4 + kk
                        P.mm(ps[:, kk * 128:(kk + 1) * 128], src[:, kc * 128:(kc + 1) * 128], identb, True, True,
                             [src, cstb], [ps])
                    P.cp("act" if hf == 0 else "dve", dst[:, hf * 4:hf * 4 + 4, :],
                         ps.ap.rearrange("p (a b) -> p a b", a=4), [ps], [dst])
                    yield
            P.dma("sp", H2T[:, :, rows].rearrange("k p t -> p k t"), Tb_.ap, [Tb_], [])
            ps = P.psum()
            n = 0
            for kc in range(8):
                for (a_, b_) in ((Tb_, Wrh), (Tb_, Wrl), (Tl_, Wrh)):
                    P.mm(ps[:, 0:36], a_[:, kc, :], b_[:, kc, :], n == 0, n == 23, [a_, b_], [ps])
                    n += 1
            P.tt("dve", LG[:, i, :], ps[:, 0:36], rowb[:, RB_RBIAS:RB_RBIAS + 36], ALU.add, [ps, rowb], [LGs[i]])
            yield

        for _ in genM(0):
            pass
        for tb in range(8):
            grp = [genM(tb + 1)] if tb + 1 < 8 else []
            grp += [genT(tb * 4 + k) for k in range(4)]
            run_rr4(iter(grp), 3 if tb + 1 < 8 else 2)
        P.barrier()
        P.off = base_off

        if flags.get("stop") == 3:
            P.emit(es)
            return nc
        mark2 = P.off
        glog = LG[:, :, 0:4]
        elog = LG[:, :, 4:36].rearrange("p t (g j) -> p t g j", g=4)
        gmax = P.alloc([NT])
        gsh = P.alloc([NT, 4])
        gex = P.alloc([NT, 4])
        gsum = P.alloc([NT])
        gtop = P.alloc([NT])
        gmask = P.alloc([NT, 4])
        tmp4 = P.alloc([NT, 4, 8])
        esel = P.alloc([NT, 8])
        esel2 = P.alloc([NT, 8])
        m1 = P.alloc([NT])
        m2 = P.alloc([NT])
        mk1 = P.alloc([NT, 8])
        mk2 = P.alloc([NT, 8])
        dm = P.alloc([NT])
        w1 = P.alloc([NT])
        w2 = P.alloc([NT])
        wsel = P.alloc([NT, 8])
        wsel2 = P.alloc([NT, 8])
        P.red(gmax.ap, glog, ALU.max, [LG], [gmax])
        P.tt("dve", gsh.ap, glog, bc(gmax.ap.unsqueeze(2), [128, NT, 4]), ALU.subtract, [LG, gmax], [gsh])
        P.act(gex.ap, gsh.ap, AF.Exp, [gsh], [gex])
        P.red(gsum.ap, gex.ap, ALU.add, [gex], [gsum])
        P.recip(gtop.ap, gsum.ap, [gsum], [gtop])
        P.ts("dve", gmask.ap, gsh.ap, 0.0, None, ALU.is_equal, None, [gsh], [gmask])
        P.tt("dve", tmp4.ap, elog, bc(gmask.ap.unsqueeze(3), [128, NT, 4, 8]), ALU.mult, [LG, gmask], [tmp4])
        P.red(esel.ap, tmp4.ap.rearrange("p t g j -> p t j g"), ALU.add, [tmp4], [esel])
        P.red(m1.ap, esel.ap, ALU.max, [esel], [m1])
        P.tt("dve", mk1.ap, esel.ap, bc(m1.ap.unsqueeze(2), [128, NT, 8]), ALU.is_equal, [esel, m1], [mk1])
        P.stt("dve", esel2.ap, mk1.ap, -1e30, esel.ap, ALU.mult, ALU.add, [mk1, esel], [esel2])
        P.red(m2.ap, esel2.ap, ALU.max, [esel2], [m2])
        P.tt("dve", mk2.ap, esel2.ap, bc(m2.ap.unsqueeze(2), [128, NT, 8]), ALU.is_equal, [esel2, m2], [mk2])
        P.tt("dve", dm.ap, m1.ap, m2.ap, ALU.subtract, [m1, m2], [dm])
        P.act(w1.ap, dm.ap, AF.Sigmoid, [dm], [w1])
        P.tt("dve", w1.ap, w1.ap, gtop.ap, ALU.mult, [w1, gtop], [w1])
        P.tt("dve", w2.ap, gtop.ap, w1.ap, ALU.subtract, [gtop, w1], [w2])
        P.tt("dve", wsel.ap, mk1.ap, bc(w1.ap.unsqueeze(2), [128, NT, 8]), ALU.mult, [mk1, w1], [wsel])
        P.tt("dve", wsel2.ap, mk2.ap, bc(w2.ap.unsqueeze(2), [128, NT, 8]), ALU.mult, [mk2, w2], [wsel2])
        P.tt("dve", wsel.ap, wsel.ap, wsel2.ap, ALU.add, [wsel, wsel2], [wsel])
        P.tt("dve", WT.ap.rearrange("p t (g j) -> p t g j", g=4), bc(gmask.ap.unsqueeze(3), [128, NT, 4, 8]),
             bc(wsel.ap.unsqueeze(2), [128, NT, 4, 8]), ALU.mult, [gmask, wsel], [WT])
        P.barrier()
        P.off = mark2

        if flags.get("stop") == 4:
            P.emit(es)
            return nc
        acc = [P.alloc([D]) for _ in range(16)]
        h2h = P.alloc([8, 2048], BF16)
        Wgu = [P.alloc([8, 1024], BF16) for _ in range(2)]
        Wd = [P.alloc([4, D], BF16) for _ in range(2)]
        sg = [P.alloc([512]) for _ in range(2)]
        actT = [P.alloc([4, 512], BF16) for _ in range(2)]
        ssq3s = [P.alloc([1]) for _ in range(2)]
        rs3s = [P.alloc([1]) for _ in range(2)]
        xn3 = [P.alloc([D], BF16) for _ in range(2)]
        x3T = [P.alloc([8, 128], BF16) for _ in range(2)]
        pt_ = [P.alloc([256]) for _ in range(2)]
        ptb = [P.alloc([256], BF16) for _ in range(2)]
        pT = [P.alloc([2, 128], BF16) for _ in range(2)]
        sgp = [P.alloc([D])] * 2
        yo = [P.alloc([D]) for _ in range(2)]
        NE = flags.get("n_experts", 32)
        for half in range(2):
            t0 = half * 2048
            for k in range(16):
                P.dma("sp", acc[k].ap, X2[t0 + k * 128:t0 + (k + 1) * 128, :], [], [acc[k]])
            P.dma("sp", h2h.ap, H2T[:, :, t0:t0 + 2048].rearrange("k p t -> p k t"), [], [h2h])
            def load_expert(e):
                wg_, wd_ = Wgu[e % 2], Wd[e % 2]
                for kc in range(8):
                    wload(wg_[:, kc, 0:512], wg_d[e, kc * 128:(kc + 1) * 128, :], [], [wg_])
                    wload(wg_[:, kc, 512:1024], wu_d[e, kc * 128:(kc + 1) * 128, :], [], [wg_])
                for kc in range(4):
                    wload(wd_[:, kc, :], wd_d[e, kc * 128:(kc + 1) * 128, :], [], [wd_])

            def stageG(u):
                e, tb = divmod(u, 4)
                if tb == 0 and e + 1 < NE:
                    load_expert(e + 1)
                wg_ = Wgu[e % 2]
                at = actT[u % 2]
                tsl = slice(tb * 512, (tb + 1) * 512)
                for fc in range(4):
                    q = fc % 2
                    psg = P.psum()
                    for kc in range(8):
                        P.mm(psg.ap, wg_[:, kc, fc * 128:(fc + 1) * 128], h2h[:, kc, tsl], kc == 0, kc == 7,
                             [wg_, h2h], [psg])
                    psu = P.psum()
                    for kc in range(8):
                        P.mm(psu.ap, wg_[:, kc, 512 + fc * 128:512 + (fc + 1) * 128], h2h[:, kc, tsl], kc == 0,
                             kc == 7, [wg_, h2h], [psu])
                    P.act(sg[q].ap, psg.ap, AF.Silu, [psg], [sg[q]])
                    P.tt("dve", at[:, fc, :], psu.ap, sg[q].ap, ALU.mult, [psu, sg[q]], [at])

            def stageD(u):
                e, tb = divmod(u, 4)
                wd_ = Wd[e % 2]
                at = actT[u % 2]
                for tt_ in range(4):
                    k = tb * 4 + tt_
                    gi = half * 16 + k
                    for hc in range(2):
                        ps = P.psum()
                        for fc in range(4):
                            P.mm(ps.ap, at[:, fc, tt_ * 128:(tt_ + 1) * 128], wd_[:, fc, hc * 512:(hc + 1) * 512],
                                 fc == 0, fc == 3, [at, wd_], [ps])
                        P.stt("dve", acc[k][:, hc * 512:(hc + 1) * 512], ps.ap, WT[:, gi, e:e + 1],
                              acc[k][:, hc * 512:(hc + 1) * 512], ALU.mult, ALU.add, [ps, WT, acc[k]], [acc[k]])
            if NE > 0:
                load_expert(0)
                for u in range(NE * 4):
                    stageG(u)
                    stageD(u)
            Wpg, Wpp = Wgu[0], Wd[0]
            for kc in range(8):
                wload(Wpg[:, kc, :], wpg_d[kc * 128:(kc + 1) * 128, :], [], [Wpg])
            for kc in range(2):
                wload(Wpp[:, kc, :], wpp_d[kc * 128:(kc + 1) * 128, :], [], [Wpp])
            def genP(k):
                gi = half * 16 + k
                q = k % 2
                rows = slice(gi * 128, (gi + 1) * 128)
                a = acc[k]
                P.dma("sp", pt_[q].ap, p_d[rows, :], [], [pt_[q]])
                P.act(yo[q].ap, a.ap, AF.Square, [a], [yo[q], ssq3s[q]], accum_out=ssq3s[q].ap)
                yield
                P.rstd(rs3s[q].ap, ssq3s[q].ap, 1.0 / D, 1e-6, [ssq3s[q]], [rs3s[q]])
                yield
                P.act(xn3[q].ap, a.ap, AF.Copy, [a, rs3s[q]], [xn3[q]], scale=rs3s[q][:, 0:1])
                P.cp("pool", ptb[q].ap, pt_[q].ap, [pt_[q]], [ptb[q]])
                yield
                for hf in range(2):
                    ps = P.psum()
                    for kk in range(4):
                        kc = hf * 4 + kk
                        P.mm(ps[:, kk * 128:(kk + 1) * 128], xn3[q][:, kc * 128:(kc + 1) * 128], identb, True, True,
                             [xn3[q], cstb], [ps])
                    P.tt("dve", x3T[q][:, hf * 4:hf * 4 + 4, :], ps.ap.rearrange("p (a b) -> p a b", a=4),
                         bc(colp[:, CP_GPLE + hf * 4:CP_GPLE + hf * 4 + 4].unsqueeze(2), [128, 4, 128]), ALU.mult,
                         [ps, colp], [x3T[q]])
                    yield
                ps = P.psum()
                for kc in range(2):
                    P.mm(ps[:, kc * 128:(kc + 1) * 128], ptb[q][:, kc * 128:(kc + 1) * 128], identb, True, True,
                         [ptb[q], cstb], [ps])
                P.cp("act", pT[q].ap, ps[:, 0:256].rearrange("p (a b) -> p a b", a=2), [ps], [pT[q]])
                yield
                for hc in range(2):
                    cs = slice(hc * 512, (hc + 1) * 512)
                    ps = P.psum()
                    for kc in range(8):
                        P.mm(ps.ap, x3T[q][:, kc, :], Wpg[:, kc, cs], kc == 0, kc == 7, [x3T[q], Wpg], [ps])
                    P.act(sgp[q][:, cs], ps.ap, AF.Sigmoid, [ps], [sgp[q]])
                    ps = P.psum()
                    for kc in range(2):
                        P.mm(ps.ap, pT[q][:, kc, :], Wpp[:, kc, cs], kc == 0, kc == 1, [pT[q], Wpp], [ps])
                    P.tt("dve", yo[q][:, cs], ps.ap, sgp[q][:, cs], ALU.mult, [ps, sgp[q]], [yo[q]])
                    yield
                P.tt("pool", yo[q].ap, yo[q].ap, a.ap, ALU.add, [yo[q], a], [yo[q]])
                P.dma("sp", y_d[rows, :], yo[q].ap, [yo[q]], [])
                yield
            run_rr4((genP(k) for k in range(16)), 2)
        P.emit(es)
    return nc


def _host_consts():
    c = np.zeros((128, NCST), np.float32)
    i = np.arange(128)
    c[:, C_ID:C_ID + 128] = np.eye(128)
    c[:, C_US:C_US + 128] = (i[:, None] < i[None, :])
    c[:, C_UI:C_UI + 128] = (i[:, None] <= i[None, :])
    c[:, C_LS:C_LS + 128] = (i[:, None] > i[None, :])
    c[:, C_BO:C_BO + 128] = (i[:, None] // 64 == i[None, :] // 64)
    rm = np.zeros((128, 128), np.float32)
    for p in range(128):
        d = p % 64
        if d < 32:
            rm[p + 32, p] = -1.0
        else:
            rm[p - 32, p] = 1.0
    c[:, C_RM:C_RM + 128] = rm
    c[:, C_HS] = (i < 64)
    c[:, C_HS + 1] = (i >= 64)
    c[:, C_ONE:C_ONE + 128] = 1.0
    inv = (10000.0 ** (-np.arange(0, 64, 2, dtype=np.float32) / 64)).astype(np.float32)
    ang = np.arange(S, dtype=np.float32)[:, None] * inv[None, :]
    ang = np.concatenate([ang, ang], -1)
    cos = np.cos(ang).astype(np.float32).T
    sin = np.sin(ang).astype(np.float32).T
    rope = np.concatenate([np.tile(cos, (2, 1)), np.tile(sin, (2, 1))], axis=1)
    return c, np.ascontiguousarray(rope)


_CACHE = {}


def kernel(**inp):
    flags = inp.pop("_flags", {})
    key = tuple(sorted(flags.items()))
    if key not in _CACHE:
        _CACHE[key] = build(flags)
    nc = _CACHE[key]
    f = lambda a: np.ascontiguousarray(np.asarray(a, dtype=np.float32))
    colp = np.zeros((128, NCP), np.float32)
    colp[:, CP_GMIX:CP_GMIX + 8] = f(inp["norm_mix"])[0].reshape(8, 128).T
    colp[:, CP_GPLE:CP_GPLE + 8] = f(inp["norm_ple"])[0].reshape(8, 128).T
    colp[:, CP_W0:CP_W0 + 4] = f(inp["rwkv_w0"])[0].reshape(4, 128).T
    colp[:, CP_A0:CP_A0 + 4] = f(inp["rwkv_a0"])[0].reshape(4, 128).T
    colp[:, CP_KK:CP_KK + 4] = f(inp["rwkv_k_k"])[0].reshape(4, 128).T
    colp[:, CP_KA:CP_KA + 4] = f(inp["rwkv_k_a"])[0].reshape(4, 128).T
    colp[:, CP_RK:CP_RK + 4] = f(inp["rwkv_r_k"])[0].reshape(4, 128).T
    colp[:, CP_QN] = np.tile(f(inp["q_norm"])[0], 2)
    colp[:, CP_KN] = np.tile(f(inp["k_norm"])[0], 2)
    colp[:, CP_MU:CP_MU + 14] = f(inp["rwkv_mu"])[0].reshape(14, 128).T
    rowb = np.zeros((128, NRB), np.float32)
    rowb[:, RB_GFFN:RB_GFFN + D] = f(inp["norm_ffn"])[0][None, :]
    rowb[:, RB_RBIAS:RB_RBIAS + 4] = f(inp["b_group"])[0][None, :]
    rowb[:, RB_RBIAS + 4:RB_RBIAS + 36] = f(inp["b_expert_router"])[0][None, :]
    rowb[:, RB_LNW:RB_LNW + 512] = f(inp["rwkv_ln_w"])[0][None, :]
    rowb[:, RB_LNB:RB_LNB + 512] = f(inp["rwkv_ln_b"])[0][None, :]
    rowb[:, RB_SUBLN:RB_SUBLN + 128] = f(inp["subln_w"])[0][None, :]
    for k, nm in enumerate(["lambda_q1", "lambda_k1", "lambda_q2", "lambda_k2"]):
        rowb[:, RB_LAM + 64 * k:RB_LAM + 64 * (k + 1)] = f(inp[nm])[0][None, :]
    rowb[:, RB_W0:RB_W0 + 512] = f(inp["rwkv_w0"])[0][None, :]
    murow = np.ascontiguousarray(np.broadcast_to(f(inp["rwkv_mu"])[0][None, :], (128, 1792)))
    cst, rope = _host_consts()
    wr = np.concatenate([f(inp["w_group"])[0], f(inp["w_expert_router"])[0]], axis=1)
    shared = {
        "w_in": f(inp["w_in"])[0], "w2": f(inp["rwkv_w2"])[0], "a2": f(inp["rwkv_a2"])[0],
        "g2": f(inp["rwkv_g2"])[0], "wbr": f(inp["w_branch_rwkv"])[0], "wbd": f(inp["w_branch_diff"])[0],
        "wout": f(inp["w_out"])[0], "wr": np.ascontiguousarray(wr), "w_gate": f(inp["w_gate"])[0],
        "w_up": f(inp["w_up"])[0], "w_down": f(inp["w_down"])[0], "wpg": f(inp["w_ple_gate"])[0],
        "wpp": f(inp["w_ple_proj"])[0], "colp": colp, "rowb": rowb, "cst": cst, "rope": rope, "murow": murow,
    }
    x = f(inp["x"])
    p = f(inp["p"])[0]
    ncores = flags.get("ncores", 8)
    in_maps = [dict(shared, x=x[c], p=p[c]) for c in range(ncores)]
    if flags.get("trace"):
        res = run_bass_kernel_spmd(nc, in_maps, core_ids=list(range(ncores)), trace=True)
        kernel.exec_ns = res.exec_time_ns
    else:
        res = run_bass_kernel_spmd(nc, in_maps, core_ids=list(range(ncores)))
    out = np.stack([np.asarray(r["y"], dtype=np.float32) for r in res.results], axis=0)
    if flags.get("dbgout"):
        kernel.dbg = [(np.asarray(r["orts"]).astype(np.float32), np.asarray(r["odts"]).astype(np.float32)) for r in res.results]
    return out
```

```python
import numpy as np
from contextlib import ExitStack
import concourse.bass as bass
import concourse.mybir as mybir
from concourse.bass_utils import run_bass_kernel_spmd

F32 = mybir.dt.float32
BF16 = mybir.dt.bfloat16
ALU = mybir.AluOpType
AF = mybir.ActivationFunctionType
AX = mybir.AxisListType

S = 4096
D = 1024
NT = 32
ENG = ("pe", "act", "dve", "pool", "sp")
EPOCH = 8000
NDMA = 24
ARENA_WORDS = 53000

CP_GMIX = 0
CP_GPLE = 8
CP_W0 = 16
CP_A0 = 20
CP_KK = 24
CP_KA = 28
CP_RK = 32
CP_QN = 36
CP_KN = 37
CP_MU1 = 38
CP_MU = 52
CP_SUBLN = 66
NCP = 67
RB_GFFN = 0
RB_RBIAS = 1024
RB_LNW = 1060
RB_LNB = 1572
RB_SUBLN = 2084
RB_LAM = 2212
RB_W0 = 2468
NRB = 2980
C_ID = 0
C_US = 128
C_UI = 256
C_LS = 384
C_BO = 512
C_RM = 640
C_HS = 768
C_ONE = 770
NCST = 898


class _Stop(Exception):
    pass


class Buf:
    __slots__ = ("w", "r")

    def __init__(self):
        self.w = None
        self.r = []


class T:
    def __init__(self, ap, b=None):
        self.ap = ap
        self.b = b if b is not None else Buf()

    def __getitem__(self, k):
        return self.ap[k]


class Prog:
    def __init__(self, nc, arena, psums):
        self.nc = nc
        self.arena = arena
        self.off = 0
        self.streams = {e: [] for e in ENG}
        self.seq = {e: 0 for e in ENG}
        self.seen = {e: {} for e in ENG}
        self.seen_d = {e: {} for e in ENG}
        self.dcnt = [0] * NDMA
        self.drr = 0
        self.ps_tiles = [T(p) for p in psums]
        self.ps_pool = list(range(len(psums)))
        self.ps_rr = 0
        self.sec = ""
        self.annot = False

    def alloc(self, shape, dt=F32):
        n = int(np.prod(shape))
        words = n if dt == F32 else (n + 1) // 2
        assert self.off + words <= ARENA_WORDS, ("arena overflow", self.off, words)
        ap = self.arena[:, self.off:self.off + words]
        self.off += words
        if dt != F32:
            ap = ap.bitcast(dt)[:, 0:n]
        if len(shape) == 2:
            ap = ap.rearrange("p (a b) -> p a b", a=shape[0])
        elif len(shape) == 3:
            ap = ap.rearrange("p (a b c) -> p a b c", a=shape[0], b=shape[1])
        return T(ap)

    def psum(self):
        t = self.ps_tiles[self.ps_pool[self.ps_rr % len(self.ps_pool)]]
        self.ps_rr = (self.ps_rr + 1) % len(self.ps_pool)
        return t

    def _need(self, eng, r, w):
        toks = set()
        for t in r:
            if t.b.w is not None:
                toks.add(t.b.w)
        for t in w:
            if t.b.w is not None:
                toks.add(t.b.w)
            toks.update(t.b.r)
        st = self.streams[eng]
        for tok in sorted(toks):
            if tok[0] == "e":
                _, pe, sq = tok
                if pe == eng and eng in ("pe", "sp"):
                    continue
                if self.seen[eng].get(pe, 0) >= sq:
                    continue
                self.seen[eng][pe] = sq
                st.append(("wait", tok))
            else:
                _, slot, cnt = tok
                if self.seen_d[eng].get(slot, 0) >= cnt:
                    continue
                self.seen_d[eng][slot] = cnt
                st.append(("wait", tok))

    def _mark(self, tok, r, w):
        for t in r:
            t.b.r.append(tok)
        for t in w:
            t.b.w = tok
            t.b.r = []

    def op(self, eng, fn, r, w):
        self._need(eng, r, w)
        self.seq[eng] += 1
        tok = ("e", eng, self.seq[eng])
        if self.annot:
            sec = self.sec
            fn0 = fn
            fn = lambda e: fn0(e).annotate(sec)
        self.streams[eng].append(("op", fn, tok))
        self._mark(tok, r, w)

    def dma(self, q, out, in_, r, w):
        slot = self.drr
        self.drr = (slot + 1) % NDMA
        prev = self.dcnt[slot]
        self._need(q, r, w)
        if prev > 0 and self.seen_d[q].get(slot, 0) < prev:
            self.seen_d[q][slot] = prev
            self.streams[q].append(("wait", ("d", slot, prev)))
        self.dcnt[slot] = prev + 1
        tok = ("d", slot, prev + 1)
        self.streams[q].append(("dma", lambda e: e.dma_start(out=out, in_=in_), tok))
        self._mark(tok, r, w)

    def barrier(self):
        toks = [("e", e, self.seq[e]) for e in ENG if self.seq[e] > 0]
        toks += [("d", s, c) for s, c in enumerate(self.dcnt) if c > 0]
        for eng in ENG:
            for tok in toks:
                if tok[0] == "e":
                    if tok[1] == eng or self.seen[eng].get(tok[1], 0) >= tok[2]:
                        continue
                    self.seen[eng][tok[1]] = tok[2]
                else:
                    if self.seen_d[eng].get(tok[1], 0) >= tok[2]:
                        continue
                    self.seen_d[eng][tok[1]] = tok[2]
                self.streams[eng].append(("wait", tok))

    def act(self, out, in_, func, r, w, **kw):
        self.op("act", lambda e: e.activation(out=out, in_=in_, func=func, **kw), r, w)

    def tt(self, eng, out, in0, in1, op, r, w):
        self.op(eng, lambda e: e.tensor_tensor(out=out, in0=in0, in1=in1, op=op), r, w)

    def ts(self, eng, out, in0, s1, s2, op0, op1, r, w):
        if op1 is None:
            self.op(eng, lambda e: e.tensor_scalar(out=out, in0=in0, scalar1=s1, scalar2=None, op0=op0), r, w)
        else:
            self.op(eng, lambda e: e.tensor_scalar(out=out, in0=in0, scalar1=s1, scalar2=s2, op0=op0, op1=op1), r, w)

    def stt(self, eng, out, in0, scalar, in1, op0, op1, r, w):
        self.op(eng, lambda e: e.scalar_tensor_tensor(out=out, in0=in0, scalar=scalar, in1=in1, op0=op0, op1=op1), r, w)

    def cp(self, eng, out, in_, r, w):
        if eng == "act":
            self.act(out, in_, AF.Copy, r, w)
        else:
            self.op(eng, lambda e: e.tensor_copy(out=out, in_=in_), r, w)

    def red(self, out, in_, op, r, w):
        self.op("dve", lambda e: e.tensor_reduce(out=out, in_=in_, axis=AX.X, op=op), r, w)

    def recip(self, out, in_, r, w):
        self.op("dve", lambda e: e.reciprocal(out=out, in_=in_), r, w)

    def memset(self, eng, ap, val, w):
        self.op(eng, lambda e: e.memset(ap, val), [], w)

    def mm(self, out, lhsT, rhs, start, stop, r, w, skip=False):
        if skip:
            self.op("pe", lambda e: e.matmul(out, lhsT=lhsT, rhs=rhs, start=start, stop=stop, skip_group_check=True), r, w)
        else:
            self.op("pe", lambda e: e.matmul(out, lhsT=lhsT, rhs=rhs, start=start, stop=stop), r, w)

    def rstd(self, out, ssq, scale, eps, r, w):
        et = self.eps_tiles[eps]
        n = out.shape[0]
        self.act(out, ssq, AF.Sqrt, list(r) + [self.eps_T], w, scale=float(scale), bias=et[0:n, :])
        self.recip(out, out, w, w)

    def emit(self, es):
        nc = self.nc
        sems = {}

        def semh(tok):
            if tok[0] == "e":
                key = ("e", tok[1], (tok[2] - 1) // EPOCH)
                val = tok[2] - key[2] * EPOCH
            else:
                key = ("d", tok[1])
                val = 16 * tok[2]
            if key not in sems:
                sems[key] = es.enter_context(nc.semaphore("s_" + "_".join(str(k) for k in key)))
            return sems[key], val

        for s, c in enumerate(self.dcnt):
            if c > 0 and self.seen_d["sp"].get(s, 0) < c:
                self.streams["sp"].append(("wait", ("d", s, c)))
        for e in ENG:
            for it in self.streams[e]:
                semh(it[1] if it[0] == "wait" else it[2])
        block = es.enter_context(nc.Block())

        def replay(name):
            def f(eng):
                for it in self.streams[name]:
                    if it[0] == "wait":
                        s, v = semh(it[1])
                        eng.wait_ge(s, v)
                    elif it[0] == "op":
                        s, _ = semh(it[2])
                        it[1](eng).then_inc(s, 1)
                    else:
                        s, _ = semh(it[2])
                        it[1](eng).then_inc(s, 16)
            return f

        block.sync(replay("sp"))
        block.scalar(replay("act"))
        block.vector(replay("dve"))
        block.gpsimd(replay("pool"))
        block.tensor(replay("pe"))


def bc(ap, shape):
    return ap.to_broadcast(shape)


def build(flags):
    nc = bass.Bass("TRN2", target_bir_lowering=False)
    dr = {}

    def din(name, shape, dt=F32):
        dr[name] = nc.dram_tensor(name, list(shape), dt, kind="ExternalInput").ap()
        return dr[name]

    x_d = din("x", [S, D])
    p_d = din("p", [S, 256])
    w_in = din("w_in", [D, 5376])
    w2_d = din("w2", [64, 512])
    a2_d = din("a2", [64, 512])
    g2_d = din("g2", [128, 512])
    wbr_d = din("wbr", [512, D])
    wbd_d = din("wbd", [512, D])
    wout_d = din("wout", [D, D])
    wr_d = din("wr", [D, 36])
    wg_d = din("w_gate", [32, D, 512])
    wu_d = din("w_up", [32, D, 512])
    wd_d = din("w_down", [32, 512, D])
    wpg_d = din("wpg", [D, D])
    wpp_d = din("wpp", [256, D])
    colp_d = din("colp", [128, NCP])
    rowb_d = din("rowb", [128, NRB])
    cst_d = din("cst", [128, NCST])
    rope_d = din("rope", [128, 2 * S])
    murow_d = din("murow", [128, 1792])
    y_d = nc.dram_tensor("y", [S, D], F32, kind="ExternalOutput").ap()
    X2 = nc.dram_tensor("x2s", [S, D], F32, kind="Internal").ap()
    H2T = nc.dram_tensor("h2ts", [8, 128, S], BF16, kind="Internal").ap()
    okind = "ExternalOutput" if flags.get("dbgout") else "Internal"
    ORT = nc.dram_tensor("orts", [4, 128, S], BF16, kind=okind).ap()
    ODT = nc.dram_tensor("odts", [4, 128, S], BF16, kind=okind).ap()
    HTD = nc.dram_tensor("htd", [128, 8, S], BF16, kind="Internal").ap()
    dbg_d = None
    if flags.get("dbg"):
        dbg_d = nc.dram_tensor("dbg", [128, flags["dbg"]], F32, kind="ExternalOutput").ap()

    with ExitStack() as es:
        arena = es.enter_context(nc.sbuf_tensor("arena", [128, ARENA_WORDS], F32))
        psums = [es.enter_context(nc.psum_tensor("ps%d" % i, [128, 512], F32)) for i in range(8)]
        P = Prog(nc, arena, [p[:, :] for p in psums])
        P.annot = bool(flags.get("annot"))

        cst = P.alloc([NCST])
        colp = P.alloc([NCP])
        rowb = P.alloc([NRB])
        cstb = P.alloc([NCST], BF16)
        P.dma("sp", cst.ap, cst_d, [], [cst])
        P.dma("sp", colp.ap, colp_d, [], [colp])
        P.dma("sp", rowb.ap, rowb_d, [], [rowb])
        P.cp("dve", cstb.ap, cst.ap, [cst], [cstb])
        identb = cstb[:, C_ID:C_ID + 128]
        identf = cst[:, C_ID:C_ID + 128]
        LG = P.alloc([NT, 36])
        WT = P.alloc([NT, 32])
        epsc = P.alloc([4])
        P.eps_T = epsc
        P.eps_tiles = {}
        for ci, ev in enumerate((1e-6, 1e-24, 64e-5)):
            P.memset("dve", epsc[:, ci:ci + 1], ev, [epsc])
            P.eps_tiles[ev] = epsc[:, ci:ci + 1]
        base_off = P.off
        hblk = [P.alloc([8, 512], BF16) for _ in range(2)]
        HTDt = T(HTD)

        def wload(dst, src, r, w):
            P.dma("pool", dst, src, r, w)

        def run_rr4(gen_list, width):
            active = []
            it = iter(gen_list)
            done = False
            while True:
                while len(active) < width and not done:
                    try:
                        active.append(next(it))
                    except StopIteration:
                        done = True
                if not active:
                    break
                for g in list(active):
                    try:
                        next(g)
                    except StopIteration:
                        active.remove(g)

        mark1 = P.off
        xt1 = [P.alloc([D]) for _ in range(3)]
        xn1 = [P.alloc([D], BF16) for _ in range(3)]
        junk1 = [P.alloc([D], BF16) for _ in range(3)]
        ssq1 = [P.alloc([1]) for _ in range(3)]
        rs1 = [P.alloc([1]) for _ in range(3)]

        def gen1(i):
            k = i % 3
            a, b_ = xt1[k], xn1[k]
            P.dma("sp", a.ap, x_d[i * 128:(i + 1) * 128, :], [], [a])
            P.act(junk1[k].ap, a.ap, AF.Square, [a], [junk1[k], ssq1[k]], accum_out=ssq1[k].ap)
            yield
            P.rstd(rs1[k].ap, ssq1[k].ap, 1.0 / D, 1e-6, [ssq1[k]], [rs1[k]])
            yield
            P.act(b_.ap, a.ap, AF.Copy, [a, rs1[k]], [b_], scale=rs1[k][:, 0:1])
            yield
            for hf in range(2):
                ps = P.psum()
                for kk in range(4):
                    kc = hf * 4 + kk
                    P.mm(ps[:, kk * 128:(kk + 1) * 128], b_[:, kc * 128:(kc + 1) * 128], identb, True, True,
                         [b_, cstb], [ps])
                hb_ = hblk[(i // 4) % 2]
                P.tt("dve", hb_[:, hf * 4:hf * 4 + 4, (i % 4) * 128:(i % 4 + 1) * 128],
                     ps.ap.rearrange("p (a b) -> p a b", a=4),
                     bc(colp[:, CP_GMIX + hf * 4:CP_GMIX + hf * 4 + 4].unsqueeze(2), [128, 4, 128]), ALU.mult,
                     [ps, colp], [hb_])
                yield
            if i % 4 == 3:
                P.dma("sp", HTD[:, :, (i // 4) * 512:(i // 4 + 1) * 512], hblk[(i // 4) % 2].ap, [hblk[(i // 4) % 2]], [HTDt])
        run_rr4((gen1(i) for i in range(NT)), 3)
        P.barrier()
        P.off = mark1

        if flags.get("stop") == 1:
            P.emit(es)
            return nc
        if flags.get("mixers", True):

            EC = float(flags.get("ec", np.exp(-0.5)))
            if flags.get("rwkv", True):
                W1 = P.alloc([8, 1792], BF16)
                W2 = P.alloc([8, 1792], BF16)
                w2a2 = P.alloc([512], BF16)
                g2b = P.alloc([512], BF16)
                wload(w2a2[0:64, :], w2_d, [], [w2a2])
                wload(w2a2[64:128, :], a2_d, [], [w2a2])
                wload(g2b.ap, g2_d, [], [g2b])
                mk = P.off
                stage = P.alloc([1792])
                mur = P.alloc([1792])
                omm = P.alloc([1792])
                P.dma("sp", mur.ap, murow_d, [], [mur])
                P.ts("dve", omm.ap, mur.ap, -1.0, 1.0, ALU.mult, ALU.add, [mur], [omm])
                for kc in range(8):
                    P.dma("sp", stage.ap, w_in[kc * 128:(kc + 1) * 128, 0:1792], [], [stage])
                    P.tt("dve", W1[:, kc, :], stage.ap, omm.ap, ALU.mult, [stage, omm], [W1])
                    P.tt("pool", W2[:, kc, :], stage.ap, mur.ap, ALU.mult, [stage, mur], [W2])
                P.barrier()
                P.off = mk
                hprevs = [P.alloc([8, 512], BF16) for _ in range(2)]
                loras = [P.alloc([512], BF16) for _ in range(2)]
                sxgs = [P.alloc([512], BF16) for _ in range(2)]
                r32, k32, v32, a32, kq, rn, t1_, kmod = [P.alloc([512]) for _ in range(8)]
                kk_, tmp = kq, rn
                eng, egm = [P.alloc([512]) for _ in range(2)]
                egs = [P.alloc([512]) for _ in range(2)]
                vb, kq2b, BT, KTt = [P.alloc([512], BF16) for _ in range(4)]
                prodks = [P.alloc([512], BF16) for _ in range(2)]
                ARs = [P.alloc([4, 2, 128], BF16) for _ in range(2)]
                TMs = [P.alloc([4, 384], BF16) for _ in range(2)]
                ztoks = [P.alloc([128])] * 2
                sgt32s = [P.alloc([128])] * 2
                sghis = [P.alloc([128], BF16) for _ in range(2)]
                sglos = [P.alloc([128], BF16) for _ in range(2)]
                NRss = [[P.alloc([2, 2, 128], BF16) for _ in range(4)] for _ in range(2)]
                AKss = [[P.alloc([2, 2, 128], BF16) for _ in range(4)] for _ in range(2)]
                WtAss = [[P.alloc([2, 128], BF16) for _ in range(4)] for _ in range(2)]
                WtBss = [[P.alloc([2, 128], BF16) for _ in range(4)] for _ in range(2)]
                LLs = [P.alloc([2, 128], BF16) for _ in range(4)]
                PQs = [[P.alloc([512], BF16) for _ in range(2)] for _ in range(4)]
                Xb = P.alloc([128], BF16)
                Ub = P.alloc([128], BF16)
                T32 = [P.alloc([128]) for _ in range(4)]
                Tb = [P.alloc([128], BF16) for _ in range(4)]
                y32s = [P.alloc([128]) for _ in range(2)]
                ysqs = [P.alloc([128]) for _ in range(2)]
                yns = [P.alloc([128]) for _ in range(2)]
                stats = [[P.alloc([2]) for _ in range(7)] for _ in range(2)]
                otoks = [P.alloc([128], BF16) for _ in range(2)]
                orblks = [P.alloc([4, 512], BF16)] * 2
                for j in range(4):
                    P.memset("pool", T32[j].ap, 0.0, [T32[j]])
                    P.memset("pool", Tb[j].ap, 0.0, [Tb[j]])
                idb2 = bc(identb.unsqueeze(1), [128, 2, 128])
                NTB = flags.get("ntb", 8)
                WtFinal = {}

                def proj(tb, g, hc, hprev):
                    ps = P.psum()
                    cs = slice(g * 128, (g + 1) * 128)
                    for kc in range(8):
                        P.mm(ps.ap, W1[:, kc, cs], hc[:, kc, :], kc == 0, False, [W1, hc], [ps])
                        P.mm(ps.ap, W2[:, kc, cs], hprev[:, kc, :], False, kc == 7, [W2, hprev], [ps])
                    return ps

                def genS(tb):
                    P.sec = "r_S"
                    t0 = tb * 512
                    tsl = slice(t0, t0 + 512)
                    hc = hblk[tb % 2]
                    hprev = hprevs[tb % 2]
                    lora = loras[tb % 2]
                    sxg = sxgs[tb % 2]
                    P.dma("sp", hc.ap, HTD[:, :, tsl], [HTDt], [hc])
                    if tb == 0:
                        P.memset("dve", hprev.ap, 0.0, [hprev])
                        P.dma("sp", hprev[:, :, 1:512], HTD[:, :, 0:511], [HTDt], [hprev])
                    else:
                        P.dma("sp", hprev.ap, HTD[:, :, t0 - 1:t0 + 511], [HTDt], [hprev])
                    yield
                    ps = proj(tb, 12, hc, hprev)
                    P.act(lora[0:64, :], ps[0:64, :], AF.Tanh, [ps], [lora])
                    P.cp("dve", lora[64:128, :], ps[64:128, :], [ps], [lora])
                    yield
                    ps = proj(tb, 13, hc, hprev)
                    P.act(sxg.ap, ps.ap, AF.Sigmoid, [ps], [sxg])
                    yield

                def genA(tb, j):
                    par = (tb * 4 + j) % 2
                    hc = hblk[tb % 2]
                    hprev = hprevs[tb % 2]
                    lora = loras[tb % 2]
                    AR, TM, eg, prodk = ARs[par], TMs[par], egs[par], prodks[par]
                    ARf = AR.ap.rearrange("p t a b -> p t (a b)")
                    NRs, AKs, WtAs, WtBs = NRss[par], AKss[par], WtAss[par], WtBss[par]
                    js = slice(j * 128, (j + 1) * 128)
                    P.sec = "r_proj"
                    ps = proj(tb, j, hc, hprev)
                    P.cp("act", r32.ap, ps.ap, [ps], [r32])
                    yield
                    P.sec = "r_proj"
                    ps = proj(tb, 4 + j, hc, hprev)
                    P.cp("dve", k32.ap, ps.ap, [ps], [k32])
                    yield
                    P.sec = "r_proj"
                    ps = proj(tb, 8 + j, hc, hprev)
                    P.cp("act", v32.ap, ps.ap, [ps], [v32])
                    P.cp("pool", vb.ap, v32.ap, [v32], [vb])
                    yield
                    P.sec = "r_preptok"
                    ps = P.psum()
                    P.mm(ps.ap, w2a2[64:128, js], lora[64:128, :], True, True, [w2a2, lora], [ps])
                    P.act(a32.ap, ps.ap, AF.Sigmoid, [ps, colp], [a32], bias=colp[:, CP_A0 + j:CP_A0 + j + 1])
                    for tl in range(4):
                        yield
                        P.sec = "r_preptok"
                        ztok, sgt32, sghi, sglo = ztoks[tl % 2], sgt32s[tl % 2], sghis[tl % 2], sglos[tl % 2]
                        ts_ = slice(tl * 128, (tl + 1) * 128)
                        ps = P.psum()
                        P.mm(ps[:, 0:128], lora[0:64, ts_], w2a2[0:64, js], True, True, [lora, w2a2], [ps])
                        P.tt("dve", ztok.ap, ps[:, 0:128], rowb[:, RB_W0 + j * 128:RB_W0 + (j + 1) * 128], ALU.add,
                             [ps, rowb], [ztok])
                        P.act(sgt32.ap, ztok.ap, AF.Sigmoid, [ztok], [sgt32])
                        P.cp("pool", sghi.ap, sgt32.ap, [sgt32], [sghi])
                        P.tt("pool", sglo.ap, sgt32.ap, sghi.ap, ALU.subtract, [sgt32, sghi], [sglo])
                        yield
                        P.sec = "r_preptok"
                        psG = P.psum()
                        P.mm(psG[:, 0:256], sghi.ap, cstb[:, C_US:C_US + 256], True, False, [sghi, cstb], [psG])
                        P.mm(psG[:, 0:256], sglo.ap, cstb[:, C_US:C_US + 256], False, True, [sglo, cstb], [psG])
                        P.act(egm[:, ts_], psG[:, 0:128], AF.Exp, [psG], [egm], scale=-EC)
                        P.act(eg[:, ts_], psG[:, 128:256], AF.Exp, [psG], [eg], scale=-EC)
                        P.act(eng[:, ts_], psG[:, 128:256], AF.Exp, [psG], [eng], scale=EC)
                    yield
                    P.sec = "r_prepfm"
                    v4 = lambda t: t.ap.rearrange("p (a b) -> p a b", a=4)
                    P.ts("dve", kq.ap, k32.ap, colp[:, CP_KK + j:CP_KK + j + 1], None, ALU.mult, None, [k32, colp], [kq])
                    P.tt("pool", kq2b.ap, kq.ap, kq.ap, ALU.mult, [kq], [kq2b])
                    ps = P.psum()
                    P.mm(ps.ap, cstb[:, C_BO:C_BO + 128], kq2b.ap, True, True, [cstb, kq2b], [ps])
                    P.rstd(rn.ap, ps.ap, 1.0, 1e-24, [ps], [rn])
                    yield
                    P.sec = "r_prepfm"
                    P.tt("dve", kk_.ap, kq.ap, rn.ap, ALU.mult, [kq, rn], [kk_])
                    P.stt("dve", AR[:, :, 0, :], v4(kk_), -1.0, v4(egm), ALU.mult, ALU.mult, [kk_, egm], [AR])
                    yield
                    P.sec = "r_prepfm"
                    P.tt("dve", tmp.ap, kk_.ap, a32.ap, ALU.mult, [kk_, a32], [tmp])
                    P.tt("pool", BT.ap, tmp.ap, eng.ap, ALU.mult, [tmp, eng], [BT])
                    yield
                    P.sec = "r_prepfm"
                    P.ts("dve", t1_.ap, a32.ap, -1.0, colp[:, CP_KA + j:CP_KA + j + 1], ALU.add, ALU.mult, [a32, colp], [t1_])
                    P.stt("dve", kmod.ap, t1_.ap, 1.0, k32.ap, ALU.add, ALU.mult, [t1_, k32], [kmod])
                    yield
                    P.sec = "r_prepfm"
                    P.tt("dve", KTt.ap, kmod.ap, eng.ap, ALU.mult, [kmod, eng], [KTt])
                    P.tt("pool", AR[:, :, 1, :], v4(r32), v4(eg), ALU.mult, [r32, eg], [AR])
                    P.stt("dve", prodk.ap, r32.ap, colp[:, CP_RK + j:CP_RK + j + 1], kmod.ap, ALU.mult, ALU.mult,
                          [r32, colp, kmod], [prodk])
                    for tl in range(4):
                        yield
                        P.sec = "r_transp"
                        ts_ = slice(tl * 128, (tl + 1) * 128)
                        ps = P.psum()
                        P.mm(ps[:, 0:128], BT[:, ts_], identb, True, True, [BT, cstb], [ps])
                        P.mm(ps[:, 128:256], KTt[:, ts_], identb, True, True, [KTt, cstb], [ps])
                        P.mm(ps[:, 256:384], vb[:, ts_], identb, True, True, [vb, cstb], [ps])
                        P.cp("act", TM[:, tl, :], ps[:, 0:384], [ps], [TM])
                    for tl in range(4):
                        ts_ = slice(tl * 128, (tl + 1) * 128)
                        yield
                        P.sec = "r_masks"
                        hss = [slice(0, 64), slice(64, 128)]
                        psXs = [P.psum(), P.psum()]
                        for h in range(2):
                            P.mm(psXs[h][:, 0:256], BT[hss[h], ts_], ARf[hss[h], tl, :], True, True, [BT, AR], [psXs[h]])
                        psYs = [P.psum(), P.psum()]
                        for h in range(2):
                            P.mm(psYs[h][:, 0:256], KTt[hss[h], ts_], ARf[hss[h], tl, :], True, True, [KTt, AR], [psYs[h]])
                        psZs = [P.psum(), P.psum()]
                        for h in range(2):
                            P.mm(psZs[h][:, 0:128], AR[hss[h], tl, 0, :], BT[hss[h], ts_], True, True, [AR, BT], [psZs[h]])
                        for h in range(2):
                            P.tt("dve", NRs[tl][:, h, :, :].rearrange("p a b -> p (a b)"), psXs[h][:, 0:256],
                                 cstb[:, C_US:C_US + 256], ALU.mult, [psXs[h], cstb], [NRs[tl]])
                            P.tt("dve", AKs[tl][:, h, :, :].rearrange("p a b -> p (a b)"), psYs[h][:, 0:256],
                                 cstb[:, C_US:C_US + 256], ALU.mult, [psYs[h], cstb], [AKs[tl]])
                            P.tt("dve", LLs[tl][:, h, :], psZs[h][:, 0:128], cstb[:, C_LS:C_LS + 128], ALU.mult,
                                 [psZs[h], cstb], [LLs[tl]])
                    yield
                    P.sec = "r_inv"
                    Wts = []
                    Pcs, Qcs, Pbs = [], [], []
                    for tl in range(4):
                        P.tt("pool", WtAs[tl].ap, NRs[tl][:, :, 0, :], idb2, ALU.add, [NRs[tl], cstb], [WtAs[tl]])
                        Wts.append([WtAs[tl], WtBs[tl]])
                        Pcs.append([NRs[tl][:, 0, 0, :], NRs[tl][:, 1, 0, :]])
                        Qcs.append([LLs[tl][:, 0, :], LLs[tl][:, 1, :]])
                        Pbs.append([NRs[tl], LLs[tl]])
                    for lvl in range(1, 7):
                        for tl in range(4):
                            yield
                            P.sec = "r_inv"
                            ps = P.psum()
                            for h in range(2):
                                P.mm(ps[:, 128 * h:128 * h + 128], Qcs[tl][h], Pcs[tl][h], True, True, Pbs[tl], [ps])
                            for h in range(2):
                                P.mm(ps[:, 256 + 128 * h:256 + 128 * h + 128], Pcs[tl][h], Qcs[tl][h], True, True, Pbs[tl], [ps])
                            pq = PQs[tl][lvl % 2]
                            P.cp("act" if tl % 2 == 0 else "dve", pq.ap, ps.ap, [ps], [pq])
                            Pcs[tl] = [pq[:, 0:128], pq[:, 128:256]]
                            Qcs[tl] = [pq[:, 256:384], pq[:, 384:512]]
                            Pbs[tl] = [pq]
                        for tl in range(4):
                            yield
                            P.sec = "r_inv"
                            Wt, Wo = Wts[tl]
                            pq = PQs[tl][lvl % 2]
                            ps2 = P.psum()
                            for h in range(2):
                                P.mm(ps2[:, 128 * h:128 * h + 128], identb, Wt[:, h, :], True, False, [cstb, Wt], [ps2])
                                P.mm(ps2[:, 128 * h:128 * h + 128], pq[:, 256 + 128 * h:256 + 128 * h + 128], Wt[:, h, :],
                                     False, True, [pq, Wt], [ps2])
                            P.cp("dve" if tl % 2 == 0 else "act", Wo.ap, ps2[:, 0:256].rearrange("p (h c) -> p h c", h=2), [ps2], [Wo])
                            Wts[tl] = [Wo, Wt]
                    WtFinal[(tb, j)] = [w[0] for w in Wts]
                    yield

                def genB(tb, j):
                    par = (tb * 4 + j) % 2
                    sxg = sxgs[tb % 2]
                    orblk = orblks[tb % 2]
                    AR, TM, eg, prodk = ARs[par], TMs[par], egs[par], prodks[par]
                    NRs, AKs = NRss[par], AKss[par]
                    js = slice(j * 128, (j + 1) * 128)
                    for tl in range(4):
                        ts_ = slice(tl * 128, (tl + 1) * 128)
                        Wt = WtFinal[(tb, j)][tl]
                        NR, AK = NRs[tl], AKs[tl]
                        y32, ysq, yn, otok = y32s[tl % 2], ysqs[tl % 2], yns[tl % 2], otoks[tl % 2]
                        ysum, yss, mean, msq, var, rsd, sb = stats[tl % 2]
                        for h in range(2):
                            P.sec = "r_chain"
                            hs = slice(64 * h, 64 * h + 64)
                            vs = slice(64 * h, 64 * h + 64)
                            psA = P.psum()
                            P.mm(psA[:, 0:64], AR[hs, tl, 0, :], Tb[j][hs, vs], True, False, [AR, Tb[j]], [psA])
                            P.mm(psA[:, 0:64], AK[:, h, 0, :], TM[:, tl, 256 + 64 * h:256 + 64 * h + 64], False, True,
                                 [AK, TM], [psA])
                            P.cp("act" if h == 0 else "dve", Xb[:, vs], psA[:, 0:64], [psA], [Xb])
                        yield
                        P.sec = "r_chain"
                        psB = P.psum()
                        for h in range(2):
                            vs = slice(64 * h, 64 * h + 64)
                            P.mm(psB[:, vs], Wt[:, h, :], Xb[:, vs], True, True, [Wt, Xb], [psB])
                        P.cp("dve", Ub.ap, psB[:, 0:128], [psB], [Ub])
                        yield
                        P.sec = "r_chain"
                        psD = P.psum()
                        P.mm(psD[:, 0:128], TM[:, tl, 0:128], Ub.ap, True, False, [TM, Ub], [psD])
                        P.mm(psD[:, 0:128], TM[:, tl, 128:256], TM[:, tl, 256:384], False, True, [TM], [psD])
                        for h in range(2):
                            hs = slice(64 * h, 64 * h + 64)
                            vs = slice(64 * h, 64 * h + 64)
                            psC = P.psum()
                            P.mm(psC[:, 0:64], AR[hs, tl, 1, :], Tb[j][hs, vs], True, False, [AR, Tb[j]], [psC])
                            P.mm(psC[:, 0:64], NR[:, h, 1, :], Ub[:, vs], False, False, [NR, Ub], [psC])
                            P.mm(psC[:, 0:64], AK[:, h, 1, :], TM[:, tl, 256 + 64 * h:256 + 64 * h + 64], False, True,
                                 [AK, TM], [psC])
                            P.cp("act", y32[:, vs], psC[:, 0:64], [psC], [y32])
                        P.tt("dve", T32[j].ap, psD[:, 0:128], T32[j].ap, ALU.add, [psD, T32[j]], [T32[j]])
                        P.ts("dve", T32[j].ap, T32[j].ap, eg[:, tl * 128 + 127:tl * 128 + 128], None, ALU.mult, None,
                             [T32[j], eg], [T32[j]])
                        P.cp("act", Tb[j].ap, T32[j].ap, [T32[j]], [Tb[j]])
                        yield
                        P.sec = "r_post"
                        y3 = lambda t: t.ap.rearrange("p (h c) -> p h c", h=2)
                        P.red(ysum.ap, y3(y32), ALU.add, [y32], [ysum])
                        P.tt("pool", ysq.ap, y32.ap, y32.ap, ALU.mult, [y32], [ysq])
                        P.red(yss.ap, y3(ysq), ALU.add, [ysq], [yss])
                        P.ts("pool", mean.ap, ysum.ap, 1.0 / 64, None, ALU.mult, None, [ysum], [mean])
                        P.tt("pool", msq.ap, mean.ap, mean.ap, ALU.mult, [mean], [msq])
                        yield
                        P.sec = "r_post"
                        P.stt("dve", var.ap, yss.ap, 1.0 / 64, msq.ap, ALU.mult, ALU.subtract, [yss, msq], [var])
                        P.rstd(rsd.ap, var.ap, 1.0, 64e-5, [var], [rsd])
                        yield
                        P.sec = "r_post"
                        for h in range(2):
                            vs = slice(64 * h, 64 * h + 64)
                            P.ts("pool", yn[:, vs], y32[:, vs], mean[:, h:h + 1], rsd[:, h:h + 1], ALU.subtract, ALU.mult,
                                 [y32, mean, rsd], [yn])
                        P.tt("pool", yn.ap, yn.ap, rowb[:, RB_LNW + j * 128:RB_LNW + (j + 1) * 128], ALU.mult, [yn, rowb], [yn])
                        P.tt("pool", yn.ap, yn.ap, rowb[:, RB_LNB + j * 128:RB_LNB + (j + 1) * 128], ALU.add, [yn, rowb], [yn])
                        psS = P.psum()
                        P.mm(psS[:, 0:2], prodk[:, ts_], cstb[:, C_HS:C_HS + 2], True, True, [prodk, cstb], [psS])
                        P.cp("act", sb.ap, psS[:, 0:2], [psS], [sb])
                        yield
                        P.sec = "r_post"
                        for h in range(2):
                            vs = slice(64 * h, 64 * h + 64)
                            P.stt("dve", yn[:, vs], TM[:, tl, 256 + 64 * h:256 + 64 * h + 64], sb[:, h:h + 1], yn[:, vs],
                                  ALU.mult, ALU.add, [TM, sb, yn], [yn])
                        psT = P.psum()
                        P.mm(psT[:, 0:128], sxg[:, ts_], g2b[:, js], True, True, [sxg, g2b], [psT])
                        P.tt("dve", otok.ap, yn.ap, psT[:, 0:128], ALU.mult, [yn, psT], [otok])
                        yield
                        P.sec = "r_post"
                        psO = P.psum()
                        P.mm(psO[:, 0:128], otok.ap, identb, True, True, [otok, cstb], [psO])
                        P.cp("act", orblk[:, j, ts_], psO[:, 0:128], [psO], [orblk])
                        yield
                    if j == 3:
                        P.dma("sp", ORT[:, :, tb * 512:(tb + 1) * 512].rearrange("k p t -> p k t"), orblk.ap, [orblk], [])

                def chain(*gens):
                    for g in gens:
                        yield from g

                def interleave(ga, gb, ra=1, rb=1):
                    alive_a, alive_b = True, True
                    while alive_a or alive_b:
                        for _ in range(ra):
                            if alive_a:
                                try:
                                    next(ga)
                                except StopIteration:
                                    alive_a = False
                        for _ in range(rb):
                            if alive_b:
                                try:
                                    next(gb)
                                except StopIteration:
                                    alive_b = False
                units = [(tb, j) for tb in range(NTB) for j in range(4)]

                def genAfull(u):
                    tb, j = units[u]
                    if j == 0:
                        return chain(genS(tb), genA(tb, j))
                    return genA(tb, j)
                for _ in genAfull(0):
                    pass
                for u in range(len(units)):
                    gb = genB(*units[u])
                    if flags.get("seqB"):
                        for _ in gb:
                            pass
                        if u + 1 < len(units):
                            for _ in genAfull(u + 1):
                                pass
                    elif u + 1 < len(units):
                        interleave(genAfull(u + 1), gb, flags.get("ra", 1), flags.get("rb", 1))
                    else:
                        for _ in gb:
                            pass
                P.barrier()
                P.off = mark1
            else:
                z = P.alloc([4, S], BF16)
                P.memset("pool", z.ap, 0.0, [z])
                for j in range(4):
                    P.dma("sp", ORT[j], z[:, j, :], [z], [])
                P.barrier()
                P.off = mark1
            if flags.get("attn", True):
                Wq = P.alloc([8, 512], BF16)
                ropeb = P.alloc([2 * S], BF16)
                for kc in range(8):
                    rws = slice(kc * 128, (kc + 1) * 128)
                    wload(Wq[:, kc, :], w_in[rws, 1792:2304], [], [Wq])
                for q4 in range(4):
                    wload(ropeb[:, q4 * 2048:(q4 + 1) * 2048], rope_d[:, q4 * 2048:(q4 + 1) * 2048], [], [ropeb])
                cosb = ropeb[:, 0:S]
                sinb = ropeb[:, S:2 * S]
                KT = P.alloc([4, S], BF16)
                Vx = P.alloc([NT, 4, 130], BF16)
                P.memset("pool", Vx[:, :, :, 128:130], 1.0, [Vx])
                lt = P.alloc([64])
                ls = P.alloc([2])
                lam = P.alloc([1])
                nlam = P.alloc([1])
                for k in range(2):
                    P.tt("dve", lt.ap, rowb[:, RB_LAM + 128 * k:RB_LAM + 128 * k + 64],
                         rowb[:, RB_LAM + 128 * k + 64:RB_LAM + 128 * k + 128], ALU.mult, [rowb], [lt])
                    P.red(ls[:, k:k + 1], lt.ap, ALU.add, [lt], [ls])
                P.act(ls.ap, ls.ap, AF.Exp, [ls], [ls])
                P.tt("dve", lam.ap, ls[:, 0:1], ls[:, 1:2], ALU.subtract, [ls], [lam])
                P.ts("dve", nlam.ap, lam.ap, 0.2, -1.0, ALU.add, ALU.mult, [lam], [nlam])
                qsets = [(P.alloc([512]), P.alloc([512], BF16), P.alloc([512]), P.alloc([512], BF16), P.alloc([512]),
                          P.alloc([512])) for _ in range(3)]
                QT = P.alloc([512], BF16)
                mkW = P.off
                Wk = P.alloc([8, 512], BF16)
                Wv = P.alloc([8, 512], BF16)
                for kc in range(8):
                    rws = slice(kc * 128, (kc + 1) * 128)
                    wload(Wk[:, kc, :], w_in[rws, 2304:2816], [], [Wk])
                    wload(Wv[:, kc, :], w_in[rws, 2816:3328], [], [Wv])

                hld = {}

                def hload(blk):
                    if hld.get(blk % 2) != blk:
                        P.dma("sp", hblk[blk % 2].ap, HTD[:, :, blk * 512:(blk + 1) * 512], [HTDt], [hblk[blk % 2]])
                        hld[blk % 2] = blk
                    return hblk[blk % 2]

                def qk_gen(W, gcol, blk, h, dst, dstbuf, k):
                    raw, sqb, rsb, knb, ta, tb_ = qsets[k]
                    P.sec = "a_prep"
                    tsl = slice(blk * 512, (blk + 1) * 512)
                    hc = hload(blk)
                    ps = P.psum()
                    for kc in range(8):
                        P.mm(ps.ap, W[:, kc, h * 128:(h + 1) * 128], hc[:, kc, :], kc == 0, kc == 7, [W, hc], [ps])
                    P.cp("act", raw.ap, ps.ap, [ps], [raw])
                    yield
                    P.sec = "a_prep"
                    P.tt("pool", sqb.ap, raw.ap, raw.ap, ALU.mult, [raw], [sqb])
                    yield
                    P.sec = "a_prep"
                    ps2 = P.psum()
                    P.mm(ps2.ap, cstb[:, C_BO:C_BO + 128], sqb.ap, True, True, [cstb, sqb], [ps2])
                    P.rstd(rsb.ap, ps2.ap, 1.0 / 64, 1e-6, [ps2], [rsb])
                    yield
                    P.sec = "a_prep"
                    P.stt("dve", knb.ap, raw.ap, colp[:, gcol:gcol + 1], rsb.ap, ALU.mult, ALU.mult, [raw, colp, rsb], [knb])
                    yield
                    P.sec = "a_prep"
                    ps3 = P.psum()
                    P.mm(ps3.ap, cstb[:, C_RM:C_RM + 128], knb.ap, True, True, [cstb, knb], [ps3])
                    P.tt("dve", tb_.ap, ps3.ap, sinb[:, tsl], ALU.mult, [ps3, ropeb], [tb_])
                    yield
                    P.sec = "a_prep"
                    P.tt("pool", ta.ap, knb.ap, cosb[:, tsl], ALU.mult, [knb, ropeb], [ta])
                    yield
                    P.sec = "a_prep"
                    P.tt("pool", dst, ta.ap, tb_.ap, ALU.add, [ta, tb_], [dstbuf])
                    yield

                def qk_prep(W, gcol, blk, h, dst, dstbuf):
                    for _ in qk_gen(W, gcol, blk, h, dst, dstbuf, 0):
                        pass

                def v_gen(blk):
                    hc = hload(blk)
                    for i in range(blk * 4, blk * 4 + 4):
                        P.sec = "a_v"
                        ps = P.psum()
                        for kc in range(8):
                            P.mm(ps.ap, hc[:, kc, (i % 4) * 128:(i % 4 + 1) * 128], Wv[:, kc, :], kc == 0, kc == 7, [hc, Wv], [ps])
                        P.cp("act" if i % 2 else "dve", Vx[:, i, :, 0:128], ps.ap.rearrange("p (h c) -> p h c", h=4), [ps], [Vx])
                        yield

                def run_rr(gen_list, width):
                    active = []
                    it = iter(gen_list)
                    done = False
                    while True:
                        while len(active) < width and not done:
                            try:
                                active.append(next(it))
                            except StopIteration:
                                done = True
                        if not active:
                            break
                        for g in list(active):
                            try:
                                next(g)
                            except StopIteration:
                                active.remove(g)
                KTs = [T(KT.ap) for _ in range(4)]
                kgens = []
                cnt_ = 0
                for blk in range(8):
                    for h in range(4):
                        kgens.append((Wk, CP_KN, blk, h, KT[:, h, blk * 512:(blk + 1) * 512], KTs[h], cnt_ % 3))
                        cnt_ += 1
                    kgens.append(("v", blk))

                def mk(g):
                    if g[0] == "v":
                        return v_gen(g[1])
                    return qk_gen(*g)
                run_rr((mk(g) for g in kgens), 3)
                P.barrier()
                P.off = mkW
                P.ps_pool = [0, 1, 2, 3, 4]
                P.ps_rr = 0
                Ob = [P.ps_tiles[5], P.ps_tiles[6], P.ps_tiles[7]]

                def Oacc(c, qt):
                    n = c * 4 + qt
                    return Ob[n // 3], slice((n % 3) * 130, (n % 3) * 130 + 129)
                ptl = [P.alloc([512], BF16) for _ in range(6)]
                QT2 = P.alloc([512], BF16)
                OT = [P.ps_tiles[5], P.ps_tiles[6]]
                P.ps_pool = [0, 1, 2, 3]
                Zb = [P.ps_tiles[4], P.ps_tiles[7]]
                Zacc = [[P.alloc([512]) for _ in range(2)] for _ in range(2)]
                zhi = P.alloc([512], BF16)
                zlo = P.alloc([512], BF16)
                rzs = [P.alloc([512]) for _ in range(2)]
                tAs = [P.alloc([512]) for _ in range(2)]
                o32 = P.alloc([512])
                sqb_ = P.alloc([512], BF16)
                rsq = P.alloc([512])
                odblk = P.alloc([4, 512], BF16)
                QTs = [QT, QT2]
                LOOK = flags.get("look", 4)
                NQB = flags.get("nqb", 8)
                heads = [(qb, h) for qb in range(NQB) for h in range(4)]
                ptc = [0]
                zb = cstb[:, C_LS:C_LS + 128]

                def clear_banks():
                    for ob_ in Ob:
                        P.mm(ob_[:, 400:402], zb, cstb[:, C_HS:C_HS + 2], True, True, [cstb], [ob_])

                class Ctx:
                    pass

                def make(n):
                    c = Ctx()
                    c.qb, c.h = heads[n]
                    c.n = n
                    c.QT = QTs[n % 2]
                    c.items = [(kt, cc) for kt in range(4 * c.qb + 4) for cc in range(2)]
                    c.pts = {}
                    return c

                def stage1(c, i):
                    P.sec = "a_s1"
                    kt, cc = c.items[i]
                    r_ = max(kt - 4 * c.qb, 0)
                    c0 = r_ * 128
                    N = 512 - c0
                    cs = slice(64 * cc, 64 * cc + 64)
                    ps = P.psum()
                    P.mm(ps[:, 0:N], KT[cs, c.h, kt * 128:(kt + 1) * 128], c.QT[cs, c0:512], True, True, [KT, c.QT], [ps])
                    pt = ptl[ptc[0] % 6]
                    ptc[0] += 1
                    c.pts[i] = pt
                    P.act(pt[:, 0:N], ps[:, 0:N], AF.Exp, [ps], [pt], scale=0.125)
                    if kt >= 4 * c.qb:
                        P.tt("pool", pt[:, 0:128], pt[:, 0:128], cstb[:, C_UI:C_UI + 128], ALU.mult, [pt, cstb], [pt])

                def stage2(c, i):
                    P.sec = "a_s2"
                    kt, cc = c.items[i]
                    r_ = max(kt - 4 * c.qb, 0)
                    c0 = r_ * 128
                    N = 512 - c0
                    pt = c.pts.pop(i)
                    za = Zacc[c.n % 2][cc]
                    P.mm(OT[cc][:, c0:512], Vx[:, kt, c.h, 0:128], pt[:, 0:N], kt == 0, kt == 4 * c.qb + 3, [Vx, pt], [OT[cc]])
                    P.mm(Zb[cc][:, c0:512], cstb[:, C_ONE:C_ONE + 128], pt[:, 0:N], kt == 0, kt == 4 * c.qb + 3, [cstb, pt], [Zb[cc]])
                qk_prep(Wq, CP_QN, 0, 0, QTs[0].ap, QTs[0])
                cur = make(0)
                for i in range(min(LOOK, len(cur.items))):
                    stage1(cur, i)
                for n in range(len(heads)):
                    c = cur
                    qb, h = c.qb, c.h
                    tsl = slice(qb * 512, (qb + 1) * 512)
                    qg = None
                    if n + 1 < len(heads):
                        nqb, nh_ = heads[n + 1]
                        qg = qk_gen(Wq, CP_QN, nqb, nh_, QTs[(n + 1) % 2].ap, QTs[(n + 1) % 2], n % 3)
                    if flags.get("pair", 1):
                        for i in range(0, len(c.items), 2):
                            for d_ in (0, 1):
                                if i + d_ + LOOK < len(c.items):
                                    stage1(c, i + d_ + LOOK)
                            for d_ in (0, 1):
                                stage2(c, i + d_)
                                if qg is not None:
                                    try:
                                        next(qg)
                                    except StopIteration:
                                        qg = None
                    else:
                        for i in range(len(c.items)):
                            if i + LOOK < len(c.items):
                                stage1(c, i + LOOK)
                            stage2(c, i)
                            if qg is not None:
                                try:
                                    next(qg)
                                except StopIteration:
                                    qg = None
                    if qg is not None:
                        for _ in qg:
                            pass
                    P.sec = "a_tail"
                    onesb = cstb[:, C_ONE:C_ONE + 128]
                    for cc in range(2):
                        P.recip(rzs[cc].ap, Zb[cc].ap, [Zb[cc]], [rzs[cc]])
                        P.tt("dve", tAs[cc].ap, OT[cc].ap, rzs[cc].ap, ALU.mult, [OT[cc], rzs[cc]], [tAs[cc]])
                    if n + 1 < len(heads):
                        cur = make(n + 1)
                        for i in range(min(LOOK, len(cur.items))):
                            stage1(cur, i)
                    P.sec = "a_tail"
                    P.stt("dve", o32.ap, tAs[1].ap, nlam[:, 0:1], tAs[0].ap, ALU.mult, ALU.add, [tAs[1], nlam, tAs[0]], [o32])
                    P.tt("pool", sqb_.ap, o32.ap, o32.ap, ALU.mult, [o32], [sqb_])
                    pss = P.psum()
                    P.mm(pss.ap, onesb, sqb_.ap, True, True, [cstb, sqb_], [pss])
                    P.rstd(rsq.ap, pss.ap, 1.0 / 128, 1e-6, [pss], [rsq])
                    P.tt("dve", o32.ap, o32.ap, rsq.ap, ALU.mult, [o32, rsq], [o32])
                    P.ts("dve", odblk[:, h, :], o32.ap, colp[:, CP_SUBLN:CP_SUBLN + 1], 0.8, ALU.mult, ALU.mult, [o32, colp], [odblk])
                    if h == 3:
                        P.dma("sp", ODT[:, :, tsl].rearrange("k p t -> p k t"), odblk.ap, [odblk], [])
                P.ps_pool = list(range(8))
                P.ps_rr = 0
                P.barrier()
                P.off = mark1
            else:
                z = P.alloc([4, S], BF16)
                P.memset("pool", z.ap, 0.0, [z])
                for j in range(4):
                    P.dma("sp", ODT[j], z[:, j, :], [z], [])
                P.barrier()
                P.off = mark1
        else:
            z = P.alloc([4, S], BF16)
            P.memset("pool", z.ap, 0.0, [z])
            for j in range(4):
                P.dma("sp", ORT[j], z[:, j, :], [z], [])
                P.dma("sp", ODT[j], z[:, j, :], [z], [])
            P.barrier()
            P.off = mark1

        if flags.get("stop") == 2:
            P.emit(es)
            return nc
        Wg = P.alloc([8, 2048], BF16)
        Wbr = P.alloc([4, D], BF16)
        Wbd = P.alloc([4, D], BF16)
        Wout = P.alloc([8, D], BF16)
        Wr = P.alloc([8, 36])
        for kc in range(8):
            wload(Wg[:, kc, :], w_in[kc * 128:(kc + 1) * 128, 3328:5376], [], [Wg])
            wload(Wout[:, kc, :], wout_d[kc * 128:(kc + 1) * 128, :], [], [Wout])
            P.dma("sp", Wr[:, kc, :], wr_d[kc * 128:(kc + 1) * 128, :], [], [Wr])
        for kc in range(4):
            wload(Wbr[:, kc, :], wbr_d[kc * 128:(kc + 1) * 128, :], [], [Wbr])
            wload(Wbd[:, kc, :], wbd_d[kc * 128:(kc + 1) * 128, :], [], [Wbd])
        orb = [P.alloc([4, 512], BF16) for _ in range(2)]
        odb = [P.alloc([4, 512], BF16) for _ in range(2)]
        sgr = [P.alloc([512]) for _ in range(2)]
        sgd = [P.alloc([512]) for _ in range(2)]
        t1 = [P.alloc([512]) for _ in range(2)]
        t2 = [P.alloc([512]) for _ in range(2)]
        mixT = [P.alloc([8, 512], BF16) for _ in range(2)]
        NS4 = 2
        xt = [P.alloc([D]) for _ in range(NS4)]
        x2 = [P.alloc([D]) for _ in range(NS4)]
        h2 = [P.alloc([D]) for _ in range(NS4)]
        junks = [P.alloc([D], BF16) for _ in range(NS4)]
        ssq2 = [P.alloc([1]) for _ in range(NS4)]
        rs2 = [P.alloc([1]) for _ in range(NS4)]
        h2his = [P.alloc([D], BF16) for _ in range(NS4)]
        h2los = [P.alloc([D], BF16) for _ in range(NS4)]
        h2Tls = [P.alloc([8, 128], BF16) for _ in range(NS4)]
        h2Tbs = [P.alloc([8, 128], BF16) for _ in range(NS4)]
        Wrh = P.alloc([8, 36], BF16)
        Wrl = P.alloc([8, 36], BF16)
        P.cp("dve", Wrh.ap, Wr.ap, [Wr], [Wrh])
        P.tt("dve", Wrl.ap, Wr.ap, Wrh.ap, ALU.subtract, [Wr, Wrh], [Wrl])
        LGs = [T(LG.ap) for _ in range(NT)]

        def genM(tb):
            ob, db, mx = orb[tb % 2], odb[tb % 2], mixT[tb % 2]
            tsl = slice(tb * 512, (tb + 1) * 512)
            hc = hblk[tb % 2]
            P.dma("sp", hc.ap, HTD[:, :, tsl], [HTDt], [hc])
            P.dma("sp", ob.ap, ORT[:, :, tsl].rearrange("k p t -> p k t"), [], [ob])
            P.dma("sp", db.ap, ODT[:, :, tsl].rearrange("k p t -> p k t"), [], [db])
            yield
            for m in range(8):
                q = m % 2
                ps = P.psum()
                for kc in range(8):
                    P.mm(ps.ap, Wg[:, kc, m * 128:(m + 1) * 128], hc[:, kc, :], kc == 0, kc == 7, [Wg, hc], [ps])
                P.act(sgr[q].ap, ps.ap, AF.Sigmoid, [ps], [sgr[q]])
                ps = P.psum()
                for kc in range(8):
                    P.mm(ps.ap, Wg[:, kc, 1024 + m * 128:1024 + (m + 1) * 128], hc[:, kc, :], kc == 0, kc == 7,
                         [Wg, hc], [ps])
                P.act(sgd[q].ap, ps.ap, AF.Sigmoid, [ps], [sgd[q]])
                yield
                ps = P.psum()
                for kc in range(4):
                    P.mm(ps.ap, Wbr[:, kc, m * 128:(m + 1) * 128], ob[:, kc, :], kc == 0, kc == 3, [Wbr, ob], [ps])
                P.tt("dve", t1[q].ap, ps.ap, sgr[q].ap, ALU.mult, [ps, sgr[q]], [t1[q]])
                ps = P.psum()
                for kc in range(4):
                    P.mm(ps.ap, Wbd[:, kc, m * 128:(m + 1) * 128], db[:, kc, :], kc == 0, kc == 3, [Wbd, db], [ps])
                P.tt("dve", t2[q].ap, ps.ap, sgd[q].ap, ALU.mult, [ps, sgd[q]], [t2[q]])
                P.tt("pool", mx[:, m, :], t1[q].ap, t2[q].ap, ALU.add, [t1[q], t2[q]], [mx])
                yield

        def genT(i):
            tb, tt_ = divmod(i, 4)
            mx = mixT[tb % 2]
            k = i % NS4
            rows = slice(i * 128, (i + 1) * 128)
            xt_, x2_, h2_, junk_, ssq_, rs_, hi_, lo_, Tl_, Tb_ = (xt[k], x2[k], h2[k], junks[k], ssq2[k], rs2[k], h2his[k],
                                                                  h2los[k], h2Tls[k], h2Tbs[k])
            P.dma("sp", xt_.ap, x_d[rows, :], [], [xt_])
            for hc_ in range(2):
                ps = P.psum()
                for m in range(8):
                    P.mm(ps.ap, mx[:, m, tt_ * 128:(tt_ + 1) * 128], Wout[:, m, hc_ * 512:(hc_ + 1) * 512],
                         m == 0, m == 7, [mx, Wout], [ps])
                P.tt("dve", x2_[:, hc_ * 512:(hc_ + 1) * 512], ps.ap, xt_[:, hc_ * 512:(hc_ + 1) * 512], ALU.add,
                     [ps, xt_], [x2_])
            P.dma("sp", X2[rows, :], x2_.ap, [x2_], [])
            yield
            P.act(junk_.ap, x2_.ap, AF.Square, [x2_], [junk_, ssq_], accum_out=ssq_.ap)
            yield
            P.rstd(rs_.ap, ssq_.ap, 1.0 / D, 1e-6, [ssq_], [rs_])
            yield
            P.act(h2_.ap, x2_.ap, AF.Copy, [x2_, rs_], [h2_], scale=rs_[:, 0:1])
            yield
            P.tt("pool", h2_.ap, h2_.ap, rowb[:, RB_GFFN:RB_GFFN + D], ALU.mult, [h2_, rowb], [h2_])
            yield
            P.cp("act", hi_.ap, h2_.ap, [h2_], [hi_])
            yield
            P.tt("pool", lo_.ap, h2_.ap, hi_.ap, ALU.subtract, [h2_, hi_], [lo_])
            yield
            for src, dst in ((hi_, Tb_), (lo_, Tl_)):
                for hf in range(2):
                    ps = P.psum()
                    for kk in range(4):
                        kc = hf * 4 + kk
                        P.mm(ps[:, kk * 128:(kk + 1) * 128], src[:, kc * 128:(kc + 1) * 128], identb, True, True,
                             [src, cstb], [ps])
                    P.cp("act" if hf == 0 else "dve", dst[:, hf * 4:hf * 4 + 4, :],
                         ps.ap.rearrange("p (a b) -> p a b", a=4), [ps], [dst])
                    yield
            P.dma("sp", H2T[:, :, rows].rearrange("k p t -> p k t"), Tb_.ap, [Tb_], [])
            ps = P.psum()
            n = 0
            for kc in range(8):
                for (a_, b_) in ((Tb_, Wrh), (Tb_, Wrl), (Tl_, Wrh)):
                    P.mm(ps[:, 0:36], a_[:, kc, :], b_[:, kc, :], n == 0, n == 23, [a_, b_], [ps])
                    n += 1
            P.tt("dve", LG[:, i, :], ps[:, 0:36], rowb[:, RB_RBIAS:RB_RBIAS + 36], ALU.add, [ps, rowb], [LGs[i]])
            yield

        for _ in genM(0):
            pass
        for tb in range(8):
            grp = [genM(tb + 1)] if tb + 1 < 8 else []
            grp += [genT(tb * 4 + k) for k in range(4)]
            run_rr4(iter(grp), 3 if tb + 1 < 8 else 2)
        P.barrier()
        P.off = base_off

        if flags.get("stop") == 3:
            P.emit(es)
            return nc
        mark2 = P.off
        glog = LG[:, :, 0:4]
        elog = LG[:, :, 4:36].rearrange("p t (g j) -> p t g j", g=4)
        gmax = P.alloc([NT])
        gsh = P.alloc([NT, 4])
        gex = P.alloc([NT, 4])
        gsum = P.alloc([NT])
        gtop = P.alloc([NT])
        gmask = P.alloc([NT, 4])
        tmp4 = P.alloc([NT, 4, 8])
        esel = P.alloc([NT, 8])
        esel2 = P.alloc([NT, 8])
        m1 = P.alloc([NT])
        m2 = P.alloc([NT])
        mk1 = P.alloc([NT, 8])
        mk2 = P.alloc([NT, 8])
        dm = P.alloc([NT])
        w1 = P.alloc([NT])
        w2 = P.alloc([NT])
        wsel = P.alloc([NT, 8])
        wsel2 = P.alloc([NT, 8])
        P.red(gmax.ap, glog, ALU.max, [LG], [gmax])
        P.tt("dve", gsh.ap, glog, bc(gmax.ap.unsqueeze(2), [128, NT, 4]), ALU.subtract, [LG, gmax], [gsh])
        P.act(gex.ap, gsh.ap, AF.Exp, [gsh], [gex])
        P.red(gsum.ap, gex.ap, ALU.add, [gex], [gsum])
        P.recip(gtop.ap, gsum.ap, [gsum], [gtop])
        P.ts("dve", gmask.ap, gsh.ap, 0.0, None, ALU.is_equal, None, [gsh], [gmask])
        P.tt("dve", tmp4.ap, elog, bc(gmask.ap.unsqueeze(3), [128, NT, 4, 8]), ALU.mult, [LG, gmask], [tmp4])
        P.red(esel.ap, tmp4.ap.rearrange("p t g j -> p t j g"), ALU.add, [tmp4], [esel])
        P.red(m1.ap, esel.ap, ALU.max, [esel], [m1])
        P.tt("dve", mk1.ap, esel.ap, bc(m1.ap.unsqueeze(2), [128, NT, 8]), ALU.is_equal, [esel, m1], [mk1])
        P.stt("dve", esel2.ap, mk1.ap, -1e30, esel.ap, ALU.mult, ALU.add, [mk1, esel], [esel2])
        P.red(m2.ap, esel2.ap, ALU.max, [esel2], [m2])
        P.tt("dve", mk2.ap, esel2.ap, bc(m2.ap.unsqueeze(2), [128, NT, 8]), ALU.is_equal, [esel2, m2], [mk2])
        P.tt("dve", dm.ap, m1.ap, m2.ap, ALU.subtract, [m1, m2], [dm])
        P.act(w1.ap, dm.ap, AF.Sigmoid, [dm], [w1])
        P.tt("dve", w1.ap, w1.ap, gtop.ap, ALU.mult, [w1, gtop], [w1])
        P.tt("dve", w2.ap, gtop.ap, w1.ap, ALU.subtract, [gtop, w1], [w2])
        P.tt("dve", wsel.ap, mk1.ap, bc(w1.ap.unsqueeze(2), [128, NT, 8]), ALU.mult, [mk1, w1], [wsel])
        P.tt("dve", wsel2.ap, mk2.ap, bc(w2.ap.unsqueeze(2), [128, NT, 8]), ALU.mult, [mk2, w2], [wsel2])
        P.tt("dve", wsel.ap, wsel.ap, wsel2.ap, ALU.add, [wsel, wsel2], [wsel])
        P.tt("dve", WT.ap.rearrange("p t (g j) -> p t g j", g=4), bc(gmask.ap.unsqueeze(3), [128, NT, 4, 8]),
             bc(wsel.ap.unsqueeze(2), [128, NT, 4, 8]), ALU.mult, [gmask, wsel], [WT])
        P.barrier()
        P.off = mark2

        if flags.get("stop") == 4:
            P.emit(es)
            return nc
        acc = [P.alloc([D]) for _ in range(16)]
        h2h = P.alloc([8, 2048], BF16)
        Wgu = [P.alloc([8, 1024], BF16) for _ in range(2)]
        Wd = [P.alloc([4, D], BF16) for _ in range(2)]
        sg = [P.alloc([512]) for _ in range(2)]
        actT = [P.alloc([4, 512], BF16) for _ in range(2)]
        ssq3s = [P.alloc([1]) for _ in range(2)]
        rs3s = [P.alloc([1]) for _ in range(2)]
        xn3 = [P.alloc([D], BF16) for _ in range(2)]
        x3T = [P.alloc([8, 128], BF16) for _ in range(2)]
        pt_ = [P.alloc([256]) for _ in range(2)]
        ptb = [P.alloc([256], BF16) for _ in range(2)]
        pT = [P.alloc([2, 128], BF16) for _ in range(2)]
        sgp = [P.alloc([D])] * 2
        yo = [P.alloc([D]) for _ in range(2)]
        NE = flags.get("n_experts", 32)
        for half in range(2):
            t0 = half * 2048
            for k in range(16):
                P.dma("sp", acc[k].ap, X2[t0 + k * 128:t0 + (k + 1) * 128, :], [], [acc[k]])
            P.dma("sp", h2h.ap, H2T[:, :, t0:t0 + 2048].rearrange("k p t -> p k t"), [], [h2h])
            def load_expert(e):
                wg_, wd_ = Wgu[e % 2], Wd[e % 2]
                for kc in range(8):
                    wload(wg_[:, kc, 0:512], wg_d[e, kc * 128:(kc + 1) * 128, :], [], [wg_])
                    wload(wg_[:, kc, 512:1024], wu_d[e, kc * 128:(kc + 1) * 128, :], [], [wg_])
                for kc in range(4):
                    wload(wd_[:, kc, :], wd_d[e, kc * 128:(kc + 1) * 128, :], [], [wd_])

            def stageG(u):
                e, tb = divmod(u, 4)
                if tb == 0 and e + 1 < NE:
                    load_expert(e + 1)
                wg_ = Wgu[e % 2]
                at = actT[u % 2]
                tsl = slice(tb * 512, (tb + 1) * 512)
                for fc in range(4):
                    q = fc % 2
                    psg = P.psum()
                    for kc in range(8):
                        P.mm(psg.ap, wg_[:, kc, fc * 128:(fc + 1) * 128], h2h[:, kc, tsl], kc == 0, kc == 7,
                             [wg_, h2h], [psg])
                    psu = P.psum()
                    for kc in range(8):
                        P.mm(psu.ap, wg_[:, kc, 512 + fc * 128:512 + (fc + 1) * 128], h2h[:, kc, tsl], kc == 0,
                             kc == 7, [wg_, h2h], [psu])
                    P.act(sg[q].ap, psg.ap, AF.Silu, [psg], [sg[q]])
                    P.tt("dve", at[:, fc, :], psu.ap, sg[q].ap, ALU.mult, [psu, sg[q]], [at])

            def stageD(u):
                e, tb = divmod(u, 4)
                wd_ = Wd[e % 2]
                at = actT[u % 2]
                for tt_ in range(4):
                    k = tb * 4 + tt_
                    gi = half * 16 + k
                    for hc in range(2):
                        ps = P.psum()
                        for fc in range(4):
                            P.mm(ps.ap, at[:, fc, tt_ * 128:(tt_ + 1) * 128], wd_[:, fc, hc * 512:(hc + 1) * 512],
                                 fc == 0, fc == 3, [at, wd_], [ps])
                        P.stt("dve", acc[k][:, hc * 512:(hc + 1) * 512], ps.ap, WT[:, gi, e:e + 1],
                              acc[k][:, hc * 512:(hc + 1) * 512], ALU.mult, ALU.add, [ps, WT, acc[k]], [acc[k]])
            if NE > 0:
                load_expert(0)
                for u in range(NE * 4):
                    stageG(u)
                    stageD(u)
            Wpg, Wpp = Wgu[0], Wd[0]
            for kc in range(8):
                wload(Wpg[:, kc, :], wpg_d[kc * 128:(kc + 1) * 128, :], [], [Wpg])
            for kc in range(2):
                wload(Wpp[:, kc, :], wpp_d[kc * 128:(kc + 1) * 128, :], [], [Wpp])
            def genP(k):
                gi = half * 16 + k
                q = k % 2
                rows = slice(gi * 128, (gi + 1) * 128)
                a = acc[k]
                P.dma("sp", pt_[q].ap, p_d[rows, :], [], [pt_[q]])
                P.act(yo[q].ap, a.ap, AF.Square, [a], [yo[q], ssq3s[q]], accum_out=ssq3s[q].ap)
                yield
                P.rstd(rs3s[q].ap, ssq3s[q].ap, 1.0 / D, 1e-6, [ssq3s[q]], [rs3s[q]])
                yield
                P.act(xn3[q].ap, a.ap, AF.Copy, [a, rs3s[q]], [xn3[q]], scale=rs3s[q][:, 0:1])
                P.cp("pool", ptb[q].ap, pt_[q].ap, [pt_[q]], [ptb[q]])
                yield
                for hf in range(2):
                    ps = P.psum()
                    for kk in range(4):
                        kc = hf * 4 + kk
                        P.mm(ps[:, kk * 128:(kk + 1) * 128], xn3[q][:, kc * 128:(kc + 1) * 128], identb, True, True,
                             [xn3[q], cstb], [ps])
                    P.tt("dve", x3T[q][:, hf * 4:hf * 4 + 4, :], ps.ap.rearrange("p (a b) -> p a b", a=4),
                         bc(colp[:, CP_GPLE + hf * 4:CP_GPLE + hf * 4 + 4].unsqueeze(2), [128, 4, 128]), ALU.mult,
                         [ps, colp], [x3T[q]])
                    yield
                ps = P.psum()
                for kc in range(2):
                    P.mm(ps[:, kc * 128:(kc + 1) * 128], ptb[q][:, kc * 128:(kc + 1) * 128], identb, True, True,
                         [ptb[q], cstb], [ps])
                P.cp("act", pT[q].ap, ps[:, 0:256].rearrange("p (a b) -> p a b", a=2), [ps], [pT[q]])
                yield
                for hc in range(2):
                    cs = slice(hc * 512, (hc + 1) * 512)
                    ps = P.psum()
                    for kc in range(8):
                        P.mm(ps.ap, x3T[q][:, kc, :], Wpg[:, kc, cs], kc == 0, kc == 7, [x3T[q], Wpg], [ps])
                    P.act(sgp[q][:, cs], ps.ap, AF.Sigmoid, [ps], [sgp[q]])
                    ps = P.psum()
                    for kc in range(2):
                        P.mm(ps.ap, pT[q][:, kc, :], Wpp[:, kc, cs], kc == 0, kc == 1, [pT[q], Wpp], [ps])
                    P.tt("dve", yo[q][:, cs], ps.ap, sgp[q][:, cs], ALU.mult, [ps, sgp[q]], [yo[q]])
                    yield
                P.tt("pool", yo[q].ap, yo[q].ap, a.ap, ALU.add, [yo[q], a], [yo[q]])
                P.dma("sp", y_d[rows, :], yo[q].ap, [yo[q]], [])
                yield
            run_rr4((genP(k) for k in range(16)), 2)
        P.emit(es)
    return nc


def _host_consts():
    c = np.zeros((128, NCST), np.float32)
    i = np.arange(128)
    c[:, C_ID:C_ID + 128] = np.eye(128)
    c[:, C_US:C_US + 128] = (i[:, None] < i[None, :])
    c[:, C_UI:C_UI + 128] = (i[:, None] <= i[None, :])
    c[:, C_LS:C_LS + 128] = (i[:, None] > i[None, :])
    c[:, C_BO:C_BO + 128] = (i[:, None] // 64 == i[None, :] // 64)
    rm = np.zeros((128, 128), np.float32)
    for p in range(128):
        d = p % 64
        if d < 32:
            rm[p + 32, p] = -1.0
        else:
            rm[p - 32, p] = 1.0
    c[:, C_RM:C_RM + 128] = rm
    c[:, C_HS] = (i < 64)
    c[:, C_HS + 1] = (i >= 64)
    c[:, C_ONE:C_ONE + 128] = 1.0
    inv = (10000.0 ** (-np.arange(0, 64, 2, dtype=np.float32) / 64)).astype(np.float32)
    ang = np.arange(S, dtype=np.float32)[:, None] * inv[None, :]
    ang = np.concatenate([ang, ang], -1)
    cos = np.cos(ang).astype(np.float32).T
    sin = np.sin(ang).astype(np.float32).T
    rope = np.concatenate([np.tile(cos, (2, 1)), np.tile(sin, (2, 1))], axis=1)
    return c, np.ascontiguousarray(rope)


_CACHE = {}


def kernel(**inp):
    flags = inp.pop("_flags", {})
    key = tuple(sorted(flags.items()))
    if key not in _CACHE:
        _CACHE[key] = build(flags)
    nc = _CACHE[key]
    f = lambda a: np.ascontiguousarray(np.asarray(a, dtype=np.float32))
    colp = np.zeros((128, NCP), np.float32)
    colp[:, CP_GMIX:CP_GMIX + 8] = f(inp["norm_mix"])[0].reshape(8, 128).T
    colp[:, CP_GPLE:CP_GPLE + 8] = f(inp["norm_ple"])[0].reshape(8, 128).T
    colp[:, CP_W0:CP_W0 + 4] = f(inp["rwkv_w0"])[0].reshape(4, 128).T
    colp[:, CP_A0:CP_A0 + 4] = f(inp["rwkv_a0"])[0].reshape(4, 128).T
    colp[:, CP_KK:CP_KK + 4] = f(inp["rwkv_k_k"])[0].reshape(4, 128).T
    colp[:, CP_KA:CP_KA + 4] = f(inp["rwkv_k_a"])[0].reshape(4, 128).T
    colp[:, CP_RK:CP_RK + 4] = f(inp["rwkv_r_k"])[0].reshape(4, 128).T
    colp[:, CP_QN] = np.tile(f(inp["q_norm"])[0], 2)
    colp[:, CP_KN] = np.tile(f(inp["k_norm"])[0], 2)
    colp[:, CP_MU:CP_MU + 14] = f(inp["rwkv_mu"])[0].reshape(14, 128).T
    colp[:, CP_SUBLN] = f(inp["subln_w"])[0]
    rowb = np.zeros((128, NRB), np.float32)
    rowb[:, RB_GFFN:RB_GFFN + D] = f(inp["norm_ffn"])[0][None, :]
    rowb[:, RB_RBIAS:RB_RBIAS + 4] = f(inp["b_group"])[0][None, :]
    rowb[:, RB_RBIAS + 4:RB_RBIAS + 36] = f(inp["b_expert_router"])[0][None, :]
    rowb[:, RB_LNW:RB_LNW + 512] = f(inp["rwkv_ln_w"])[0][None, :]
    rowb[:, RB_LNB:RB_LNB + 512] = f(inp["rwkv_ln_b"])[0][None, :]
    rowb[:, RB_SUBLN:RB_SUBLN + 128] = f(inp["subln_w"])[0][None, :]
    for k, nm in enumerate(["lambda_q1", "lambda_k1", "lambda_q2", "lambda_k2"]):
        rowb[:, RB_LAM + 64 * k:RB_LAM + 64 * (k + 1)] = f(inp[nm])[0][None, :]
    rowb[:, RB_W0:RB_W0 + 512] = f(inp["rwkv_w0"])[0][None, :]
    murow = np.ascontiguousarray(np.broadcast_to(f(inp["rwkv_mu"])[0][None, :], (128, 1792)))
    cst, rope = _host_consts()
    wr = np.concatenate([f(inp["w_group"])[0], f(inp["w_expert_router"])[0]], axis=1)
    shared = {
        "w_in": f(inp["w_in"])[0], "w2": f(inp["rwkv_w2"])[0], "a2": f(inp["rwkv_a2"])[0],
        "g2": f(inp["rwkv_g2"])[0], "wbr": f(inp["w_branch_rwkv"])[0], "wbd": f(inp["w_branch_diff"])[0],
        "wout": f(inp["w_out"])[0], "wr": np.ascontiguousarray(wr), "w_gate": f(inp["w_gate"])[0],
        "w_up": f(inp["w_up"])[0], "w_down": f(inp["w_down"])[0], "wpg": f(inp["w_ple_gate"])[0],
        "wpp": f(inp["w_ple_proj"])[0], "colp": colp, "rowb": rowb, "cst": cst, "rope": rope, "murow": murow,
    }
    x = f(inp["x"])
    p = f(inp["p"])[0]
    ncores = flags.get("ncores", 8)
    in_maps = [dict(shared, x=x[c], p=p[c]) for c in range(ncores)]
    if flags.get("trace"):
        res = run_bass_kernel_spmd(nc, in_maps, core_ids=list(range(ncores)), trace=True)
        kernel.exec_ns = res.exec_time_ns
    else:
        res = run_bass_kernel_spmd(nc, in_maps, core_ids=list(range(ncores)))
    out = np.stack([np.asarray(r["y"], dtype=np.float32) for r in res.results], axis=0)
    if flags.get("dbgout"):
        kernel.dbg = [(np.asarray(r["orts"]).astype(np.float32), np.asarray(r["odts"]).astype(np.float32)) for r in res.results]
    return out
```

```python
import numpy as np
from contextlib import ExitStack
import concourse.bass as bass
import concourse.mybir as mybir
from concourse.bass_utils import run_bass_kernel_spmd

F32 = mybir.dt.float32
BF16 = mybir.dt.bfloat16
ALU = mybir.AluOpType
AF = mybir.ActivationFunctionType
AX = mybir.AxisListType

S = 4096
D = 1024
NT = 32
ENG = ("pe", "act", "dve", "pool", "sp")
EPOCH = 8000
NDMA = 24
ARENA_WORDS = 53000

CP_GMIX = 0
CP_GPLE = 8
CP_W0 = 16
CP_A0 = 20
CP_KK = 24
CP_KA = 28
CP_RK = 32
CP_QN = 36
CP_KN = 37
CP_MU1 = 38
CP_MU = 52
CP_SUBLN = 66
NCP = 67
RB_GFFN = 0
RB_RBIAS = 1024
RB_LNW = 1060
RB_LNB = 1572
RB_SUBLN = 2084
RB_LAM = 2212
RB_W0 = 2468
NRB = 2980
C_ID = 0
C_US = 128
C_UI = 256
C_LS = 384
C_BO = 512
C_RM = 640
C_HS = 768
C_ONE = 770
NCST = 898


class _Stop(Exception):
    pass


class Buf:
    __slots__ = ("w", "r")

    def __init__(self):
        self.w = None
        self.r = []


class T:
    def __init__(self, ap, b=None):
        self.ap = ap
        self.b = b if b is not None else Buf()

    def __getitem__(self, k):
        return self.ap[k]


class Prog:
    def __init__(self, nc, arena, psums):
        self.nc = nc
        self.arena = arena
        self.off = 0
        self.streams = {e: [] for e in ENG}
        self.seq = {e: 0 for e in ENG}
        self.seen = {e: {} for e in ENG}
        self.seen_d = {e: {} for e in ENG}
        self.dcnt = [0] * NDMA
        self.drr = 0
        self.ps_tiles = [T(p) for p in psums]
        self.ps_pool = list(range(len(psums)))
        self.ps_rr = 0
        self.sec = ""
        self.annot = False

    def alloc(self, shape, dt=F32):
        n = int(np.prod(shape))
        words = n if dt == F32 else (n + 1) // 2
        assert self.off + words <= ARENA_WORDS, ("arena overflow", self.off, words)
        ap = self.arena[:, self.off:self.off + words]
        self.off += words
        if dt != F32:
            ap = ap.bitcast(dt)[:, 0:n]
        if len(shape) == 2:
            ap = ap.rearrange("p (a b) -> p a b", a=shape[0])
        elif len(shape) == 3:
            ap = ap.rearrange("p (a b c) -> p a b c", a=shape[0], b=shape[1])
        return T(ap)

    def psum(self):
        t = self.ps_tiles[self.ps_pool[self.ps_rr % len(self.ps_pool)]]
        self.ps_rr = (self.ps_rr + 1) % len(self.ps_pool)
        return t

    def _need(self, eng, r, w):
        toks = set()
        for t in r:
            if t.b.w is not None:
                toks.add(t.b.w)
        for t in w:
            if t.b.w is not None:
                toks.add(t.b.w)
            toks.update(t.b.r)
        st = self.streams[eng]
        for tok in sorted(toks):
            if tok[0] == "e":
                _, pe, sq = tok
                if pe == eng and eng in ("pe", "sp"):
                    continue
                if self.seen[eng].get(pe, 0) >= sq:
                    continue
                self.seen[eng][pe] = sq
                st.append(("wait", tok))
            else:
                _, slot, cnt = tok
                if self.seen_d[eng].get(slot, 0) >= cnt:
                    continue
                self.seen_d[eng][slot] = cnt
                st.append(("wait", tok))

    def _mark(self, tok, r, w):
        for t in r:
            t.b.r.append(tok)
        for t in w:
            t.b.w = tok
            t.b.r = []

    def op(self, eng, fn, r, w):
        self._need(eng, r, w)
        self.seq[eng] += 1
        tok = ("e", eng, self.seq[eng])
        if self.annot:
            sec = self.sec
            fn0 = fn
            fn = lambda e: fn0(e).annotate(sec)
        self.streams[eng].append(("op", fn, tok))
        self._mark(tok, r, w)

    def dma(self, q, out, in_, r, w):
        slot = self.drr
        self.drr = (slot + 1) % NDMA
        prev = self.dcnt[slot]
        self._need(q, r, w)
        if prev > 0 and self.seen_d[q].get(slot, 0) < prev:
            self.seen_d[q][slot] = prev
            self.streams[q].append(("wait", ("d", slot, prev)))
        self.dcnt[slot] = prev + 1
        tok = ("d", slot, prev + 1)
        self.streams[q].append(("dma", lambda e: e.dma_start(out=out, in_=in_), tok))
        self._mark(tok, r, w)

    def barrier(self):
        toks = [("e", e, self.seq[e]) for e in ENG if self.seq[e] > 0]
        toks += [("d", s, c) for s, c in enumerate(self.dcnt) if c > 0]
        for eng in ENG:
            for tok in toks:
                if tok[0] == "e":
                    if tok[1] == eng or self.seen[eng].get(tok[1], 0) >= tok[2]:
                        continue
                    self.seen[eng][tok[1]] = tok[2]
                else:
                    if self.seen_d[eng].get(tok[1], 0) >= tok[2]:
                        continue
                    self.seen_d[eng][tok[1]] = tok[2]
                self.streams[eng].append(("wait", tok))

    def act(self, out, in_, func, r, w, **kw):
        self.op("act", lambda e: e.activation(out=out, in_=in_, func=func, **kw), r, w)

    def tt(self, eng, out, in0, in1, op, r, w):
        self.op(eng, lambda e: e.tensor_tensor(out=out, in0=in0, in1=in1, op=op), r, w)

    def ts(self, eng, out, in0, s1, s2, op0, op1, r, w):
        if op1 is None:
            self.op(eng, lambda e: e.tensor_scalar(out=out, in0=in0, scalar1=s1, scalar2=None, op0=op0), r, w)
        else:
            self.op(eng, lambda e: e.tensor_scalar(out=out, in0=in0, scalar1=s1, scalar2=s2, op0=op0, op1=op1), r, w)

    def stt(self, eng, out, in0, scalar, in1, op0, op1, r, w):
        self.op(eng, lambda e: e.scalar_tensor_tensor(out=out, in0=in0, scalar=scalar, in1=in1, op0=op0, op1=op1), r, w)

    def cp(self, eng, out, in_, r, w):
        if eng == "act":
            self.act(out, in_, AF.Copy, r, w)
        else:
            self.op(eng, lambda e: e.tensor_copy(out=out, in_=in_), r, w)

    def red(self, out, in_, op, r, w):
        self.op("dve", lambda e: e.tensor_reduce(out=out, in_=in_, axis=AX.X, op=op), r, w)

    def recip(self, out, in_, r, w):
        self.op("dve", lambda e: e.reciprocal(out=out, in_=in_), r, w)

    def memset(self, eng, ap, val, w):
        self.op(eng, lambda e: e.memset(ap, val), [], w)

    def mm(self, out, lhsT, rhs, start, stop, r, w, skip=False):
        if skip:
            self.op("pe", lambda e: e.matmul(out, lhsT=lhsT, rhs=rhs, start=start, stop=stop, skip_group_check=True), r, w)
        else:
            self.op("pe", lambda e: e.matmul(out, lhsT=lhsT, rhs=rhs, start=start, stop=stop), r, w)

    def rstd(self, out, ssq, scale, eps, r, w):
        et = self.eps_tiles[eps]
        n = out.shape[0]
        self.act(out, ssq, AF.Sqrt, list(r) + [self.eps_T], w, scale=float(scale), bias=et[0:n, :])
        self.recip(out, out, w, w)

    def emit(self, es):
        nc = self.nc
        sems = {}

        def semh(tok):
            if tok[0] == "e":
                key = ("e", tok[1], (tok[2] - 1) // EPOCH)
                val = tok[2] - key[2] * EPOCH
            else:
                key = ("d", tok[1])
                val = 16 * tok[2]
            if key not in sems:
                sems[key] = es.enter_context(nc.semaphore("s_" + "_".join(str(k) for k in key)))
            return sems[key], val

        for s, c in enumerate(self.dcnt):
            if c > 0 and self.seen_d["sp"].get(s, 0) < c:
                self.streams["sp"].append(("wait", ("d", s, c)))
        for e in ENG:
            for it in self.streams[e]:
                semh(it[1] if it[0] == "wait" else it[2])
        block = es.enter_context(nc.Block())

        def replay(name):
            def f(eng):
                for it in self.streams[name]:
                    if it[0] == "wait":
                        s, v = semh(it[1])
                        eng.wait_ge(s, v)
                    elif it[0] == "op":
                        s, _ = semh(it[2])
                        it[1](eng).then_inc(s, 1)
                    else:
                        s, _ = semh(it[2])
                        it[1](eng).then_inc(s, 16)
            return f

        block.sync(replay("sp"))
        block.scalar(replay("act"))
        block.vector(replay("dve"))
        block.gpsimd(replay("pool"))
        block.tensor(replay("pe"))


def bc(ap, shape):
    return ap.to_broadcast(shape)


def build(flags):
    nc = bass.Bass("TRN2", target_bir_lowering=False)
    dr = {}

    def din(name, shape, dt=F32):
        dr[name] = nc.dram_tensor(name, list(shape), dt, kind="ExternalInput").ap()
        return dr[name]

    x_d = din("x", [S, D])
    p_d = din("p", [S, 256])
    w_in = din("w_in", [D, 5376])
    w2_d = din("w2", [64, 512])
    a2_d = din("a2", [64, 512])
    g2_d = din("g2", [128, 512])
    wbr_d = din("wbr", [512, D])
    wbd_d = din("wbd", [512, D])
    wout_d = din("wout", [D, D])
    wr_d = din("wr", [D, 36])
    wg_d = din("w_gate", [32, D, 512])
    wu_d = din("w_up", [32, D, 512])
    wd_d = din("w_down", [32, 512, D])
    wpg_d = din("wpg", [D, D])
    wpp_d = din("wpp", [256, D])
    colp_d = din("colp", [128, NCP])
    rowb_d = din("rowb", [128, NRB])
    cst_d = din("cst", [128, NCST])
    rope_d = din("rope", [128, 2 * S])
    murow_d = din("murow", [128, 1792])
    y_d = nc.dram_tensor("y", [S, D], F32, kind="ExternalOutput").ap()
    X2 = nc.dram_tensor("x2s", [S, D], F32, kind="Internal").ap()
    H2T = nc.dram_tensor("h2ts", [8, 128, S], BF16, kind="Internal").ap()
    okind = "ExternalOutput" if flags.get("dbgout") else "Internal"
    ORT = nc.dram_tensor("orts", [4, 128, S], BF16, kind=okind).ap()
    ODT = nc.dram_tensor("odts", [4, 128, S], BF16, kind=okind).ap()
    HTD = nc.dram_tensor("htd", [128, 8, S], BF16, kind="Internal").ap()
    dbg_d = None
    if flags.get("dbg"):
        dbg_d = nc.dram_tensor("dbg", [128, flags["dbg"]], F32, kind="ExternalOutput").ap()

    with ExitStack() as es:
        arena = es.enter_context(nc.sbuf_tensor("arena", [128, ARENA_WORDS], F32))
        psums = [es.enter_context(nc.psum_tensor("ps%d" % i, [128, 512], F32)) for i in range(8)]
        P = Prog(nc, arena, [p[:, :] for p in psums])
        P.annot = bool(flags.get("annot"))

        cst = P.alloc([NCST])
        colp = P.alloc([NCP])
        rowb = P.alloc([NRB])
        cstb = P.alloc([NCST], BF16)
        P.dma("sp", cst.ap, cst_d, [], [cst])
        P.dma("sp", colp.ap, colp_d, [], [colp])
        P.dma("sp", rowb.ap, rowb_d, [], [rowb])
        P.cp("dve", cstb.ap, cst.ap, [cst], [cstb])
        identb = cstb[:, C_ID:C_ID + 128]
        identf = cst[:, C_ID:C_ID + 128]
        LG = P.alloc([NT, 36])
        WT = P.alloc([NT, 32])
        epsc = P.alloc([4])
        P.eps_T = epsc
        P.eps_tiles = {}
        for ci, ev in enumerate((1e-6, 1e-24, 64e-5)):
            P.memset("dve", epsc[:, ci:ci + 1], ev, [epsc])
            P.eps_tiles[ev] = epsc[:, ci:ci + 1]
        base_off = P.off
        hblk = [P.alloc([8, 512], BF16) for _ in range(2)]
        HTDt = T(HTD)

        def wload(dst, src, r, w):
            P.dma("pool", dst, src, r, w)

        def run_rr4(gen_list, width):
            active = []
            it = iter(gen_list)
            done = False
            while True:
                while len(active) < width and not done:
                    try:
                        active.append(next(it))
                    except StopIteration:
                        done = True
                if not active:
                    break
                for g in list(active):
                    try:
                        next(g)
                    except StopIteration:
                        active.remove(g)

        mark1 = P.off
        xt1 = [P.alloc([D]) for _ in range(3)]
        xn1 = [P.alloc([D], BF16) for _ in range(3)]
        junk1 = [P.alloc([D], BF16) for _ in range(3)]
        ssq1 = [P.alloc([1]) for _ in range(3)]
        rs1 = [P.alloc([1]) for _ in range(3)]

        def gen1(i):
            k = i % 3
            a, b_ = xt1[k], xn1[k]
            P.dma("sp", a.ap, x_d[i * 128:(i + 1) * 128, :], [], [a])
            P.act(junk1[k].ap, a.ap, AF.Square, [a], [junk1[k], ssq1[k]], accum_out=ssq1[k].ap)
            yield
            P.rstd(rs1[k].ap, ssq1[k].ap, 1.0 / D, 1e-6, [ssq1[k]], [rs1[k]])
            yield
            P.act(b_.ap, a.ap, AF.Copy, [a, rs1[k]], [b_], scale=rs1[k][:, 0:1])
            yield
            for hf in range(2):
                ps = P.psum()
                for kk in range(4):
                    kc = hf * 4 + kk
                    P.mm(ps[:, kk * 128:(kk + 1) * 128], b_[:, kc * 128:(kc + 1) * 128], identb, True, True,
                         [b_, cstb], [ps])
                hb_ = hblk[(i // 4) % 2]
                P.tt("dve", hb_[:, hf * 4:hf * 4 + 4, (i % 4) * 128:(i % 4 + 1) * 128],
                     ps.ap.rearrange("p (a b) -> p a b", a=4),
                     bc(colp[:, CP_GMIX + hf * 4:CP_GMIX + hf * 4 + 4].unsqueeze(2), [128, 4, 128]), ALU.mult,
                     [ps, colp], [hb_])
                yield
            if i % 4 == 3:
                P.dma("sp", HTD[:, :, (i // 4) * 512:(i // 4 + 1) * 512], hblk[(i // 4) % 2].ap, [hblk[(i // 4) % 2]], [HTDt])
        run_rr4((gen1(i) for i in range(NT)), 3)
        P.barrier()
        P.off = mark1

        if flags.get("stop") == 1:
            P.emit(es)
            return nc
        if flags.get("mixers", True):

            EC = float(flags.get("ec", np.exp(-0.5)))
            if flags.get("rwkv", True):
                W1 = P.alloc([8, 1792], BF16)
                W2 = P.alloc([8, 1792], BF16)
                w2a2 = P.alloc([512], BF16)
                g2b = P.alloc([512], BF16)
                wload(w2a2[0:64, :], w2_d, [], [w2a2])
                wload(w2a2[64:128, :], a2_d, [], [w2a2])
                wload(g2b.ap, g2_d, [], [g2b])
                mk = P.off
                stage = P.alloc([1792])
                mur = P.alloc([1792])
                omm = P.alloc([1792])
                P.dma("sp", mur.ap, murow_d, [], [mur])
                P.ts("dve", omm.ap, mur.ap, -1.0, 1.0, ALU.mult, ALU.add, [mur], [omm])
                for kc in range(8):
                    P.dma("sp", stage.ap, w_in[kc * 128:(kc + 1) * 128, 0:1792], [], [stage])
                    P.tt("dve", W1[:, kc, :], stage.ap, omm.ap, ALU.mult, [stage, omm], [W1])
                    P.tt("pool", W2[:, kc, :], stage.ap, mur.ap, ALU.mult, [stage, mur], [W2])
                P.barrier()
                P.off = mk
                hprevs = [P.alloc([8, 512], BF16) for _ in range(2)]
                loras = [P.alloc([512], BF16) for _ in range(2)]
                sxgs = [P.alloc([512], BF16) for _ in range(2)]
                r32, k32, v32, a32, kq, rn, t1_, kmod = [P.alloc([512]) for _ in range(8)]
                kk_, tmp = kq, rn
                eng, egm = [P.alloc([512]) for _ in range(2)]
                egs = [P.alloc([512]) for _ in range(2)]
                vb, kq2b, BT, KTt = [P.alloc([512], BF16) for _ in range(4)]
                prodks = [P.alloc([512], BF16) for _ in range(2)]
                ARs = [P.alloc([4, 2, 128], BF16) for _ in range(2)]
                TMs = [P.alloc([4, 384], BF16) for _ in range(2)]
                ztoks = [P.alloc([128])] * 2
                sgt32s = [P.alloc([128])] * 2
                sghis = [P.alloc([128], BF16) for _ in range(2)]
                sglos = [P.alloc([128], BF16) for _ in range(2)]
                NRss = [[P.alloc([2, 2, 128], BF16) for _ in range(4)] for _ in range(2)]
                AKss = [[P.alloc([2, 2, 128], BF16) for _ in range(4)] for _ in range(2)]
                WtAss = [[P.alloc([2, 128], BF16) for _ in range(4)] for _ in range(2)]
                WtBss = [[P.alloc([2, 128], BF16) for _ in range(4)] for _ in range(2)]
                LLs = [P.alloc([2, 128], BF16) for _ in range(4)]
                PQs = [[P.alloc([512], BF16) for _ in range(2)] for _ in range(4)]
                Xb = P.alloc([128], BF16)
                Ub = P.alloc([128], BF16)
                T32 = [P.alloc([128]) for _ in range(4)]
                Tb = [P.alloc([128], BF16) for _ in range(4)]
                y32s = [P.alloc([128]) for _ in range(2)]
                ysqs = [P.alloc([128]) for _ in range(2)]
                yns = [P.alloc([128]) for _ in range(2)]
                stats = [[P.alloc([2]) for _ in range(7)] for _ in range(2)]
                otoks = [P.alloc([128], BF16) for _ in range(2)]
                orblks = [P.alloc([4, 512], BF16)] * 2
                for j in range(4):
                    P.memset("pool", T32[j].ap, 0.0, [T32[j]])
                    P.memset("pool", Tb[j].ap, 0.0, [Tb[j]])
                idb2 = bc(identb.unsqueeze(1), [128, 2, 128])
                NTB = flags.get("ntb", 8)
                WtFinal = {}

                def proj(tb, g, hc, hprev):
                    ps = P.psum()
                    cs = slice(g * 128, (g + 1) * 128)
                    for kc in range(8):
                        P.mm(ps.ap, W1[:, kc, cs], hc[:, kc, :], kc == 0, False, [W1, hc], [ps])
                        P.mm(ps.ap, W2[:, kc, cs], hprev[:, kc, :], False, kc == 7, [W2, hprev], [ps])
                    return ps

                def genS(tb):
                    P.sec = "r_S"
                    t0 = tb * 512
                    tsl = slice(t0, t0 + 512)
                    hc = hblk[tb % 2]
                    hprev = hprevs[tb % 2]
                    lora = loras[tb % 2]
                    sxg = sxgs[tb % 2]
                    P.dma("sp", hc.ap, HTD[:, :, tsl], [HTDt], [hc])
                    if tb == 0:
                        P.memset("dve", hprev.ap, 0.0, [hprev])
                        P.dma("sp", hprev[:, :, 1:512], HTD[:, :, 0:511], [HTDt], [hprev])
                    else:
                        P.dma("sp", hprev.ap, HTD[:, :, t0 - 1:t0 + 511], [HTDt], [hprev])
                    yield
                    ps = proj(tb, 12, hc, hprev)
                    P.act(lora[0:64, :], ps[0:64, :], AF.Tanh, [ps], [lora])
                    P.cp("dve", lora[64:128, :], ps[64:128, :], [ps], [lora])
                    yield
                    ps = proj(tb, 13, hc, hprev)
                    P.act(sxg.ap, ps.ap, AF.Sigmoid, [ps], [sxg])
                    yield

                def genA(tb, j):
                    par = (tb * 4 + j) % 2
                    hc = hblk[tb % 2]
                    hprev = hprevs[tb % 2]
                    lora = loras[tb % 2]
                    AR, TM, eg, prodk = ARs[par], TMs[par], egs[par], prodks[par]
                    ARf = AR.ap.rearrange("p t a b -> p t (a b)")
                    NRs, AKs, WtAs, WtBs = NRss[par], AKss[par], WtAss[par], WtBss[par]
                    js = slice(j * 128, (j + 1) * 128)
                    P.sec = "r_proj"
                    ps = proj(tb, j, hc, hprev)
                    P.cp("act", r32.ap, ps.ap, [ps], [r32])
                    yield
                    P.sec = "r_proj"
                    ps = proj(tb, 4 + j, hc, hprev)
                    P.cp("dve", k32.ap, ps.ap, [ps], [k32])
                    yield
                    P.sec = "r_proj"
                    ps = proj(tb, 8 + j, hc, hprev)
                    P.cp("act", v32.ap, ps.ap, [ps], [v32])
                    P.cp("pool", vb.ap, v32.ap, [v32], [vb])
                    yield
                    P.sec = "r_preptok"
                    ps = P.psum()
                    P.mm(ps.ap, w2a2[64:128, js], lora[64:128, :], True, True, [w2a2, lora], [ps])
                    P.act(a32.ap, ps.ap, AF.Sigmoid, [ps, colp], [a32], bias=colp[:, CP_A0 + j:CP_A0 + j + 1])
                    for tl in range(4):
                        yield
                        P.sec = "r_preptok"
                        ztok, sgt32, sghi, sglo = ztoks[tl % 2], sgt32s[tl % 2], sghis[tl % 2], sglos[tl % 2]
                        ts_ = slice(tl * 128, (tl + 1) * 128)
                        ps = P.psum()
                        P.mm(ps[:, 0:128], lora[0:64, ts_], w2a2[0:64, js], True, True, [lora, w2a2], [ps])
                        P.tt("dve", ztok.ap, ps[:, 0:128], rowb[:, RB_W0 + j * 128:RB_W0 + (j + 1) * 128], ALU.add,
                             [ps, rowb], [ztok])
                        P.act(sgt32.ap, ztok.ap, AF.Sigmoid, [ztok], [sgt32])
                        P.cp("pool", sghi.ap, sgt32.ap, [sgt32], [sghi])
                        P.tt("pool", sglo.ap, sgt32.ap, sghi.ap, ALU.subtract, [sgt32, sghi], [sglo])
                        yield
                        P.sec = "r_preptok"
                        psG = P.psum()
                        P.mm(psG[:, 0:256], sghi.ap, cstb[:, C_US:C_US + 256], True, False, [sghi, cstb], [psG])
                        P.mm(psG[:, 0:256], sglo.ap, cstb[:, C_US:C_US + 256], False, True, [sglo, cstb], [psG])
                        P.act(egm[:, ts_], psG[:, 0:128], AF.Exp, [psG], [egm], scale=-EC)
                        P.act(eg[:, ts_], psG[:, 128:256], AF.Exp, [psG], [eg], scale=-EC)
                        P.act(eng[:, ts_], psG[:, 128:256], AF.Exp, [psG], [eng], scale=EC)
                    yield
                    P.sec = "r_prepfm"
                    v4 = lambda t: t.ap.rearrange("p (a b) -> p a b", a=4)
                    P.ts("dve", kq.ap, k32.ap, colp[:, CP_KK + j:CP_KK + j + 1], None, ALU.mult, None, [k32, colp], [kq])
                    P.tt("pool", kq2b.ap, kq.ap, kq.ap, ALU.mult, [kq], [kq2b])
                    ps = P.psum()
                    P.mm(ps.ap, cstb[:, C_BO:C_BO + 128], kq2b.ap, True, True, [cstb, kq2b], [ps])
                    P.rstd(rn.ap, ps.ap, 1.0, 1e-24, [ps], [rn])
                    yield
                    P.sec = "r_prepfm"
                    P.tt("dve", kk_.ap, kq.ap, rn.ap, ALU.mult, [kq, rn], [kk_])
                    P.stt("dve", AR[:, :, 0, :], v4(kk_), -1.0, v4(egm), ALU.mult, ALU.mult, [kk_, egm], [AR])
                    yield
                    P.sec = "r_prepfm"
                    P.tt("dve", tmp.ap, kk_.ap, a32.ap, ALU.mult, [kk_, a32], [tmp])
                    P.tt("pool", BT.ap, tmp.ap, eng.ap, ALU.mult, [tmp, eng], [BT])
                    yield
                    P.sec = "r_prepfm"
                    P.ts("dve", t1_.ap, a32.ap, -1.0, colp[:, CP_KA + j:CP_KA + j + 1], ALU.add, ALU.mult, [a32, colp], [t1_])
                    P.stt("dve", kmod.ap, t1_.ap, 1.0, k32.ap, ALU.add, ALU.mult, [t1_, k32], [kmod])
                    yield
                    P.sec = "r_prepfm"
                    P.tt("dve", KTt.ap, kmod.ap, eng.ap, ALU.mult, [kmod, eng], [KTt])
                    P.tt("pool", AR[:, :, 1, :], v4(r32), v4(eg), ALU.mult, [r32, eg], [AR])
                    P.stt("dve", prodk.ap, r32.ap, colp[:, CP_RK + j:CP_RK + j + 1], kmod.ap, ALU.mult, ALU.mult,
                          [r32, colp, kmod], [prodk])
                    for tl in range(4):
                        yield
                        P.sec = "r_transp"
                        ts_ = slice(tl * 128, (tl + 1) * 128)
                        ps = P.psum()
                        P.mm(ps[:, 0:128], BT[:, ts_], identb, True, True, [BT, cstb], [ps])
                        P.mm(ps[:, 128:256], KTt[:, ts_], identb, True, True, [KTt, cstb], [ps])
                        P.mm(ps[:, 256:384], vb[:, ts_], identb, True, True, [vb, cstb], [ps])
                        P.cp("act", TM[:, tl, :], ps[:, 0:384], [ps], [TM])
                    for tl in range(4):
                        ts_ = slice(tl * 128, (tl + 1) * 128)
                        yield
                        P.sec = "r_masks"
                        hss = [slice(0, 64), slice(64, 128)]
                        psXs = [P.psum(), P.psum()]
                        for h in range(2):
                            P.mm(psXs[h][:, 0:256], BT[hss[h], ts_], ARf[hss[h], tl, :], True, True, [BT, AR], [psXs[h]])
                        psYs = [P.psum(), P.psum()]
                        for h in range(2):
                            P.mm(psYs[h][:, 0:256], KTt[hss[h], ts_], ARf[hss[h], tl, :], True, True, [KTt, AR], [psYs[h]])
                        psZs = [P.psum(), P.psum()]
                        for h in range(2):
                            P.mm(psZs[h][:, 0:128], AR[hss[h], tl, 0, :], BT[hss[h], ts_], True, True, [AR, BT], [psZs[h]])
                        for h in range(2):
                            P.tt("dve", NRs[tl][:, h, :, :].rearrange("p a b -> p (a b)"), psXs[h][:, 0:256],
                                 cstb[:, C_US:C_US + 256], ALU.mult, [psXs[h], cstb], [NRs[tl]])
                            P.tt("dve", AKs[tl][:, h, :, :].rearrange("p a b -> p (a b)"), psYs[h][:, 0:256],
                                 cstb[:, C_US:C_US + 256], ALU.mult, [psYs[h], cstb], [AKs[tl]])
                            P.tt("dve", LLs[tl][:, h, :], psZs[h][:, 0:128], cstb[:, C_LS:C_LS + 128], ALU.mult,
                                 [psZs[h], cstb], [LLs[tl]])
                    yield
                    P.sec = "r_inv"
                    Wts = []
                    Pcs, Qcs, Pbs = [], [], []
                    for tl in range(4):
                        P.tt("pool", WtAs[tl].ap, NRs[tl][:, :, 0, :], idb2, ALU.add, [NRs[tl], cstb], [WtAs[tl]])
                        Wts.append([WtAs[tl], WtBs[tl]])
                        Pcs.append([NRs[tl][:, 0, 0, :], NRs[tl][:, 1, 0, :]])
                        Qcs.append([LLs[tl][:, 0, :], LLs[tl][:, 1, :]])
                        Pbs.append([NRs[tl], LLs[tl]])
                    for lvl in range(1, 7):
                        for tl in range(4):
                            yield
                            P.sec = "r_inv"
                            ps = P.psum()
                            for h in range(2):
                                P.mm(ps[:, 128 * h:128 * h + 128], Qcs[tl][h], Pcs[tl][h], True, True, Pbs[tl], [ps])
                            for h in range(2):
                                P.mm(ps[:, 256 + 128 * h:256 + 128 * h + 128], Pcs[tl][h], Qcs[tl][h], True, True, Pbs[tl], [ps])
                            pq = PQs[tl][lvl % 2]
                            P.cp("act" if tl % 2 == 0 else "dve", pq.ap, ps.ap, [ps], [pq])
                            Pcs[tl] = [pq[:, 0:128], pq[:, 128:256]]
                            Qcs[tl] = [pq[:, 256:384], pq[:, 384:512]]
                            Pbs[tl] = [pq]
                        for tl in range(4):
                            yield
                            P.sec = "r_inv"
                            Wt, Wo = Wts[tl]
                            pq = PQs[tl][lvl % 2]
                            ps2 = P.psum()
                            for h in range(2):
                                P.mm(ps2[:, 128 * h:128 * h + 128], identb, Wt[:, h, :], True, False, [cstb, Wt], [ps2])
                                P.mm(ps2[:, 128 * h:128 * h + 128], pq[:, 256 + 128 * h:256 + 128 * h + 128], Wt[:, h, :],
                                     False, True, [pq, Wt], [ps2])
                            P.cp("dve" if tl % 2 == 0 else "act", Wo.ap, ps2[:, 0:256].rearrange("p (h c) -> p h c", h=2), [ps2], [Wo])
                            Wts[tl] = [Wo, Wt]
                    WtFinal[(tb, j)] = [w[0] for w in Wts]
                    yield

                def genB(tb, j):
                    par = (tb * 4 + j) % 2
                    sxg = sxgs[tb % 2]
                    orblk = orblks[tb % 2]
                    AR, TM, eg, prodk = ARs[par], TMs[par], egs[par], prodks[par]
                    NRs, AKs = NRss[par], AKss[par]
                    js = slice(j * 128, (j + 1) * 128)
                    for tl in range(4):
                        ts_ = slice(tl * 128, (tl + 1) * 128)
                        Wt = WtFinal[(tb, j)][tl]
                        NR, AK = NRs[tl], AKs[tl]
                        y32, ysq, yn, otok = y32s[tl % 2], ysqs[tl % 2], yns[tl % 2], otoks[tl % 2]
                        ysum, yss, mean, msq, var, rsd, sb = stats[tl % 2]
                        for h in range(2):
                            P.sec = "r_chain"
                            hs = slice(64 * h, 64 * h + 64)
                            vs = slice(64 * h, 64 * h + 64)
                            psA = P.psum()
                            P.mm(psA[:, 0:64], AR[hs, tl, 0, :], Tb[j][hs, vs], True, False, [AR, Tb[j]], [psA])
                            P.mm(psA[:, 0:64], AK[:, h, 0, :], TM[:, tl, 256 + 64 * h:256 + 64 * h + 64], False, True,
                                 [AK, TM], [psA])
                            P.cp("act" if h == 0 else "dve", Xb[:, vs], psA[:, 0:64], [psA], [Xb])
                        yield
                        P.sec = "r_chain"
                        psB = P.psum()
                        for h in range(2):
                            vs = slice(64 * h, 64 * h + 64)
                            P.mm(psB[:, vs], Wt[:, h, :], Xb[:, vs], True, True, [Wt, Xb], [psB])
                        P.cp("dve", Ub.ap, psB[:, 0:128], [psB], [Ub])
                        yield
                        P.sec = "r_chain"
                        psD = P.psum()
                        P.mm(psD[:, 0:128], TM[:, tl, 0:128], Ub.ap, True, False, [TM, Ub], [psD])
                        P.mm(psD[:, 0:128], TM[:, tl, 128:256], TM[:, tl, 256:384], False, True, [TM], [psD])
                        for h in range(2):
                            hs = slice(64 * h, 64 * h + 64)
                            vs = slice(64 * h, 64 * h + 64)
                            psC = P.psum()
                            P.mm(psC[:, 0:64], AR[hs, tl, 1, :], Tb[j][hs, vs], True, False, [AR, Tb[j]], [psC])
                            P.mm(psC[:, 0:64], NR[:, h, 1, :], Ub[:, vs], False, False, [NR, Ub], [psC])
                            P.mm(psC[:, 0:64], AK[:, h, 1, :], TM[:, tl, 256 + 64 * h:256 + 64 * h + 64], False, True,
                                 [AK, TM], [psC])
                            P.cp("act", y32[:, vs], psC[:, 0:64], [psC], [y32])
                        P.tt("dve", T32[j].ap, psD[:, 0:128], T32[j].ap, ALU.add, [psD, T32[j]], [T32[j]])
                        P.ts("dve", T32[j].ap, T32[j].ap, eg[:, tl * 128 + 127:tl * 128 + 128], None, ALU.mult, None,
                             [T32[j], eg], [T32[j]])
                        P.cp("act", Tb[j].ap, T32[j].ap, [T32[j]], [Tb[j]])
                        yield
                        P.sec = "r_post"
                        y3 = lambda t: t.ap.rearrange("p (h c) -> p h c", h=2)
                        P.red(ysum.ap, y3(y32), ALU.add, [y32], [ysum])
                        P.tt("pool", ysq.ap, y32.ap, y32.ap, ALU.mult, [y32], [ysq])
                        P.red(yss.ap, y3(ysq), ALU.add, [ysq], [yss])
                        P.ts("pool", mean.ap, ysum.ap, 1.0 / 64, None, ALU.mult, None, [ysum], [mean])
                        P.tt("pool", msq.ap, mean.ap, mean.ap, ALU.mult, [mean], [msq])
                        yield
                        P.sec = "r_post"
                        P.stt("dve", var.ap, yss.ap, 1.0 / 64, msq.ap, ALU.mult, ALU.subtract, [yss, msq], [var])
                        P.rstd(rsd.ap, var.ap, 1.0, 64e-5, [var], [rsd])
                        yield
                        P.sec = "r_post"
                        for h in range(2):
                            vs = slice(64 * h, 64 * h + 64)
                            P.ts("pool", yn[:, vs], y32[:, vs], mean[:, h:h + 1], rsd[:, h:h + 1], ALU.subtract, ALU.mult,
                                 [y32, mean, rsd], [yn])
                        P.tt("pool", yn.ap, yn.ap, rowb[:, RB_LNW + j * 128:RB_LNW + (j + 1) * 128], ALU.mult, [yn, rowb], [yn])
                        P.tt("pool", yn.ap, yn.ap, rowb[:, RB_LNB + j * 128:RB_LNB + (j + 1) * 128], ALU.add, [yn, rowb], [yn])
                        psS = P.psum()
                        P.mm(psS[:, 0:2], prodk[:, ts_], cstb[:, C_HS:C_HS + 2], True, True, [prodk, cstb], [psS])
                        P.cp("act", sb.ap, psS[:, 0:2], [psS], [sb])
                        yield
                        P.sec = "r_post"
                        for h in range(2):
                            vs = slice(64 * h, 64 * h + 64)
                            P.stt("dve", yn[:, vs], TM[:, tl, 256 + 64 * h:256 + 64 * h + 64], sb[:, h:h + 1], yn[:, vs],
                                  ALU.mult, ALU.add, [TM, sb, yn], [yn])
                        psT = P.psum()
                        P.mm(psT[:, 0:128], sxg[:, ts_], g2b[:, js], True, True, [sxg, g2b], [psT])
                        P.tt("dve", otok.ap, yn.ap, psT[:, 0:128], ALU.mult, [yn, psT], [otok])
                        yield
                        P.sec = "r_post"
                        psO = P.psum()
                        P.mm(psO[:, 0:128], otok.ap, identb, True, True, [otok, cstb], [psO])
                        P.cp("act", orblk[:, j, ts_], psO[:, 0:128], [psO], [orblk])
                        yield
                    if j == 3:
                        P.dma("sp", ORT[:, :, tb * 512:(tb + 1) * 512].rearrange("k p t -> p k t"), orblk.ap, [orblk], [])

                def chain(*gens):
                    for g in gens:
                        yield from g

                def interleave(ga, gb, ra=1, rb=1):
                    alive_a, alive_b = True, True
                    while alive_a or alive_b:
                        for _ in range(ra):
                            if alive_a:
                                try:
                                    next(ga)
                                except StopIteration:
                                    alive_a = False
                        for _ in range(rb):
                            if alive_b:
                                try:
                                    next(gb)
                                except StopIteration:
                                    alive_b = False
                units = [(tb, j) for tb in range(NTB) for j in range(4)]

                def genAfull(u):
                    tb, j = units[u]
                    if j == 0:
                        return chain(genS(tb), genA(tb, j))
                    return genA(tb, j)
                for _ in genAfull(0):
                    pass
                for u in range(len(units)):
                    gb = genB(*units[u])
                    if flags.get("seqB"):
                        for _ in gb:
                            pass
                        if u + 1 < len(units):
                            for _ in genAfull(u + 1):
                                pass
                    elif u + 1 < len(units):
                        interleave(genAfull(u + 1), gb, flags.get("ra", 1), flags.get("rb", 1))
                    else:
                        for _ in gb:
                            pass
                P.barrier()
                P.off = mark1
            else:
                z = P.alloc([4, S], BF16)
                P.memset("pool", z.ap, 0.0, [z])
                for j in range(4):
                    P.dma("sp", ORT[j], z[:, j, :], [z], [])
                P.barrier()
                P.off = mark1
            if flags.get("attn", True):
                Wq = P.alloc([8, 512], BF16)
                ropeb = P.alloc([2 * S], BF16)
                for kc in range(8):
                    rws = slice(kc * 128, (kc + 1) * 128)
                    wload(Wq[:, kc, :], w_in[rws, 1792:2304], [], [Wq])
                for q4 in range(4):
                    wload(ropeb[:, q4 * 2048:(q4 + 1) * 2048], rope_d[:, q4 * 2048:(q4 + 1) * 2048], [], [ropeb])
                cosb = ropeb[:, 0:S]
                sinb = ropeb[:, S:2 * S]
                KT = P.alloc([4, S], BF16)
                Vx = P.alloc([NT, 4, 130], BF16)
                P.memset("pool", Vx[:, :, :, 128:130], 1.0, [Vx])
                lt = P.alloc([64])
                ls = P.alloc([2])
                lam = P.alloc([1])
                nlam = P.alloc([1])
                for k in range(2):
                    P.tt("dve", lt.ap, rowb[:, RB_LAM + 128 * k:RB_LAM + 128 * k + 64],
                         rowb[:, RB_LAM + 128 * k + 64:RB_LAM + 128 * k + 128], ALU.mult, [rowb], [lt])
                    P.red(ls[:, k:k + 1], lt.ap, ALU.add, [lt], [ls])
                P.act(ls.ap, ls.ap, AF.Exp, [ls], [ls])
                P.tt("dve", lam.ap, ls[:, 0:1], ls[:, 1:2], ALU.subtract, [ls], [lam])
                P.ts("dve", nlam.ap, lam.ap, 0.2, -1.0, ALU.add, ALU.mult, [lam], [nlam])
                qsets = [(P.alloc([512]), P.alloc([512], BF16), P.alloc([512]), P.alloc([512], BF16), P.alloc([512]),
                          P.alloc([512])) for _ in range(3)]
                QT = P.alloc([512], BF16)
                mkW = P.off
                Wk = P.alloc([8, 512], BF16)
                Wv = P.alloc([8, 512], BF16)
                for kc in range(8):
                    rws = slice(kc * 128, (kc + 1) * 128)
                    wload(Wk[:, kc, :], w_in[rws, 2304:2816], [], [Wk])
                    wload(Wv[:, kc, :], w_in[rws, 2816:3328], [], [Wv])

                hld = {}

                def hload(blk):
                    if hld.get(blk % 2) != blk:
                        P.dma("sp", hblk[blk % 2].ap, HTD[:, :, blk * 512:(blk + 1) * 512], [HTDt], [hblk[blk % 2]])
                        hld[blk % 2] = blk
                    return hblk[blk % 2]

                def qk_gen(W, gcol, blk, h, dst, dstbuf, k):
                    raw, sqb, rsb, knb, ta, tb_ = qsets[k]
                    P.sec = "a_prep"
                    tsl = slice(blk * 512, (blk + 1) * 512)
                    hc = hload(blk)
                    ps = P.psum()
                    for kc in range(8):
                        P.mm(ps.ap, W[:, kc, h * 128:(h + 1) * 128], hc[:, kc, :], kc == 0, kc == 7, [W, hc], [ps])
                    P.cp("act", raw.ap, ps.ap, [ps], [raw])
                    yield
                    P.sec = "a_prep"
                    P.tt("pool", sqb.ap, raw.ap, raw.ap, ALU.mult, [raw], [sqb])
                    yield
                    P.sec = "a_prep"
                    ps2 = P.psum()
                    P.mm(ps2.ap, cstb[:, C_BO:C_BO + 128], sqb.ap, True, True, [cstb, sqb], [ps2])
                    P.rstd(rsb.ap, ps2.ap, 1.0 / 64, 1e-6, [ps2], [rsb])
                    yield
                    P.sec = "a_prep"
                    P.stt("dve", knb.ap, raw.ap, colp[:, gcol:gcol + 1], rsb.ap, ALU.mult, ALU.mult, [raw, colp, rsb], [knb])
                    yield
                    P.sec = "a_prep"
                    ps3 = P.psum()
                    P.mm(ps3.ap, cstb[:, C_RM:C_RM + 128], knb.ap, True, True, [cstb, knb], [ps3])
                    P.tt("dve", tb_.ap, ps3.ap, sinb[:, tsl], ALU.mult, [ps3, ropeb], [tb_])
                    yield
                    P.sec = "a_prep"
                    P.tt("pool", ta.ap, knb.ap, cosb[:, tsl], ALU.mult, [knb, ropeb], [ta])
                    yield
                    P.sec = "a_prep"
                    P.tt("pool", dst, ta.ap, tb_.ap, ALU.add, [ta, tb_], [dstbuf])
                    yield

                def qk_prep(W, gcol, blk, h, dst, dstbuf):
                    for _ in qk_gen(W, gcol, blk, h, dst, dstbuf, 0):
                        pass

                def v_gen(blk):
                    hc = hload(blk)
                    for i in range(blk * 4, blk * 4 + 4):
                        P.sec = "a_v"
                        ps = P.psum()
                        for kc in range(8):
                            P.mm(ps.ap, hc[:, kc, (i % 4) * 128:(i % 4 + 1) * 128], Wv[:, kc, :], kc == 0, kc == 7, [hc, Wv], [ps])
                        P.cp("act" if i % 2 else "dve", Vx[:, i, :, 0:128], ps.ap.rearrange("p (h c) -> p h c", h=4), [ps], [Vx])
                        yield

                def run_rr(gen_list, width):
                    active = []
                    it = iter(gen_list)
                    done = False
                    while True:
                        while len(active) < width and not done:
                            try:
                                active.append(next(it))
                            except StopIteration:
                                done = True
                        if not active:
                            break
                        for g in list(active):
                            try:
                                next(g)
                            except StopIteration:
                                active.remove(g)
                KTs = [T(KT.ap) for _ in range(4)]
                kgens = []
                cnt_ = 0
                for blk in range(8):
                    for h in range(4):
                        kgens.append((Wk, CP_KN, blk, h, KT[:, h, blk * 512:(blk + 1) * 512], KTs[h], cnt_ % 3))
                        cnt_ += 1
                    kgens.append(("v", blk))

                def mk(g):
                    if g[0] == "v":
                        return v_gen(g[1])
                    return qk_gen(*g)
                run_rr((mk(g) for g in kgens), 3)
                P.barrier()
                P.off = mkW
                P.ps_pool = [0, 1, 2, 3, 4]
                P.ps_rr = 0
                Ob = [P.ps_tiles[5], P.ps_tiles[6], P.ps_tiles[7]]

                def Oacc(c, qt):
                    n = c * 4 + qt
                    return Ob[n // 3], slice((n % 3) * 130, (n % 3) * 130 + 129)
                ptl = [P.alloc([512], BF16) for _ in range(6)]
                QT2 = P.alloc([512], BF16)
                OT = [P.ps_tiles[5], P.ps_tiles[6]]
                P.ps_pool = [0, 1, 2, 3]
                Zb = [P.ps_tiles[4], P.ps_tiles[7]]
                Zacc = [[P.alloc([512]) for _ in range(2)] for _ in range(2)]
                zhi = P.alloc([512], BF16)
                zlo = P.alloc([512], BF16)
                rzs = [P.alloc([512]) for _ in range(2)]
                tAs = [P.alloc([512]) for _ in range(2)]
                o32 = P.alloc([512])
                sqb_ = P.alloc([512], BF16)
                rsq = P.alloc([512])
                odblk = P.alloc([4, 512], BF16)
                QTs = [QT, QT2]
                LOOK = flags.get("look", 4)
                NQB = flags.get("nqb", 8)
                heads = [(qb, h) for qb in range(NQB) for h in range(4)]
                ptc = [0]
                zb = cstb[:, C_LS:C_LS + 128]

                def clear_banks():
                    for ob_ in Ob:
                        P.mm(ob_[:, 400:402], zb, cstb[:, C_HS:C_HS + 2], True, True, [cstb], [ob_])

                class Ctx:
                    pass

                def make(n):
                    c = Ctx()
                    c.qb, c.h = heads[n]
                    c.n = n
                    c.QT = QTs[n % 2]
                    c.items = [(kt, cc) for kt in range(4 * c.qb + 4) for cc in range(2)]
                    c.pts = {}
                    return c

                def stage1(c, i):
                    P.sec = "a_s1"
                    kt, cc = c.items[i]
                    r_ = max(kt - 4 * c.qb, 0)
                    c0 = r_ * 128
                    N = 512 - c0
                    cs = slice(64 * cc, 64 * cc + 64)
                    ps = P.psum()
                    P.mm(ps[:, 0:N], KT[cs, c.h, kt * 128:(kt + 1) * 128], c.QT[cs, c0:512], True, True, [KT, c.QT], [ps])
                    pt = ptl[ptc[0] % 6]
                    ptc[0] += 1
                    c.pts[i] = pt
                    P.act(pt[:, 0:N], ps[:, 0:N], AF.Exp, [ps], [pt], scale=0.125)
                    if kt >= 4 * c.qb:
                        P.tt("pool", pt[:, 0:128], pt[:, 0:128], cstb[:, C_UI:C_UI + 128], ALU.mult, [pt, cstb], [pt])

                def stage2(c, i):
                    P.sec = "a_s2"
                    kt, cc = c.items[i]
                    r_ = max(kt - 4 * c.qb, 0)
                    c0 = r_ * 128
                    N = 512 - c0
                    pt = c.pts.pop(i)
                    za = Zacc[c.n % 2][cc]
                    P.mm(OT[cc][:, c0:512], Vx[:, kt, c.h, 0:128], pt[:, 0:N], kt == 0, kt == 4 * c.qb + 3, [Vx, pt], [OT[cc]])
                    P.mm(Zb[cc][:, c0:512], cstb[:, C_ONE:C_ONE + 128], pt[:, 0:N], kt == 0, kt == 4 * c.qb + 3, [cstb, pt], [Zb[cc]])
                qk_prep(Wq, CP_QN, 0, 0, QTs[0].ap, QTs[0])
                cur = make(0)
                for i in range(min(LOOK, len(cur.items))):
                    stage1(cur, i)
                for n in range(len(heads)):
                    c = cur
                    qb, h = c.qb, c.h
                    tsl = slice(qb * 512, (qb + 1) * 512)
                    qg = None
                    if n + 1 < len(heads):
                        nqb, nh_ = heads[n + 1]
                        qg = qk_gen(Wq, CP_QN, nqb, nh_, QTs[(n + 1) % 2].ap, QTs[(n + 1) % 2], n % 3)
                    if flags.get("pair", 1):
                        for i in range(0, len(c.items), 2):
                            for d_ in (0, 1):
                                if i + d_ + LOOK < len(c.items):
                                    stage1(c, i + d_ + LOOK)
                            for d_ in (0, 1):
                                stage2(c, i + d_)
                                if qg is not None:
                                    try:
                                        next(qg)
                                    except StopIteration:
                                        qg = None
                    else:
                        for i in range(len(c.items)):
                            if i + LOOK < len(c.items):
                                stage1(c, i + LOOK)
                            stage2(c, i)
                            if qg is not None:
                                try:
                                    next(qg)
                                except StopIteration:
                                    qg = None
                    if qg is not None:
                        for _ in qg:
                            pass
                    P.sec = "a_tail"
                    onesb = cstb[:, C_ONE:C_ONE + 128]
                    for cc in range(2):
                        P.recip(rzs[cc].ap, Zb[cc].ap, [Zb[cc]], [rzs[cc]])
                        P.tt("dve", tAs[cc].ap, OT[cc].ap, rzs[cc].ap, ALU.mult, [OT[cc], rzs[cc]], [tAs[cc]])
                    if n + 1 < len(heads):
                        cur = make(n + 1)
                        for i in range(min(LOOK, len(cur.items))):
                            stage1(cur, i)
                    P.sec = "a_tail"
                    P.stt("dve", o32.ap, tAs[1].ap, nlam[:, 0:1], tAs[0].ap, ALU.mult, ALU.add, [tAs[1], nlam, tAs[0]], [o32])
                    P.tt("pool", sqb_.ap, o32.ap, o32.ap, ALU.mult, [o32], [sqb_])
                    pss = P.psum()
                    P.mm(pss.ap, onesb, sqb_.ap, True, True, [cstb, sqb_], [pss])
                    P.rstd(rsq.ap, pss.ap, 1.0 / 128, 1e-6, [pss], [rsq])
                    P.tt("dve", o32.ap, o32.ap, rsq.ap, ALU.mult, [o32, rsq], [o32])
                    P.ts("dve", odblk[:, h, :], o32.ap, colp[:, CP_SUBLN:CP_SUBLN + 1], 0.8, ALU.mult, ALU.mult, [o32, colp], [odblk])
                    if h == 3:
                        P.dma("sp", ODT[:, :, tsl].rearrange("k p t -> p k t"), odblk.ap, [odblk], [])
                P.ps_pool = list(range(8))
                P.ps_rr = 0
                P.barrier()
                P.off = mark1
            else:
                z = P.alloc([4, S], BF16)
                P.memset("pool", z.ap, 0.0, [z])
                for j in range(4):
                    P.dma("sp", ODT[j], z[:, j, :], [z], [])
                P.barrier()
                P.off = mark1
        else:
            z = P.alloc([4, S], BF16)
            P.memset("pool", z.ap, 0.0, [z])
            for j in range(4):
                P.dma("sp", ORT[j], z[:, j, :], [z], [])
                P.dma("sp", ODT[j], z[:, j, :], [z], [])
            P.barrier()
            P.off = mark1

        if flags.get("stop") == 2:
            P.emit(es)
            return nc
        Wg = P.alloc([8, 2048], BF16)
        Wbr = P.alloc([4, D], BF16)
        Wbd = P.alloc([4, D], BF16)
        Wout = P.alloc([8, D], BF16)
        Wr = P.alloc([8, 36])
        for kc in range(8):
            wload(Wg[:, kc, :], w_in[kc * 128:(kc + 1) * 128, 3328:5376], [], [Wg])
            wload(Wout[:, kc, :], wout_d[kc * 128:(kc + 1) * 128, :], [], [Wout])
            P.dma("sp", Wr[:, kc, :], wr_d[kc * 128:(kc + 1) * 128, :], [], [Wr])
        for kc in range(4):
            wload(Wbr[:, kc, :], wbr_d[kc * 128:(kc + 1) * 128, :], [], [Wbr])
            wload(Wbd[:, kc, :], wbd_d[kc * 128:(kc + 1) * 128, :], [], [Wbd])
        orb = [P.alloc([4, 512], BF16) for _ in range(2)]
        odb = [P.alloc([4, 512], BF16) for _ in range(2)]
        sgr = [P.alloc([512]) for _ in range(2)]
        sgd = [P.alloc([512]) for _ in range(2)]
        t1 = [P.alloc([512]) for _ in range(2)]
        t2 = [P.alloc([512]) for _ in range(2)]
        mixT = [P.alloc([8, 512], BF16) for _ in range(2)]
        NS4 = 2
        xt = [P.alloc([D]) for _ in range(NS4)]
        x2 = [P.alloc([D]) for _ in range(NS4)]
        h2 = [P.alloc([D]) for _ in range(NS4)]
        junks = [P.alloc([D], BF16) for _ in range(NS4)]
        ssq2 = [P.alloc([1]) for _ in range(NS4)]
        rs2 = [P.alloc([1]) for _ in range(NS4)]
        h2his = [P.alloc([D], BF16) for _ in range(NS4)]
        h2los = [P.alloc([D], BF16) for _ in range(NS4)]
        h2Tls = [P.alloc([8, 128], BF16) for _ in range(NS4)]
        h2Tbs = [P.alloc([8, 128], BF16) for _ in range(NS4)]
        Wrh = P.alloc([8, 36], BF16)
        Wrl = P.alloc([8, 36], BF16)
        P.cp("dve", Wrh.ap, Wr.ap, [Wr], [Wrh])
        P.tt("dve", Wrl.ap, Wr.ap, Wrh.ap, ALU.subtract, [Wr, Wrh], [Wrl])
        LGs = [T(LG.ap) for _ in range(NT)]

        def genM(tb):
            ob, db, mx = orb[tb % 2], odb[tb % 2], mixT[tb % 2]
            tsl = slice(tb * 512, (tb + 1) * 512)
            hc = hblk[tb % 2]
            P.dma("sp", hc.ap, HTD[:, :, tsl], [HTDt], [hc])
            P.dma("sp", ob.ap, ORT[:, :, tsl].rearrange("k p t -> p k t"), [], [ob])
            P.dma("sp", db.ap, ODT[:, :, tsl].rearrange("k p t -> p k t"), [], [db])
            yield
            for m in range(8):
                q = m % 2
                ps = P.psum()
                for kc in range(8):
                    P.mm(ps.ap, Wg[:, kc, m * 128:(m + 1) * 128], hc[:, kc, :], kc == 0, kc == 7, [Wg, hc], [ps])
                P.act(sgr[q].ap, ps.ap, AF.Sigmoid, [ps], [sgr[q]])
                ps = P.psum()
                for kc in range(8):
                    P.mm(ps.ap, Wg[:, kc, 1024 + m * 128:1024 + (m + 1) * 128], hc[:, kc, :], kc == 0, kc == 7,
                         [Wg, hc], [ps])
                P.act(sgd[q].ap, ps.ap, AF.Sigmoid, [ps], [sgd[q]])
                yield
                ps = P.psum()
                for kc in range(4):
                    P.mm(ps.ap, Wbr[:, kc, m * 128:(m + 1) * 128], ob[:, kc, :], kc == 0, kc == 3, [Wbr, ob], [ps])
                P.tt("dve", t1[q].ap, ps.ap, sgr[q].ap, ALU.mult, [ps, sgr[q]], [t1[q]])
                ps = P.psum()
                for kc in range(4):
                    P.mm(ps.ap, Wbd[:, kc, m * 128:(m + 1) * 128], db[:, kc, :], kc == 0, kc == 3, [Wbd, db], [ps])
                P.tt("dve", t2[q].ap, ps.ap, sgd[q].ap, ALU.mult, [ps, sgd[q]], [t2[q]])
                P.tt("pool", mx[:, m, :], t1[q].ap, t2[q].ap, ALU.add, [t1[q], t2[q]], [mx])
                yield

        def genT(i):
            tb, tt_ = divmod(i, 4)
            mx = mixT[tb % 2]
            k = i % NS4
            rows = slice(i * 128, (i + 1) * 128)
            xt_, x2_, h2_, junk_, ssq_, rs_, hi_, lo_, Tl_, Tb_ = (xt[k], x2[k], h2[k], junks[k], ssq2[k], rs2[k], h2his[k],
                                                                  h2los[k], h2Tls[k], h2Tbs[k])
            P.dma("sp", xt_.ap, x_d[rows, :], [], [xt_])
            for hc_ in range(2):
                ps = P.psum()
                for m in range(8):
                    P.mm(ps.ap, mx[:, m, tt_ * 128:(tt_ + 1) * 128], Wout[:, m, hc_ * 512:(hc_ + 1) * 512],
                         m == 0, m == 7, [mx, Wout], [ps])
                P.tt("dve", x2_[:, hc_ * 512:(hc_ + 1) * 512], ps.ap, xt_[:, hc_ * 512:(hc_ + 1) * 512], ALU.add,
                     [ps, xt_], [x2_])
            P.dma("sp", X2[rows, :], x2_.ap, [x2_], [])
            yield
            P.act(junk_.ap, x2_.ap, AF.Square, [x2_], [junk_, ssq_], accum_out=ssq_.ap)
            yield
            P.rstd(rs_.ap, ssq_.ap, 1.0 / D, 1e-6, [ssq_], [rs_])
            yield
            P.act(h2_.ap, x2_.ap, AF.Copy, [x2_, rs_], [h2_], scale=rs_[:, 0:1])
            yield
            P.tt("pool", h2_.ap, h2_.ap, rowb[:, RB_GFFN:RB_GFFN + D], ALU.mult, [h2_, rowb], [h2_])
            yield
            P.cp("act", hi_.ap, h2_.ap, [h2_], [hi_])
            yield
            P.tt("pool", lo_.ap, h2_.ap, hi_.ap, ALU.subtract, [h2_, hi_], [lo_])
            yield
            for src, dst in ((hi_, Tb_), (lo_, Tl_)):
                for hf in range(2):
                    ps = P.psum()
                    for kk in range(4):
                        kc = hf * 4 + kk
                        P.mm(ps[:, kk * 128:(kk + 1) * 128], src[:, kc * 128:(kc + 1) * 128], identb, True, True,
                             [src, cstb], [ps])
                    P.cp("act" if hf == 0 else "dve", dst[:, hf * 4:hf * 4 + 4, :],
                         ps.ap.rearrange("p (a b) -> p a b", a=4), [ps], [dst])
                    yield
            P.dma("sp", H2T[:, :, rows].rearrange("k p t -> p k t"), Tb_.ap, [Tb_], [])
            ps = P.psum()
            n = 0
            for kc in range(8):
                for (a_, b_) in ((Tb_, Wrh), (Tb_, Wrl), (Tl_, Wrh)):
                    P.mm(ps[:, 0:36], a_[:, kc, :], b_[:, kc, :], n == 0, n == 23, [a_, b_], [ps])
                    n += 1
            P.tt("dve", LG[:, i, :], ps[:, 0:36], rowb[:, RB_RBIAS:RB_RBIAS + 36], ALU.add, [ps, rowb], [LGs[i]])
            yield

        for _ in genM(0):
            pass
        for tb in range(8):
            grp = [genM(tb + 1)] if tb + 1 < 8 else []
            grp += [genT(tb * 4 + k) for k in range(4)]
            run_rr4(iter(grp), 3 if tb + 1 < 8 else 2)
        P.barrier()
        P.off = base_off

        if flags.get("stop") == 3:
            P.emit(es)
            return nc
        mark2 = P.off
        glog = LG[:, :, 0:4]
        elog = LG[:, :, 4:36].rearrange("p t (g j) -> p t g j", g=4)
        gmax = P.alloc([NT])
        gsh = P.alloc([NT, 4])
        gex = P.alloc([NT, 4])
        gsum = P.alloc([NT])
        gtop = P.alloc([NT])
        gmask = P.alloc([NT, 4])
        tmp4 = P.alloc([NT, 4, 8])
        esel = P.alloc([NT, 8])
        esel2 = P.alloc([NT, 8])
        m1 = P.alloc([NT])
        m2 = P.alloc([NT])
        mk1 = P.alloc([NT, 8])
        mk2 = P.alloc([NT, 8])
        dm = P.alloc([NT])
        w1 = P.alloc([NT])
        w2 = P.alloc([NT])
        wsel = P.alloc([NT, 8])
        wsel2 = P.alloc([NT, 8])
        P.red(gmax.ap, glog, ALU.max, [LG], [gmax])
        P.tt("dve", gsh.ap, glog, bc(gmax.ap.unsqueeze(2), [128, NT, 4]), ALU.subtract, [LG, gmax], [gsh])
        P.act(gex.ap, gsh.ap, AF.Exp, [gsh], [gex])
        P.red(gsum.ap, gex.ap, ALU.add, [gex], [gsum])
        P.recip(gtop.ap, gsum.ap, [gsum], [gtop])
        P.ts("dve", gmask.ap, gsh.ap, 0.0, None, ALU.is_equal, None, [gsh], [gmask])
        P.tt("dve", tmp4.ap, elog, bc(gmask.ap.unsqueeze(3), [128, NT, 4, 8]), ALU.mult, [LG, gmask], [tmp4])
        P.red(esel.ap, tmp4.ap.rearrange("p t g j -> p t j g"), ALU.add, [tmp4], [esel])
        P.red(m1.ap, esel.ap, ALU.max, [esel], [m1])
        P.tt("dve", mk1.ap, esel.ap, bc(m1.ap.unsqueeze(2), [128, NT, 8]), ALU.is_equal, [esel, m1], [mk1])
        P.stt("dve", esel2.ap, mk1.ap, -1e30, esel.ap, ALU.mult, ALU.add, [mk1, esel], [esel2])
        P.red(m2.ap, esel2.ap, ALU.max, [esel2], [m2])
        P.tt("dve", mk2.ap, esel2.ap, bc(m2.ap.unsqueeze(2), [128, NT, 8]), ALU.is_equal, [esel2, m2], [mk2])
        P.tt("dve", dm.ap, m1.ap, m2.ap, ALU.subtract, [m1, m2], [dm])
        P.act(w1.ap, dm.ap, AF.Sigmoid, [dm], [w1])
        P.tt("dve", w1.ap, w1.ap, gtop.ap, ALU.mult, [w1, gtop], [w1])
        P.tt("dve", w2.ap, gtop.ap, w1.ap, ALU.subtract, [gtop, w1], [w2])
        P.tt("dve", wsel.ap, mk1.ap, bc(w1.ap.unsqueeze(2), [128, NT, 8]), ALU.mult, [mk1, w1], [wsel])
        P.tt("dve", wsel2.ap, mk2.ap, bc(w2.ap.unsqueeze(2), [128, NT, 8]), ALU.mult, [mk2, w2], [wsel2])
        P.tt("dve", wsel.ap, wsel.ap, wsel2.ap, ALU.add, [wsel, wsel2], [wsel])
        P.tt("dve", WT.ap.rearrange("p t (g j) -> p t g j", g=4), bc(gmask.ap.unsqueeze(3), [128, NT, 4, 8]),
             bc(wsel.ap.unsqueeze(2), [128, NT, 4, 8]), ALU.mult, [gmask, wsel], [WT])
        P.barrier()
        P.off = mark2

        if flags.get("stop") == 4:
            P.emit(es)
            return nc
        acc = [P.alloc([D]) for _ in range(16)]
        h2h = P.alloc([8, 2048], BF16)
        Wgu = [P.alloc([8, 1024], BF16) for _ in range(2)]
        Wd = [P.alloc([4, D], BF16) for _ in range(2)]
        sg = [P.alloc([512]) for _ in range(2)]
        actT = [P.alloc([4, 512], BF16) for _ in range(2)]
        ssq3s = [P.alloc([1]) for _ in range(2)]
        rs3s = [P.alloc([1]) for _ in range(2)]
        xn3 = [P.alloc([D], BF16) for _ in range(2)]
        x3T = [P.alloc([8, 128], BF16) for _ in range(2)]
        pt_ = [P.alloc([256]) for _ in range(2)]
        ptb = [P.alloc([256], BF16) for _ in range(2)]
        pT = [P.alloc([2, 128], BF16) for _ in range(2)]
        sgp = [P.alloc([D])] * 2
        yo = [P.alloc([D]) for _ in range(2)]
        NE = flags.get("n_experts", 32)
        for half in range(2):
            t0 = half * 2048
            for k in range(16):
                P.dma("sp", acc[k].ap, X2[t0 + k * 128:t0 + (k + 1) * 128, :], [], [acc[k]])
            P.dma("sp", h2h.ap, H2T[:, :, t0:t0 + 2048].rearrange("k p t -> p k t"), [], [h2h])
            def load_expert(e):
                wg_, wd_ = Wgu[e % 2], Wd[e % 2]
                for kc in range(8):
                    wload(wg_[:, kc, 0:512], wg_d[e, kc * 128:(kc + 1) * 128, :], [], [wg_])
                    wload(wg_[:, kc, 512:1024], wu_d[e, kc * 128:(kc + 1) * 128, :], [], [wg_])
                for kc in range(4):
                    wload(wd_[:, kc, :], wd_d[e, kc * 128:(kc + 1) * 128, :], [], [wd_])

            def stageG(u, fcs=(0, 1, 2, 3)):
                e, tb = divmod(u, 4)
                wg_ = Wgu[e % 2]
                at = actT[u % 2]
                tsl = slice(tb * 512, (tb + 1) * 512)
                for fc in fcs:
                    q = fc % 2
                    psg = P.psum()
                    for kc in range(8):
                        P.mm(psg.ap, wg_[:, kc, fc * 128:(fc + 1) * 128], h2h[:, kc, tsl], kc == 0, kc == 7,
                             [wg_, h2h], [psg])
                    psu = P.psum()
                    for kc in range(8):
                        P.mm(psu.ap, wg_[:, kc, 512 + fc * 128:512 + (fc + 1) * 128], h2h[:, kc, tsl], kc == 0,
                             kc == 7, [wg_, h2h], [psu])
                    P.act(sg[q].ap, psg.ap, AF.Silu, [psg], [sg[q]])
                    P.tt("dve", at[:, fc, :], psu.ap, sg[q].ap, ALU.mult, [psu, sg[q]], [at])

            def stageD(u):
                e, tb = divmod(u, 4)
                wd_ = Wd[e % 2]
                at = actT[u % 2]
                for tt_ in range(4):
                    k = tb * 4 + tt_
                    gi = half * 16 + k
                    for hc in range(2):
                        ps = P.psum()
                        for fc in range(4):
                            P.mm(ps.ap, at[:, fc, tt_ * 128:(tt_ + 1) * 128], wd_[:, fc, hc * 512:(hc + 1) * 512],
                                 fc == 0, fc == 3, [at, wd_], [ps])
                        P.stt("dve", acc[k][:, hc * 512:(hc + 1) * 512], ps.ap, WT[:, gi, e:e + 1],
                              acc[k][:, hc * 512:(hc + 1) * 512], ALU.mult, ALU.add, [ps, WT, acc[k]], [acc[k]])
                if tb == 3 and e + 2 < NE:
                    load_expert(e + 2)
            if NE > 0:
                load_expert(0)
                if NE > 1:
                    load_expert(1)
                NU = NE * 4
                nla = flags.get("moe_la", 2)
                stageG(0)
                for u in range(NU):
                    if u + 1 < NU and nla > 0:
                        stageG(u + 1, tuple(range(nla)))
                    stageD(u)
                    if u + 1 < NU and nla < 4:
                        stageG(u + 1, tuple(range(nla, 4)))
            Wpg, Wpp = Wgu[0], Wd[0]
            for kc in range(8):
                wload(Wpg[:, kc, :], wpg_d[kc * 128:(kc + 1) * 128, :], [], [Wpg])
            for kc in range(2):
                wload(Wpp[:, kc, :], wpp_d[kc * 128:(kc + 1) * 128, :], [], [Wpp])
            def genP(k):
                gi = half * 16 + k
                q = k % 2
                rows = slice(gi * 128, (gi + 1) * 128)
                a = acc[k]
                P.dma("sp", pt_[q].ap, p_d[rows, :], [], [pt_[q]])
                P.act(yo[q].ap, a.ap, AF.Square, [a], [yo[q], ssq3s[q]], accum_out=ssq3s[q].ap)
                yield
                P.rstd(rs3s[q].ap, ssq3s[q].ap, 1.0 / D, 1e-6, [ssq3s[q]], [rs3s[q]])
                yield
                P.act(xn3[q].ap, a.ap, AF.Copy, [a, rs3s[q]], [xn3[q]], scale=rs3s[q][:, 0:1])
                P.cp("pool", ptb[q].ap, pt_[q].ap, [pt_[q]], [ptb[q]])
                yield
                for hf in range(2):
                    ps = P.psum()
                    for kk in range(4):
                        kc = hf * 4 + kk
                        P.mm(ps[:, kk * 128:(kk + 1) * 128], xn3[q][:, kc * 128:(kc + 1) * 128], identb, True, True,
                             [xn3[q], cstb], [ps])
                    P.tt("dve", x3T[q][:, hf * 4:hf * 4 + 4, :], ps.ap.rearrange("p (a b) -> p a b", a=4),
                         bc(colp[:, CP_GPLE + hf * 4:CP_GPLE + hf * 4 + 4].unsqueeze(2), [128, 4, 128]), ALU.mult,
                         [ps, colp], [x3T[q]])
                    yield
                ps = P.psum()
                for kc in range(2):
                    P.mm(ps[:, kc * 128:(kc + 1) * 128], ptb[q][:, kc * 128:(kc + 1) * 128], identb, True, True,
                         [ptb[q], cstb], [ps])
                P.cp("act", pT[q].ap, ps[:, 0:256].rearrange("p (a b) -> p a b", a=2), [ps], [pT[q]])
                yield
                for hc in range(2):
                    cs = slice(hc * 512, (hc + 1) * 512)
                    ps = P.psum()
                    for kc in range(8):
                        P.mm(ps.ap, x3T[q][:, kc, :], Wpg[:, kc, cs], kc == 0, kc == 7, [x3T[q], Wpg], [ps])
                    P.act(sgp[q][:, cs], ps.ap, AF.Sigmoid, [ps], [sgp[q]])
                    ps = P.psum()
                    for kc in range(2):
                        P.mm(ps.ap, pT[q][:, kc, :], Wpp[:, kc, cs], kc == 0, kc == 1, [pT[q], Wpp], [ps])
                    P.tt("dve", yo[q][:, cs], ps.ap, sgp[q][:, cs], ALU.mult, [ps, sgp[q]], [yo[q]])
                    yield
                P.tt("pool", yo[q].ap, yo[q].ap, a.ap, ALU.add, [yo[q], a], [yo[q]])
                P.dma("sp", y_d[rows, :], yo[q].ap, [yo[q]], [])
                yield
            run_rr4((genP(k) for k in range(16)), 2)
        P.emit(es)
    return nc


def _host_consts():
    c = np.zeros((128, NCST), np.float32)
    i = np.arange(128)
    c[:, C_ID:C_ID + 128] = np.eye(128)
    c[:, C_US:C_US + 128] = (i[:, None] < i[None, :])
    c[:, C_UI:C_UI + 128] = (i[:, None] <= i[None, :])
    c[:, C_LS:C_LS + 128] = (i[:, None] > i[None, :])
    c[:, C_BO:C_BO + 128] = (i[:, None] // 64 == i[None, :] // 64)
    rm = np.zeros((128, 128), np.float32)
    for p in range(128):
        d = p % 64
        if d < 32:
            rm[p + 32, p] = -1.0
        else:
            rm[p - 32, p] = 1.0
    c[:, C_RM:C_RM + 128] = rm
    c[:, C_HS] = (i < 64)
    c[:, C_HS + 1] = (i >= 64)
    c[:, C_ONE:C_ONE + 128] = 1.0
    inv = (10000.0 ** (-np.arange(0, 64, 2, dtype=np.float32) / 64)).astype(np.float32)
    ang = np.arange(S, dtype=np.float32)[:, None] * inv[None, :]
    ang = np.concatenate([ang, ang], -1)
    cos = np.cos(ang).astype(np.float32).T
    sin = np.sin(ang).astype(np.float32).T
    rope = np.concatenate([np.tile(cos, (2, 1)), np.tile(sin, (2, 1))], axis=1)
    return c, np.ascontiguousarray(rope)


_CACHE = {}


def kernel(**inp):
    flags = inp.pop("_flags", {})
    key = tuple(sorted(flags.items()))
    if key not in _CACHE:
        _CACHE[key] = build(flags)
    nc = _CACHE[key]
    f = lambda a: np.ascontiguousarray(np.asarray(a, dtype=np.float32))
    colp = np.zeros((128, NCP), np.float32)
    colp[:, CP_GMIX:CP_GMIX + 8] = f(inp["norm_mix"])[0].reshape(8, 128).T
    colp[:, CP_GPLE:CP_GPLE + 8] = f(inp["norm_ple"])[0].reshape(8, 128).T
    colp[:, CP_W0:CP_W0 + 4] = f(inp["rwkv_w0"])[0].reshape(4, 128).T
    colp[:, CP_A0:CP_A0 + 4] = f(inp["rwkv_a0"])[0].reshape(4, 128).T
    colp[:, CP_KK:CP_KK + 4] = f(inp["rwkv_k_k"])[0].reshape(4, 128).T
    colp[:, CP_KA:CP_KA + 4] = f(inp["rwkv_k_a"])[0].reshape(4, 128).T
    colp[:, CP_RK:CP_RK + 4] = f(inp["rwkv_r_k"])[0].reshape(4, 128).T
    colp[:, CP_QN] = np.tile(f(inp["q_norm"])[0], 2)
    colp[:, CP_KN] = np.tile(f(inp["k_norm"])[0], 2)
    colp[:, CP_MU:CP_MU + 14] = f(inp["rwkv_mu"])[0].reshape(14, 128).T
    colp[:, CP_SUBLN] = f(inp["subln_w"])[0]
    rowb = np.zeros((128, NRB), np.float32)
    rowb[:, RB_GFFN:RB_GFFN + D] = f(inp["norm_ffn"])[0][None, :]
    rowb[:, RB_RBIAS:RB_RBIAS + 4] = f(inp["b_group"])[0][None, :]
    rowb[:, RB_RBIAS + 4:RB_RBIAS + 36] = f(inp["b_expert_router"])[0][None, :]
    rowb[:, RB_LNW:RB_LNW + 512] = f(inp["rwkv_ln_w"])[0][None, :]
    rowb[:, RB_LNB:RB_LNB + 512] = f(inp["rwkv_ln_b"])[0][None, :]
    rowb[:, RB_SUBLN:RB_SUBLN + 128] = f(inp["subln_w"])[0][None, :]
    for k, nm in enumerate(["lambda_q1", "lambda_k1", "lambda_q2", "lambda_k2"]):
        rowb[:, RB_LAM + 64 * k:RB_LAM + 64 * (k + 1)] = f(inp[nm])[0][None, :]
    rowb[:, RB_W0:RB_W0 + 512] = f(inp["rwkv_w0"])[0][None, :]
    murow = np.ascontiguousarray(np.broadcast_to(f(inp["rwkv_mu"])[0][None, :], (128, 1792)))
    cst, rope = _host_consts()
    wr = np.concatenate([f(inp["w_group"])[0], f(inp["w_expert_router"])[0]], axis=1)
    shared = {
        "w_in": f(inp["w_in"])[0], "w2": f(inp["rwkv_w2"])[0], "a2": f(inp["rwkv_a2"])[0],
        "g2": f(inp["rwkv_g2"])[0], "wbr": f(inp["w_branch_rwkv"])[0], "wbd": f(inp["w_branch_diff"])[0],
        "wout": f(inp["w_out"])[0], "wr": np.ascontiguousarray(wr), "w_gate": f(inp["w_gate"])[0],
        "w_up": f(inp["w_up"])[0], "w_down": f(inp["w_down"])[0], "wpg": f(inp["w_ple_gate"])[0],
        "wpp": f(inp["w_ple_proj"])[0], "colp": colp, "rowb": rowb, "cst": cst, "rope": rope, "murow": murow,
    }
    x = f(inp["x"])
    p = f(inp["p"])[0]
    ncores = flags.get("ncores", 8)
    in_maps = [dict(shared, x=x[c], p=p[c]) for c in range(ncores)]
    if flags.get("trace"):
        res = run_bass_kernel_spmd(nc, in_maps, core_ids=list(range(ncores)), trace=True)
        kernel.exec_ns = res.exec_time_ns
    else:
        res = run_bass_kernel_spmd(nc, in_maps, core_ids=list(range(ncores)))
    out = np.stack([np.asarray(r["y"], dtype=np.float32) for r in res.results], axis=0)
    if flags.get("dbgout"):
        kernel.dbg = [(np.asarray(r["orts"]).astype(np.float32), np.asarray(r["odts"]).astype(np.float32)) for r in res.results]
    return out
```
